# Optimizing a Trainium2 kernel written in Bass

```python
import math
import jax, jax.numpy as jnp
from jax import lax
import numpy as np

D_MODEL = 1024
BATCH = 16
SEQ = 4096
DEPTH = 1

CHUNK = 64
Q_BLOCK = 128
HEAD_DIM = 64
N_FOX_HEADS = 8
FOX_WIDTH = N_FOX_HEADS * HEAD_DIM
N_DIFF_HEADS = 4
DIFF_V_DIM = 2 * HEAD_DIM
DIFF_QK_WIDTH = N_DIFF_HEADS * 2 * HEAD_DIM
DIFF_WIDTH = N_DIFF_HEADS * DIFF_V_DIM
N_BRANCHES = 2
ROPE_THETA = 10000.0
COL_FQ = 0
COL_FK = COL_FQ + FOX_WIDTH
COL_FV = COL_FK + FOX_WIDTH
COL_FF = COL_FV + FOX_WIDTH
COL_DQ = COL_FF + N_FOX_HEADS
COL_DK = COL_DQ + DIFF_QK_WIDTH
COL_DV = COL_DK + DIFF_QK_WIDTH
COL_GATE = COL_DV + DIFF_WIDTH
IN_COLS = COL_GATE + N_BRANCHES * D_MODEL
N_GROUPS = 4
EXPERTS_PER_GROUP = 8
N_EXPERTS = N_GROUPS * EXPERTS_PER_GROUP
TOP_K = 2
D_EXPERT = D_MODEL // 2
MOE_BLOCK = 256
EPS = 1e-6
NEG_INF = -1e30

kernel_name = 'chunk_causal_fox_diffattn_hmoe_adaln_block'


def lambda_init(layer_idx):
    return 0.8 - 0.6 * math.exp(-0.3 * layer_idx)


def rms_norm(x, g):
    xf = x.astype(jnp.float32)
    y = xf * lax.rsqrt(jnp.mean(xf * xf, axis=-1, keepdims=True) + EPS)
    return (y * g.astype(jnp.float32)).astype(x.dtype)


def rope(x, cos, sin):
    x1, x2 = jnp.split(x, 2, axis=-1)
    cos = cos.astype(x.dtype)
    sin = sin.astype(x.dtype)
    return jnp.concatenate([x1 * cos - x2 * sin, x2 * cos + x1 * sin], axis=-1)


def split_heads(t, n, d):
    b, s, _ = t.shape
    return t.reshape(b, s, n, d).transpose(0, 2, 1, 3)


def forgetting_attention(q, k, v, log_f):
    s_len = q.shape[2]
    cum = jnp.cumsum(log_f, axis=-1)
    scale = HEAD_DIM ** -0.5
    outs = []
    for i in range(s_len // Q_BLOCK):
        q0, q1 = i * Q_BLOCK, (i + 1) * Q_BLOCK
        logits = jnp.einsum('bhqd,bhkd->bhqk', q[:, :, q0:q1], k[:, :, :q1]).astype(jnp.float32) * scale
        logits = logits + cum[:, :, q0:q1, None] - cum[:, :, None, :q1]
        t_idx = jnp.arange(q0, q1)[:, None]
        s_idx = jnp.arange(q1)[None, :]
        logits = jnp.where(s_idx <= t_idx, logits, NEG_INF)
        p = jax.nn.softmax(logits, axis=-1)
        outs.append(jnp.einsum('bhqk,bhkd->bhqd', p.astype(v.dtype), v[:, :, :q1]))
    return jnp.concatenate(outs, axis=2)


def differential_attention(q, k, v, lam):
    s_len = q.shape[3]
    scale = HEAD_DIM ** -0.5
    outs = []
    for i in range(s_len // Q_BLOCK):
        q0, q1 = i * Q_BLOCK, (i + 1) * Q_BLOCK
        logits = jnp.einsum('bhmqd,bhmkd->bhmqk', q[:, :, :, q0:q1], k[:, :, :, :q1]).astype(jnp.float32) * scale
        t_chunk = (jnp.arange(q0, q1) // CHUNK)[:, None]
        s_chunk = (jnp.arange(q1) // CHUNK)[None, :]
        logits = jnp.where(s_chunk <= t_chunk, logits, NEG_INF)
        p = jax.nn.softmax(logits, axis=-1)
        attn = p[:, :, 0] - lam * p[:, :, 1]
        outs.append(jnp.einsum('bhqk,bhkd->bhqd', attn.astype(v.dtype), v[:, :, :q1]))
    return jnp.concatenate(outs, axis=2)


def hierarchical_route(h, w_rg, b_rg, w_re, b_re):
    t = h.shape[0]
    hf = h.astype(jnp.float32)
    p_group = jax.nn.softmax(hf @ w_rg.astype(jnp.float32) + b_rg.astype(jnp.float32), axis=-1)
    g_w, g_idx = lax.top_k(p_group, 1)
    e_logits = (hf @ w_re.astype(jnp.float32) + b_re.astype(jnp.float32)).reshape(t, N_GROUPS, EXPERTS_PER_GROUP)
    e_logits = jnp.take_along_axis(e_logits, g_idx[:, :, None], axis=1)[:, 0]
    p_exp = jax.nn.softmax(e_logits, axis=-1)
    e_w, e_idx = lax.top_k(p_exp, TOP_K)
    e_w = e_w / jnp.sum(e_w, axis=-1, keepdims=True)
    weights = g_w * e_w
    expert_ids = (g_idx * EXPERTS_PER_GROUP + e_idx).astype(jnp.int32)
    return expert_ids, weights


def moe_forward(h, expert_ids, weights, w1, w3, w2):
    t, d = h.shape
    a = t * TOP_K
    n_blocks = -(-a // MOE_BLOCK) + N_EXPERTS
    p_rows = n_blocks * MOE_BLOCK
    flat_e = expert_ids.reshape(a)
    flat_tok = jnp.arange(a, dtype=jnp.int32) // TOP_K
    order = jnp.argsort(flat_e)
    sorted_e = flat_e[order]
    counts = jnp.bincount(flat_e, length=N_EXPERTS).astype(jnp.int32)
    padded = ((counts + MOE_BLOCK - 1) // MOE_BLOCK) * MOE_BLOCK
    pad_end = jnp.cumsum(padded)
    pad_start = pad_end - padded
    start = jnp.cumsum(counts) - counts
    dest = pad_start[sorted_e] + (jnp.arange(a, dtype=jnp.int32) - start[sorted_e])
    buf_tok = jnp.full((p_rows,), t, dtype=jnp.int32).at[dest].set(flat_tok[order])
    h_pad = jnp.concatenate([h, jnp.zeros((1, d), h.dtype)], axis=0)
    xb = h_pad[buf_tok].reshape(n_blocks, MOE_BLOCK, d)
    block_expert = jnp.minimum(
        jnp.searchsorted(pad_end, jnp.arange(n_blocks, dtype=jnp.int32) * MOE_BLOCK, side='right'),
        N_EXPERTS - 1).astype(jnp.int32)

    def expert_block(args):
        xblk, e = args
        return (jax.nn.silu(xblk @ w1[e]) * (xblk @ w3[e])) @ w2[e]

    yb = lax.map(expert_block, (xb, block_expert)).reshape(p_rows, d)
    dest_orig = jnp.zeros((a,), jnp.int32).at[order].set(dest)
    y_assign = yb[dest_orig].reshape(t, TOP_K, d)
    return jnp.einsum('tkd,tk->td', y_assign, weights.astype(h.dtype))


def setup_inputs(seed: int = 0) -> dict:
    key = jax.random.key(seed)
    ks = jax.random.split(key, 32)
    L, D = DEPTH, D_MODEL

    def nrm(k, shape, scale):
        return jax.random.normal(k, shape, jnp.float32) * scale

    offsets = jax.random.randint(ks[2], (BATCH, 1), 0, 64, dtype=jnp.int32) * CHUNK
    positions = (offsets + jnp.arange(SEQ, dtype=jnp.int32)[None, :]).astype(jnp.int32)
    return {
        'x': nrm(ks[0], (BATCH, SEQ, D), 1.0),
        'c': nrm(ks[1], (BATCH, D), 1.0),
        'positions': positions,
        'w_ada': nrm(ks[3], (L, D, 6 * D), D ** -0.5),
        'b_ada': nrm(ks[4], (L, 6 * D), 0.02),
        'g_norm1': 1.0 + nrm(ks[5], (L, D), 0.02),
        'w_in': nrm(ks[6], (L, D, IN_COLS), D ** -0.5),
        'b_f': 3.0 + nrm(ks[7], (L, N_FOX_HEADS), 0.5),
        'g_q_fox': 1.0 + nrm(ks[8], (L, HEAD_DIM), 0.02),
        'g_k_fox': 1.0 + nrm(ks[9], (L, HEAD_DIM), 0.02),
        'g_q_diff': 1.0 + nrm(ks[10], (L, HEAD_DIM), 0.02),
        'g_k_diff': 1.0 + nrm(ks[11], (L, HEAD_DIM), 0.02),
        'lam_q1': nrm(ks[12], (L, HEAD_DIM), 0.1),
        'lam_k1': nrm(ks[13], (L, HEAD_DIM), 0.1),
        'lam_q2': nrm(ks[14], (L, HEAD_DIM), 0.1),
        'lam_k2': nrm(ks[15], (L, HEAD_DIM), 0.1),
        'g_subln': 1.0 + nrm(ks[16], (L, DIFF_V_DIM), 0.02),
        'w_proj_fox': nrm(ks[17], (L, FOX_WIDTH, D), FOX_WIDTH ** -0.5),
        'w_proj_diff': nrm(ks[18], (L, DIFF_WIDTH, D), DIFF_WIDTH ** -0.5),
        'w_out': nrm(ks[19], (L, D, D), D ** -0.5),
        'g_norm2': 1.0 + nrm(ks[20], (L, D), 0.02),
        'w_router_group': nrm(ks[21], (L, D, N_GROUPS), D ** -0.5),
        'b_router_group': nrm(ks[22], (L, N_GROUPS), 0.01),
        'w_router_expert': nrm(ks[23], (L, D, N_EXPERTS), D ** -0.5),
        'b_router_expert': nrm(ks[24], (L, N_EXPERTS), 0.01),
        'w1': nrm(ks[25], (L, N_EXPERTS, D, D_EXPERT), D ** -0.5),
        'w3': nrm(ks[26], (L, N_EXPERTS, D, D_EXPERT), D ** -0.5),
        'w2': nrm(ks[27], (L, N_EXPERTS, D_EXPERT, D), D_EXPERT ** -0.5),
    }


def reference(x, c, positions, w_ada, b_ada, g_norm1, w_in, b_f, g_q_fox, g_k_fox,
              g_q_diff, g_k_diff, lam_q1, lam_k1, lam_q2, lam_k2, g_subln,
              w_proj_fox, w_proj_diff, w_out, g_norm2, w_router_group, b_router_group,
              w_router_expert, b_router_expert, w1, w3, w2):
    b, s, d = x.shape
    inv_freq = ROPE_THETA ** (-jnp.arange(0, HEAD_DIM, 2, dtype=jnp.float32) / HEAD_DIM)
    ang = positions.astype(jnp.float32)[..., None] * inv_freq
    cos = jnp.cos(ang)[:, None, None]
    sin = jnp.sin(ang)[:, None, None]
    c_act = jax.nn.silu(c)

    for l in range(DEPTH):
        lam0 = lambda_init(l)
        mod = c_act @ w_ada[l] + b_ada[l]
        sh1, sc1, gt1, sh2, sc2, gt2 = [m[:, None, :] for m in jnp.split(mod, 6, axis=-1)]

        h = rms_norm(x, g_norm1[l]) * (1.0 + sc1) + sh1
        z = h @ w_in[l]

        q_f = rms_norm(split_heads(z[..., COL_FQ:COL_FK], N_FOX_HEADS, HEAD_DIM), g_q_fox[l])
        k_f = rms_norm(split_heads(z[..., COL_FK:COL_FV], N_FOX_HEADS, HEAD_DIM), g_k_fox[l])
        v_f = split_heads(z[..., COL_FV:COL_FF], N_FOX_HEADS, HEAD_DIM)
        log_f = jax.nn.log_sigmoid((z[..., COL_FF:COL_DQ] + b_f[l]).astype(jnp.float32)).transpose(0, 2, 1)
        o_f = forgetting_attention(q_f, k_f, v_f, log_f)
        o_f = o_f.transpose(0, 2, 1, 3).reshape(b, s, FOX_WIDTH)

        q_d = z[..., COL_DQ:COL_DK].reshape(b, s, N_DIFF_HEADS, 2, HEAD_DIM).transpose(0, 2, 3, 1, 4)
        k_d = z[..., COL_DK:COL_DV].reshape(b, s, N_DIFF_HEADS, 2, HEAD_DIM).transpose(0, 2, 3, 1, 4)
        q_d = rope(rms_norm(q_d, g_q_diff[l]), cos, sin)
        k_d = rope(rms_norm(k_d, g_k_diff[l]), cos, sin)
        v_d = split_heads(z[..., COL_DV:COL_GATE], N_DIFF_HEADS, DIFF_V_DIM)
        lam = (jnp.exp(jnp.sum(lam_q1[l].astype(jnp.float32) * lam_k1[l].astype(jnp.float32)))
               - jnp.exp(jnp.sum(lam_q2[l].astype(jnp.float32) * lam_k2[l].astype(jnp.float32)))
               + lam0)
        o_d = differential_attention(q_d, k_d, v_d, lam)
        o_d = rms_norm(o_d, g_subln[l]) * (1.0 - lam0)
        o_d = o_d.transpose(0, 2, 1, 3).reshape(b, s, DIFF_WIDTH)

        gates = jax.nn.sigmoid(z[..., COL_GATE:]).reshape(b, s, N_BRANCHES, d)
        merged = gates[:, :, 0] * (o_f @ w_proj_fox[l]) + gates[:, :, 1] * (o_d @ w_proj_diff[l])
        x = x + gt1 * (merged @ w_out[l])

        h2 = (rms_norm(x, g_norm2[l]) * (1.0 + sc2) + sh2).reshape(b * s, d)
        expert_ids, weights = hierarchical_route(h2, w_router_group[l], b_router_group[l],
                                                 w_router_expert[l], b_router_expert[l])
        y_moe = moe_forward(h2, expert_ids, weights, w1[l], w3[l], w2[l]).reshape(b, s, d)
        x = x + gt2 * y_moe
    return x
```

```python
import contextlib
import math
import numpy as np
import concourse.bass as bass
import concourse.mybir as mybir
from concourse.bass_utils import run_bass_kernel_spmd

F32 = mybir.dt.float32
BF16 = mybir.dt.bfloat16
I32 = mybir.dt.int32
U32 = mybir.dt.uint32
AF = mybir.ActivationFunctionType
ALU = mybir.AluOpType
AX = mybir.AxisListType

D = 1024
KC = 8
HD = 64
IN_COLS = 5128
C_FQ, C_FK, C_FV, C_FF, C_DQ, C_DK, C_DV, C_G = 0, 512, 1024, 1536, 1544, 2056, 2568, 3080
NEXP = 32
DEXP = 512
EPS = 1e-6
BS = 256
LAM0 = 0.8 - 0.6 * math.exp(-0.3 * 0)
N_CORES = 8


class Res:
    __slots__ = ("name", "w", "r")

    def __init__(self, name):
        self.name = name
        self.w = {}
        self.r = {}


class T:
    def __init__(self, t, name):
        self.t = t
        self.res = Res(name)

    def __getitem__(self, k):
        return self.t[k]


class Sched:
    EPOCH = 30000

    def __init__(self, nc, es):
        self.nc = nc
        self.es = es
        self.eng = {"pe": nc.tensor, "act": nc.scalar, "dve": nc.vector, "pool": nc.gpsimd, "sp": nc.sync}
        self.sem = {}
        self.cnt = {}
        self.seen = {k: {} for k in self.eng}
        self.all_sems = {}
        for k in self.eng:
            self._new_eng_sem(k)
        self.dsem = {}
        self.dcnt = {}
        self.n_ins = 0
        self.bg_keys = set()

    def _new_eng_sem(self, k):
        s = self.es.enter_context(self.nc.semaphore("s_%s_%d" % (k, len(self.all_sems))))
        self.sem[k] = s
        self.cnt[k] = 0
        self.all_sems[s] = 0

    def _res(self, lst):
        return [x.res if isinstance(x, T) else x for x in lst]

    def _need(self, reads, writes, accum):
        need = {}
        for r in reads:
            for s, v in r.w.items():
                if need.get(s, 0) < v:
                    need[s] = v
        for w in writes:
            for dct in (w.w, w.r):
                for s, v in dct.items():
                    if need.get(s, 0) < v:
                        need[s] = v
        for w in accum:
            for s, v in w.r.items():
                if need.get(s, 0) < v:
                    need[s] = v
        return need

    def _emit_waits(self, e, need):
        eng = self.eng[e]
        seen = self.seen[e]
        waits = [(s, v) for s, v in need.items() if seen.get(s, 0) < v]
        for s, v in waits:
            seen[s] = v
        return eng, waits

    def _record(self, ev_s, ev_v, reads, writes, accum):
        self.all_sems[ev_s] = ev_v
        for r in reads:
            if r.r.get(ev_s, 0) < ev_v:
                r.r[ev_s] = ev_v
        for w in writes:
            w.w = {ev_s: ev_v}
            w.r = {}
        for w in accum:
            if w.w.get(ev_s, 0) < ev_v:
                w.w[ev_s] = ev_v

    def op(self, e, fn, reads=(), writes=(), accum=()):
        reads, writes, accum = self._res(reads), self._res(writes), self._res(accum)
        if self.cnt[e] >= self.EPOCH:
            self._new_eng_sem(e)
        need = self._need(reads, writes, accum)
        eng, waits = self._emit_waits(e, need)
        for s, v in waits:
            eng.wait_ge(s, v)
        ins = fn(eng)
        self.cnt[e] += 1
        ins.then_inc(self.sem[e], 1)
        self._record(self.sem[e], self.cnt[e], reads, writes, accum)
        self.n_ins += 1
        return ins

    def dma(self, q, key, fn, reads=(), writes=(), accum=()):
        reads, writes, accum = self._res(reads), self._res(writes), self._res(accum)
        if key not in self.dsem:
            self.dsem[key] = self.es.enter_context(self.nc.semaphore("d_" + key))
            self.dcnt[key] = 0
        need = self._need(reads, writes, accum)
        eng, waits = self._emit_waits(q, need)
        for s, v in waits:
            eng.wait_ge(s, v)
        ins = fn(eng)
        self.dcnt[key] += 16
        s = self.dsem[key]
        ins.then_inc(s, 16)
        self._record(s, self.dcnt[key], reads, writes, accum)
        self.n_ins += 1
        return ins

    def barrier(self):
        skip = {self.dsem[k] for k in self.bg_keys if k in self.dsem}
        allv = {s: v for s, v in self.all_sems.items() if s not in skip}
        for e in self.eng:
            eng, waits = self._emit_waits(e, allv)
            for s, v in waits:
                if v > 0:
                    eng.wait_ge(s, v)

    def final_wait(self):
        eng, waits = self._emit_waits("sp", dict(self.all_sems))
        for s, v in waits:
            if v > 0:
                eng.wait_ge(s, v)


def build_nc(NB, TT, dbg=False, upto=5):
    NT = TT // 128
    NG = TT // 512
    NTOK = NB * TT
    NTT = NTOK // 128
    NBLK = (NTOK * 2) // BS + NEXP
    PROWS = NBLK * BS
    nc = bass.Bass("TRN2", target_bir_lowering=False)

    def din(name, shape, dt=F32):
        return nc.dram_tensor(name, list(shape), dt, kind="ExternalInput").ap()

    DBG_OUT = ("modv", "qTd", "kTd", "qTf", "x1s", "h2s")

    def dscr(name, shape, dt):
        return nc.dram_tensor(name, list(shape), dt, kind=("ExternalOutput" if (dbg and name in DBG_OUT) else "Internal")).ap()

    x = din("x", [NB, TT, D])
    cin = din("c", [NB, D])
    pos = din("positions", [NB, TT], I32)
    w_ada = din("w_ada", [D, 6 * D])
    b_ada = din("b_ada", [1, 6 * D])
    g_norm1 = din("g_norm1", [1, D])
    w_in = din("w_in", [D, IN_COLS])
    b_f = din("b_f", [1, 8])
    g_q_fox = din("g_q_fox", [1, HD])
    g_k_fox = din("g_k_fox", [1, HD])
    g_q_diff = din("g_q_diff", [1, HD])
    g_k_diff = din("g_k_diff", [1, HD])
    lam_q1 = din("lam_q1", [1, HD])
    lam_k1 = din("lam_k1", [1, HD])
    lam_q2 = din("lam_q2", [1, HD])
    lam_k2 = din("lam_k2", [1, HD])
    g_subln = din("g_subln", [1, 128])
    w_pf = din("w_proj_fox", [512, D])
    w_pd = din("w_proj_diff", [512, D])
    w_out = din("w_out", [D, D])
    g_norm2 = din("g_norm2", [1, D])
    w_rg = din("w_router_group", [D, 4])
    b_rg = din("b_router_group", [1, 4])
    w_re = din("w_router_expert", [D, 32])
    b_re = din("b_router_expert", [1, 32])
    w1 = din("w1", [NEXP * D, DEXP])
    w3 = din("w3", [NEXP * D, DEXP])
    w2 = din("w2", [NEXP * DEXP, D])
    invf = din("invf", [1, 32])
    out = nc.dram_tensor("out", [NB, TT, D], F32, kind="ExternalOutput").ap()

    modv = dscr("modv", [NB, 6, D], F32)
    qTf = dscr("qTf", [NB, 8, 65, TT], BF16)
    kTf = dscr("kTf", [NB, 8, 65, TT], BF16)
    qTd = dscr("qTd", [NB, 8, 64, TT], BF16)
    kTd = dscr("kTd", [NB, 8, 64, TT], BF16)
    vF = dscr("vF", [NB, TT, 8 * 65], BF16)
    vD = dscr("vD", [NB, TT, 4 * 129], BF16)
    gl = dscr("gl", [NB, TT, 2048], BF16)
    x1s = dscr("x1s", [NTOK, D], F32)
    h2s = dscr("h2s", [NTOK, D], BF16)
    w1c = dscr("w1c", [NEXP * D // 4, 2048], BF16)
    w3c = dscr("w3c", [NEXP * D // 4, 2048], BF16)
    w2c = dscr("w2c", [NEXP * DEXP // 2, 2048], BF16)
    xbuf = dscr("xbuf", [PROWS, D], BF16)
    ybuf = dscr("ybuf", [PROWS, D], BF16)
    dbg_t = {}
    if dbg:
        dbg_t["oc"] = nc.dram_tensor("dbg_oc", [NB, TT, D], BF16, kind="ExternalOutput").ap()
        dbg_t["rt"] = nc.dram_tensor("dbg_rt", [128, NTT, 8], F32, kind="ExternalOutput").ap()
        dbg_t["be"] = nc.dram_tensor("dbg_be", [128, NBLK + 64], F32, kind="ExternalOutput").ap()

    es = contextlib.ExitStack()
    with es:
        S = Sched(nc, es)

        uid = [0]

        def sb(stack, name, shape, dt):
            uid[0] += 1
            name = "%s_%d" % (name, uid[0])
            return T(stack.enter_context(nc.sbuf_tensor(name, list(shape), dt)), name)

        def ps(stack, name, shape, dt):
            uid[0] += 1
            name = "%s_%d" % (name, uid[0])
            return T(stack.enter_context(nc.psum_tensor(name, list(shape), dt)), name)

        ident_b = sb(es, "ident_b", [128, 128], BF16)
        ident_f = sb(es, "ident_f", [128, 128], F32)
        tri_i = sb(es, "tri_i", [128, 128], F32)
        tri_s = sb(es, "tri_s", [128, 128], F32)
        ones_f = sb(es, "ones_f", [128, 128], F32)
        G4 = sb(es, "G4", [128, 4, HD], F32)
        bf_rep = sb(es, "bf_rep", [128, 8], F32)
        gsub = sb(es, "gsub", [128, 128], F32)
        nlam = sb(es, "nlam", [128, 1], F32)
        invf_rep = sb(es, "invf_rep", [128, 32], F32)
        ncum = sb(es, "ncum", [128, NB, NT, 8], F32)
        eid_all = sb(es, "eid_all", [128, NTT, 2], F32)
        rk_all = sb(es, "rk_all", [128, NTT, 2], F32)
        wt_all = sb(es, "wt_all", [128, NTT, 2], F32)
        dest_i = sb(es, "dest_i", [128, NTT, 2], U32)
        ecarry = sb(es, "ecarry", [128, 32], F32)
        iota32 = sb(es, "iota32", [128, 32], F32)
        ztile = sb(es, "ztile", [128, D], BF16)
        xz = Res("xbuf_zero")
        S.bg_keys.add("ztile")
        blkidx = {"idx1": sb(es, "idx1", [128, NBLK, 2], U32)}

        def bcast_rows(ap, n):
            return ap.partition_broadcast(n)

        def setup():
            with contextlib.ExitStack() as st:
                zf = sb(st, "zf", [128, 128], F32)
                S.op("pool", lambda e: e.memset(zf[:], 0.0), writes=[zf])
                S.op("pool", lambda e: e.memset(ones_f[:], 1.0), writes=[ones_f])
                S.op("pool", lambda e: e.affine_select(out=ident_f[:], in_=zf[:], pattern=[[-1, 128]],
                                                       compare_op=ALU.not_equal, fill=1.0, base=0,
                                                       channel_multiplier=1), reads=[zf], writes=[ident_f])
                S.op("pool", lambda e: e.affine_select(out=tri_i[:], in_=ones_f[:], pattern=[[1, 128]],
                                                       compare_op=ALU.is_ge, fill=0.0, base=0,
                                                       channel_multiplier=-1), reads=[ones_f], writes=[tri_i])
                S.op("pool", lambda e: e.affine_select(out=tri_s[:], in_=ones_f[:], pattern=[[1, 128]],
                                                       compare_op=ALU.is_ge, fill=0.0, base=-1,
                                                       channel_multiplier=-1), reads=[ones_f], writes=[tri_s])
                S.op("dve", lambda e: e.tensor_copy(out=ident_b[:], in_=ident_f[:]), reads=[ident_f], writes=[ident_b])
                S.op("pool", lambda e: e.memset(ecarry[:], 0.0), writes=[ecarry])
                S.op("pool", lambda e: e.memset(ztile[:], 0.0), writes=[ztile])
                xbv = xbuf.rearrange("(r p) d -> p r d", p=128)
                for r0 in range(PROWS // 128):
                    S.dma("sp", "ztile", lambda e, r0=r0: e.dma_start(out=xbv[:, r0, :], in_=ztile[:]), reads=[ztile], accum=[xz])
                io_i = sb(st, "io_i", [128, 32], I32)
                S.op("pool", lambda e: e.iota(io_i[:], pattern=[[1, 32]], base=0, channel_multiplier=0), writes=[io_i])
                S.op("dve", lambda e: e.tensor_copy(out=iota32[:], in_=io_i[:]), reads=[io_i], writes=[iota32])
                for i, (g, sc) in enumerate([(g_q_fox, 0.125), (g_k_fox, 1.0), (g_q_diff, 0.125), (g_k_diff, 1.0)]):
                    S.dma("sp", "G4", lambda e, g=g, i=i: e.dma_start(out=G4[:, i, :], in_=bcast_rows(g[0:1, :], 128)),
                          accum=[G4])
                S.dma("sp", "bf", lambda e: e.dma_start(out=bf_rep[:], in_=bcast_rows(b_f[0:1, :], 128)), writes=[bf_rep])
                S.dma("sp", "gsub", lambda e: e.dma_start(out=gsub[:], in_=bcast_rows(g_subln[0:1, :], 128)), writes=[gsub])
                S.dma("sp", "invf", lambda e: e.dma_start(out=invf_rep[:], in_=bcast_rows(invf[0:1, :], 128)), writes=[invf_rep])
                S.op("dve", lambda e: e.tensor_scalar(out=G4[:, 0, :], in0=G4[:, 0, :], scalar1=0.125, scalar2=None,
                                                      op0=ALU.mult), reads=[G4], writes=[G4])
                S.op("dve", lambda e: e.tensor_scalar(out=G4[:, 2, :], in0=G4[:, 2, :], scalar1=0.125, scalar2=None,
                                                      op0=ALU.mult), reads=[G4], writes=[G4])
                S.op("dve", lambda e: e.tensor_scalar(out=gsub[:], in0=gsub[:], scalar1=1.0 - LAM0, scalar2=None,
                                                      op0=ALU.mult), reads=[gsub], writes=[gsub])
                lv = sb(st, "lv", [128, 4, HD], F32)
                for i, g in enumerate([lam_q1, lam_k1, lam_q2, lam_k2]):
                    S.dma("sp", "lv", lambda e, g=g, i=i: e.dma_start(out=lv[:, i, :], in_=bcast_rows(g[0:1, :], 128)),
                          accum=[lv])
                lp = sb(st, "lp", [128, 2, HD], F32)
                ls = sb(st, "ls", [128, 2], F32)
                S.op("dve", lambda e: e.tensor_tensor(out=lp[:, 0, :], in0=lv[:, 0, :], in1=lv[:, 1, :], op=ALU.mult),
                     reads=[lv], writes=[lp])
                S.op("dve", lambda e: e.tensor_tensor(out=lp[:, 1, :], in0=lv[:, 2, :], in1=lv[:, 3, :], op=ALU.mult),
                     reads=[lv, lp], accum=[lp])
                S.op("dve", lambda e: e.tensor_reduce(out=ls[:], in_=lp[:], axis=AX.X, op=ALU.add), reads=[lp], writes=[ls])
                S.op("act", lambda e: e.activation(out=ls[:], in_=ls[:], func=AF.Exp), reads=[ls], writes=[ls])
                S.op("dve", lambda e: e.scalar_tensor_tensor(out=nlam[:], in0=ls[:, 1:2], scalar=-LAM0, in1=ls[:, 0:1],
                                                             op0=ALU.add, op1=ALU.subtract), reads=[ls], writes=[nlam])
                cc = sb(st, "cc", [128, 8, NB], F32)
                with nc.allow_non_contiguous_dma(reason="tiny one-time transposed load of c"):
                    for bb in range(NB):
                        S.dma("sp", "cc", lambda e, bb=bb: e.dma_start(out=cc[:, :, bb:bb + 1],
                                                                  in_=cin[bb:bb + 1, :].rearrange("b (kc k) -> k kc b", k=128)),
                              accum=[cc])
                with contextlib.ExitStack() as pst:
                    pc = cc
                    pm = ps(pst, "pm", [128, 512], F32)
                    cact = sb(st, "cact", [128, 8, NB], F32)
                    S.op("act", lambda e: e.activation(out=cact[:], in_=pc[:], func=AF.Silu), reads=[pc], writes=[cact])
                    bad = sb(st, "bad", [128, 6 * D], F32)
                    S.dma("sp", "bad", lambda e: e.dma_start(out=bad[:], in_=bcast_rows(b_ada[0:1, :], 128)), writes=[bad])
                    gg = sb(st, "gg", [128, 2, D], F32)
                    S.dma("sp", "gg", lambda e: e.dma_start(out=gg[:, 0, :], in_=bcast_rows(g_norm1[0:1, :], 128)), accum=[gg])
                    S.dma("sp", "gg", lambda e: e.dma_start(out=gg[:, 1, :], in_=bcast_rows(g_norm2[0:1, :], 128)), accum=[gg])
                    was = [sb(st, "wa%d" % i, [128, 8, 512], BF16) for i in range(2)]
                    creps = [sb(st, "crep%d" % i, [128, 8, 128], BF16) for i in range(NB)]
                    mods = [sb(st, "mod%d" % i, [128, 6 * D], F32) for i in range(NB)]
                    for bb in range(NB):
                        S.op("dve", lambda e, bb=bb: e.tensor_copy(out=creps[bb][:], in_=cact[:, :, bb:bb + 1].to_broadcast([128, 8, 128])),
                             reads=[cact], writes=[creps[bb]])
                    for j in range(12):
                        wa = was[j % 2]
                        S.dma("pool", "wa%d" % (j % 2),
                              lambda e, wa=wa, j=j: e.dma_start(
                                  out=wa[:], in_=w_ada[:, j * 512:(j + 1) * 512].rearrange("(kc k) n -> k kc n", k=128)),
                              writes=[wa])
                        for bb in range(NB):
                            for kc in range(KC):
                                S.op("pe", lambda e, wa=wa, kc=kc, bb=bb: e.matmul(pm[:], lhsT=creps[bb][:, kc, :], rhs=wa[:, kc, :],
                                                                                   start=(kc == 0), stop=(kc == KC - 1)),
                                     reads=[creps[bb], wa], accum=[pm])
                            S.op("dve", lambda e, j=j, bb=bb: e.tensor_tensor(out=mods[bb][:, j * 512:(j + 1) * 512], in0=pm[:],
                                                                              in1=bad[:, j * 512:(j + 1) * 512], op=ALU.add),
                                 reads=[pm, bad], accum=[mods[bb]])
                    for bb in range(NB):
                        mod = mods[bb]
                        for (dst, src, kind) in [(0, 1, "A1"), (1, 0, "c"), (2, 2, "c"), (3, 4, "A2"), (4, 3, "c"), (5, 5, "c")]:
                            if kind == "c":
                                S.dma("sp", "mod%d" % bb, lambda e, dst=dst, src=src, bb=bb, mod=mod: e.dma_start(
                                    out=modv[bb, dst:dst + 1, :], in_=mod[0:1, src * D:(src + 1) * D]), reads=[mod])
                            else:
                                gi = 0 if kind == "A1" else 1
                                S.op("dve", lambda e, src=src, gi=gi, mod=mod: e.scalar_tensor_tensor(
                                    out=mod[:, src * D:(src + 1) * D], in0=mod[:, src * D:(src + 1) * D], scalar=1.0, in1=gg[:, gi, :],
                                    op0=ALU.add, op1=ALU.mult), reads=[mod, gg], writes=[mod])
                                S.dma("sp", "mod%d" % bb, lambda e, dst=dst, src=src, bb=bb, mod=mod: e.dma_start(
                                    out=modv[bb, dst:dst + 1, :], in_=mod[0:1, src * D:(src + 1) * D]), reads=[mod])
                S.barrier()

        setup()

        modv_res = Res("modv")

        def load_mod_row(stack, name, b, i):
            t = sb(stack, name, [128, D], F32)
            S.dma("sp", name, lambda e: e.dma_start(out=t[:], in_=bcast_rows(modv[b, i:i + 1, :], 128)), writes=[t])
            return t

        def phase1(b):
            with contextlib.ExitStack() as st:
                win = sb(st, "win", [128, KC, IN_COLS], BF16)
                for kc in range(KC):
                    for c0 in range(0, IN_COLS, 1024):
                        c1 = min(c0 + 1024, IN_COLS)
                        S.dma("pool", "win", lambda e, kc=kc, c0=c0, c1=c1: e.dma_start(
                            out=win[:, kc, c0:c1], in_=w_in[kc * 128:(kc + 1) * 128, c0:c1]), accum=[win])
                A1 = load_mod_row(st, "A1", b, 0)
                B1 = load_mod_row(st, "B1", b, 1)
                cosT = sb(st, "cosT", [128, NT, 32], F32)
                sinT = sb(st, "sinT", [128, NT, 32], F32)
                with contextlib.ExitStack() as st2:
                    pi_ = sb(st2, "pi_", [128, NT], I32)
                    with nc.allow_non_contiguous_dma(reason="one-time transposed load of positions"):
                        S.dma("sp", "pi", lambda e: e.dma_start(out=pi_[:], in_=pos[b, :].rearrange("(t p) -> p t", p=128)), writes=[pi_])
                    posf = sb(st2, "posf", [128, NT], F32)
                    S.op("dve", lambda e: e.tensor_copy(out=posf[:], in_=pi_[:]), reads=[pi_], writes=[posf])
                    ang = sb(st2, "ang", [128, NT, 32], F32)
                    S.op("dve", lambda e: e.tensor_tensor(out=ang[:], in0=posf[:].unsqueeze(2).to_broadcast([128, NT, 32]),
                                                          in1=invf_rep[:].unsqueeze(1).to_broadcast([128, NT, 32]), op=ALU.mult),
                         reads=[posf, invf_rep], writes=[ang])
                    tq = sb(st2, "tq", [128, NT, 32], F32)
                    nq = sb(st2, "nq", [128, NT, 32], F32)
                    MAGIC = 12582912.0
                    C1 = 6.28125
                    C2 = 2.0 * math.pi - 6.28125
                    for (dst, shift) in [(sinT, 0.0), (cosT, math.pi / 2)]:
                        S.op("dve", lambda e, shift=shift: e.tensor_scalar(out=tq[:], in0=ang[:], scalar1=shift, scalar2=None,
                                                                          op0=ALU.add), reads=[ang], writes=[tq])
                        S.op("dve", lambda e: e.tensor_scalar(out=nq[:], in0=tq[:], scalar1=1.0 / (2 * math.pi), scalar2=MAGIC,
                                                              op0=ALU.mult, op1=ALU.add), reads=[tq], writes=[nq])
                        S.op("dve", lambda e: e.tensor_scalar(out=nq[:], in0=nq[:], scalar1=-MAGIC, scalar2=None,
                                                              op0=ALU.add), reads=[nq], writes=[nq])
                        S.op("dve", lambda e: e.scalar_tensor_tensor(out=tq[:], in0=nq[:], scalar=-C1, in1=tq[:],
                                                                     op0=ALU.mult, op1=ALU.add), reads=[nq, tq], writes=[tq])
                        S.op("dve", lambda e: e.scalar_tensor_tensor(out=tq[:], in0=nq[:], scalar=-C2, in1=tq[:],
                                                                     op0=ALU.mult, op1=ALU.add), reads=[nq, tq], writes=[tq])
                        S.op("dve", lambda e: e.tensor_scalar(out=tq[:], in0=tq[:], scalar1=3.1415925, scalar2=-3.1415925,
                                                              op0=ALU.min, op1=ALU.max), reads=[tq], writes=[tq])
                        S.op("act", lambda e, dst=dst: e.activation(out=dst[:], in_=tq[:], func=AF.Sin), reads=[tq], writes=[dst])
                S.barrier()
                NBUF = 2
                xt = [sb(st, "xt%d" % i, [128, D], F32) for i in range(NBUF)]
                tmp = [sb(st, "tmp%d" % i, [128, D], F32) for i in range(NBUF)]
                hb = [sb(st, "hb%d" % i, [128, D], BF16) for i in range(NBUF)]
                hT = [sb(st, "hT%d" % i, [128, KC, 128], BF16) for i in range(NBUF)]
                sq = sb(st, "sq", [128, D], F32)
                ss = [sb(st, "ss%d" % i, [128, 4], F32) for i in range(NBUF)]
                ms8 = [sb(st, "ms8_%d" % i, [128, 8], F32) for i in range(4)]
                qn = [sb(st, "qn%d" % i, [128, 8, HD], F32) for i in range(2)]
                qg = [sb(st, "qg%d" % i, [128, 8, HD], F32) for i in range(2)]
                ra = [sb(st, "ra%d" % i, [128, 8, 32], F32) for i in range(4)]
                QKF = [sb(st, "QKF%d" % i, [128, 16, 66], BF16) for i in range(NBUF)]
                QKD = [sb(st, "QKD%d" % i, [128, 16, HD], BF16) for i in range(NBUF)]
                QTs = [sb(st, "QTs%d" % i, [65, 32, 128], BF16) for i in range(NBUF)]
                VF = [sb(st, "VF%d" % i, [128, 8, 65], BF16) for i in range(NBUF)]
                VD = [sb(st, "VD%d" % i, [128, 4, 129], BF16) for i in range(NBUF)]
                GT = [sb(st, "GT%d" % i, [128, 2048], BF16) for i in range(NBUF)]
                fz = [sb(st, "fz%d" % i, [128, 8], F32) for i in range(3)]
                carry = sb(st, "carry", [128, 8], F32)
                S.op("pool", lambda e: e.memset(carry[:], 0.0), writes=[carry])
                for i in range(NBUF):
                    S.op("pool", lambda e, i=i: e.memset(QKF[i][:], 0.0), writes=[QKF[i]])
                    S.op("pool", lambda e, i=i: e.memset(QKF[i][:, 8:16, 64:65], 1.0), reads=[QKF[i]], accum=[QKF[i]])
                    S.op("pool", lambda e, i=i: e.memset(VF[i][:, :, 64:65], 1.0), writes=[VF[i]])
                    S.op("pool", lambda e, i=i: e.memset(VD[i][:, :, 128:129], 1.0), writes=[VD[i]])
                pT = ps(st, "pT", [128, KC, 128], BF16)
                zb = [ps(st, "zb%d" % i, [128, 512], F32) for i in range(3)]
                psm = ps(st, "psm", [128, 16], F32)
                qtp = ps(st, "qtp", [65, 16, 128], BF16)
                zbi = [0]

                def zgroup(hTt, col0, ncols):
                    z = zb[zbi[0] % 3]
                    zbi[0] += 1
                    for kc in range(KC):
                        S.op("pe", lambda e, kc=kc: e.matmul(z[:, 0:ncols], lhsT=hTt[:, kc, :], rhs=win[:, kc, col0:col0 + ncols],
                                                             start=(kc == 0), stop=(kc == KC - 1)),
                             reads=[hTt, win], accum=[z])
                    return z

                def headnorm(z, gi, mi, dst_fn, rope_t=None):
                    m = ms8[mi]
                    S.op("act", lambda e: e.activation(out=sq[:, 0:512], in_=z[:], func=AF.Square), reads=[z], writes=[sq])
                    S.op("dve", lambda e: e.tensor_reduce(out=m[:], in_=sq[:, 0:512].rearrange("p (h d) -> p h d", d=HD),
                                                          axis=AX.X, op=ALU.add), reads=[sq], writes=[m])
                    S.op("act", lambda e: e.activation(out=m[:], in_=m[:], func=AF.Ln, scale=1.0 / HD, bias=EPS), reads=[m], writes=[m])
                    S.op("act", lambda e: e.activation(out=m[:], in_=m[:], func=AF.Exp, scale=-0.5), reads=[m], writes=[m])
                    q1 = qn[mi % 2]
                    S.op("dve", lambda e: e.tensor_tensor(out=q1[:], in0=z[:].rearrange("p (h d) -> p h d", d=HD),
                                                          in1=m[:].unsqueeze(2).to_broadcast([128, 8, HD]), op=ALU.mult),
                         reads=[z, m], writes=[q1])
                    gb = G4[:, gi, :].unsqueeze(1).to_broadcast([128, 8, HD])
                    if rope_t is None:
                        S.op("pool", lambda e: e.tensor_tensor(out=dst_fn(0, HD), in0=q1[:], in1=gb, op=ALU.mult),
                             reads=[q1, G4], accum=[dst_fn.tile])
                    else:
                        q2 = qg[mi % 2]
                        S.op("pool", lambda e: e.tensor_tensor(out=q2[:], in0=q1[:], in1=gb, op=ALU.mult),
                             reads=[q1, G4], writes=[q2])
                        cb = cosT[:, rope_t, :].unsqueeze(1).to_broadcast([128, 8, 32])
                        sbb = sinT[:, rope_t, :].unsqueeze(1).to_broadcast([128, 8, 32])
                        x1 = q2[:, :, 0:32]
                        x2 = q2[:, :, 32:64]
                        S.op("dve", lambda e: e.tensor_tensor(out=ra[0][:], in0=x1, in1=cb, op=ALU.mult), reads=[q2, cosT], writes=[ra[0]])
                        S.op("pool", lambda e: e.tensor_tensor(out=ra[1][:], in0=x2, in1=sbb, op=ALU.mult), reads=[q2, sinT], writes=[ra[1]])
                        S.op("dve", lambda e: e.tensor_tensor(out=ra[2][:], in0=x2, in1=cb, op=ALU.mult), reads=[q2, cosT], writes=[ra[2]])
                        S.op("pool", lambda e: e.tensor_tensor(out=ra[3][:], in0=x1, in1=sbb, op=ALU.mult), reads=[q2, sinT], writes=[ra[3]])
                        S.op("dve", lambda e: e.tensor_tensor(out=dst_fn(0, 32), in0=ra[0][:], in1=ra[1][:], op=ALU.subtract),
                             reads=[ra[0], ra[1]], accum=[dst_fn.tile])
                        S.op("pool", lambda e: e.tensor_tensor(out=dst_fn(32, 64), in0=ra[2][:], in1=ra[3][:], op=ALU.add),
                             reads=[ra[2], ra[3]], accum=[dst_fn.tile])

                def mkdst(tile, h0):
                    def f(a, bb):
                        return tile[:, h0:h0 + 8, a:bb]
                    f.tile = tile
                    return f

                def load_x(t):
                    i = t % NBUF
                    S.dma("sp", "xt%d" % i, lambda e: e.dma_start(out=xt[i][:], in_=x[b, t * 128:(t + 1) * 128, :]), writes=[xt[i]])

                def pre_a(t):
                    i = t % NBUF
                    ssi = ss[i]
                    S.op("act", lambda e: e.activation(out=sq[:], in_=xt[i][:], func=AF.Square, accum_out=ssi[:, 0:1]),
                         reads=[xt[i]], writes=[sq, ssi])
                    S.op("act", lambda e: e.activation(out=ssi[:, 1:2], in_=ssi[:, 0:1], func=AF.Ln, scale=1.0 / D, bias=EPS),
                         reads=[ssi], accum=[ssi])
                    S.op("act", lambda e: e.activation(out=ssi[:, 2:3], in_=ssi[:, 1:2], func=AF.Exp, scale=-0.5),
                         reads=[ssi], accum=[ssi])
                    S.op("dve", lambda e: e.scalar_tensor_tensor(out=tmp[i][:], in0=xt[i][:], scalar=ssi[:, 2:3], in1=A1[:],
                                                                 op0=ALU.mult, op1=ALU.mult), reads=[xt[i], ssi, A1], writes=[tmp[i]])
                    S.op("pool", lambda e: e.tensor_tensor(out=hb[i][:], in0=tmp[i][:], in1=B1[:], op=ALU.add),
                         reads=[tmp[i], B1], writes=[hb[i]])

                def pre_b(t):
                    i = t % NBUF
                    for kc in range(KC):
                        S.op("pe", lambda e, kc=kc: e.transpose(out=pT[:, kc, :], in_=hb[i][:, kc * 128:(kc + 1) * 128], identity=ident_b[:]),
                             reads=[hb[i], ident_b], accum=[pT])
                    S.op("act", lambda e: e.copy(out=hT[i][:], in_=pT[:]), reads=[pT], writes=[hT[i]])

                def zstage(t):
                    i = t % NBUF
                    z = zgroup(hT[i], C_FF, 8)
                    f0, f1, f2 = fz
                    S.op("dve", lambda e: e.tensor_tensor(out=f0[:], in0=z[:, 0:8], in1=bf_rep[:], op=ALU.add), reads=[z, bf_rep], writes=[f0])
                    S.op("act", lambda e: e.activation(out=f1[:], in_=f0[:], func=AF.Exp, scale=-1.0), reads=[f0], writes=[f1])
                    S.op("act", lambda e: e.activation(out=f2[:], in_=f1[:], func=AF.Ln, bias=1.0), reads=[f1], writes=[f2])
                    z = zgroup(hT[i], C_FQ, 512)
                    headnorm(z, 0, 0, mkdst(QKF[i], 0))
                    z = zgroup(hT[i], C_FK, 512)
                    headnorm(z, 1, 1, mkdst(QKF[i], 8))
                    z = zgroup(hT[i], C_FV, 512)
                    S.op("act", lambda e, z=z: e.copy(out=VF[i][:, :, 0:64], in_=z[:].rearrange("p (h d) -> p h d", d=HD)),
                         reads=[z], accum=[VF[i]])
                    S.op("pe", lambda e: e.matmul(psm[:, 0:8], lhsT=tri_i[:], rhs=f2[:], start=True, stop=True),
                         reads=[tri_i, f2], accum=[psm])
                    S.op("pe", lambda e: e.matmul(psm[:, 8:16], lhsT=ones_f[:], rhs=f2[:], start=False, stop=True, skip_group_check=True),
                         reads=[ones_f, f2], accum=[psm])
                    S.op("dve", lambda e: e.tensor_tensor(out=ncum[:, b, t, :], in0=psm[:, 0:8], in1=carry[:], op=ALU.add),
                         reads=[psm, carry], accum=[ncum])
                    S.op("dve", lambda e: e.tensor_tensor(out=carry[:], in0=psm[:, 8:16], in1=carry[:], op=ALU.add),
                         reads=[psm, carry], writes=[carry])
                    S.op("dve", lambda e: e.tensor_scalar(out=QKF[i][:, 0:8, 64:65], in0=ncum[:, b, t, :].unsqueeze(2), scalar1=-1.0,
                                                          scalar2=None, op0=ALU.mult), reads=[ncum], accum=[QKF[i]])
                    z = zgroup(hT[i], C_DQ, 512)
                    headnorm(z, 2, 2, mkdst(QKD[i], 0), rope_t=t)
                    z = zgroup(hT[i], C_DK, 512)
                    headnorm(z, 3, 3, mkdst(QKD[i], 8), rope_t=t)
                    z = zgroup(hT[i], C_DV, 512)
                    S.op("act", lambda e, z=z: e.copy(out=VD[i][:, :, 0:128], in_=z[:].rearrange("p (h d) -> p h d", d=128)),
                         reads=[z], accum=[VD[i]])
                    for gi in range(4):
                        z = zgroup(hT[i], C_G + gi * 512, 512)
                        if gi % 2 == 0:
                            S.op("act", lambda e, z=z, gi=gi: e.copy(out=GT[i][:, gi * 512:(gi + 1) * 512], in_=z[:]), reads=[z], accum=[GT[i]])
                        else:
                            S.op("dve", lambda e, z=z, gi=gi: e.tensor_copy(out=GT[i][:, gi * 512:(gi + 1) * 512], in_=z[:]), reads=[z], accum=[GT[i]])
                    for h in range(16):
                        S.op("pe", lambda e, h=h: e.transpose(out=qtp[0:65, h, :], in_=QKF[i][:, h, 0:65], identity=ident_b[:]),
                             reads=[QKF[i], ident_b], accum=[qtp])
                    S.op("act", lambda e: e.copy(out=QTs[i][0:65, 0:16, :], in_=qtp[0:65, :, :]), reads=[qtp], accum=[QTs[i]])
                    for h in range(16):
                        S.op("pe", lambda e, h=h: e.transpose(out=qtp[0:64, h, :], in_=QKD[i][:, h, :], identity=ident_b[:]),
                             reads=[QKD[i], ident_b], accum=[qtp])
                    S.op("dve", lambda e: e.tensor_copy(out=QTs[i][0:64, 16:32, :], in_=qtp[0:64, :, :]), reads=[qtp], accum=[QTs[i]])
                    cs = slice(t * 128, (t + 1) * 128)
                    S.dma("sp", "sQ%d" % i, lambda e: e.dma_start(out=qTf[b].rearrange("h r t -> r h t")[:, :, cs], in_=QTs[i][0:65, 0:8, :]), reads=[QTs[i]])
                    S.dma("sp", "sQ%d" % i, lambda e: e.dma_start(out=kTf[b].rearrange("h r t -> r h t")[:, :, cs], in_=QTs[i][0:65, 8:16, :]), reads=[QTs[i]])
                    S.dma("sp", "sQ%d" % i, lambda e: e.dma_start(out=qTd[b].rearrange("h r t -> r h t")[:, :, cs], in_=QTs[i][0:64, 16:24, :]), reads=[QTs[i]])
                    S.dma("sp", "sQ%d" % i, lambda e: e.dma_start(out=kTd[b].rearrange("h r t -> r h t")[:, :, cs], in_=QTs[i][0:64, 24:32, :]), reads=[QTs[i]])
                    S.dma("sp", "sVF%d" % i, lambda e: e.dma_start(out=vF[b, cs, :], in_=VF[i][:].rearrange("p h d -> p (h d)")), reads=[VF[i]])
                    S.dma("sp", "sVD%d" % i, lambda e: e.dma_start(out=vD[b, cs, :], in_=VD[i][:].rearrange("p h d -> p (h d)")), reads=[VD[i]])
                    S.dma("sp", "sGT%d" % i, lambda e: e.dma_start(out=gl[b, cs, :], in_=GT[i][:]), reads=[GT[i]])

                load_x(0)
                if NT > 1:
                    load_x(1)
                pre_a(0)
                pre_b(0)
                for t in range(NT):
                    if t + 1 < NT:
                        pre_a(t + 1)
                    if t + 2 < NT:
                        load_x(t + 2)
                    zstage(t)
                    if t + 1 < NT:
                        pre_b(t + 1)
                S.barrier()


        wcres = Res("wcast")
        CH = 64
        castjobs = []
        for (src, dst, key, c) in ((w1, w1c, "wc1", 4), (w3, w3c, "wc3", 4), (w2, w2c, "wc2", 2)):
            srcv = src.rearrange("(r c) n -> r (c n)", c=c)
            for r0 in range(0, 8192, CH):
                castjobs.append((srcv, dst, key, r0))
        castpos = [0]
        for key in ("wc1", "wc2", "wc3"):
            S.bg_keys.add(key)

        def bg_cast(n=1):
            for _ in range(n):
                if castpos[0] >= len(castjobs):
                    return
                srcv, dst, key, r0 = castjobs[castpos[0]]
                castpos[0] += 1
                S.dma("pool", key, lambda e: e.dma_start(out=dst[r0:r0 + CH, :], in_=srcv[r0:r0 + CH, :]), accum=[wcres])

        def phase2(b, o_all):
            with contextlib.ExitStack() as st:
                sbk = [ps(st, "sbk%d" % i, [128, 512], F32) for i in range(3)]
                obs = [ps(st, "ob%d" % i, [128, 4, 65], F32) for i in range(2)]
                od = ps(st, "od", [128, 3, 512], F32)
                pts = [sb(st, "pt%d" % i, [128, 512], BF16) for i in range(4)]
                cnt = [0]
                with contextlib.ExitStack() as st2:
                    QT = [sb(st2, "QTf%d" % i, [65, TT], BF16) for i in range(2)]
                    KT = [sb(st2, "KTf%d" % i, [65, TT], BF16) for i in range(2)]
                    VT = [sb(st2, "VTf%d" % i, [128, NT, 65], BF16) for i in range(2)]
                    rec = [sb(st2, "rec%d" % i, [128, 4], F32) for i in range(2)]

                    def load_f(h):
                        i = h % 2
                        S.dma("sp", "QTf%d" % i, lambda e: e.dma_start(out=QT[i][:], in_=qTf[b, h, :, :]), writes=[QT[i]])
                        S.dma("sp", "KTf%d" % i, lambda e: e.dma_start(out=KT[i][:], in_=kTf[b, h, :, :]), writes=[KT[i]])
                        S.dma("sp", "VTf%d" % i, lambda e: e.dma_start(
                            out=VT[i][:], in_=vF[b].rearrange("(t p) c -> p t c", p=128)[:, :, h * 65:(h + 1) * 65]), writes=[VT[i]])

                    tasks = [(h, g, kt) for h in range(8) for g in range(NG) for kt in range(4 * g + 4)]

                    def qk_f(p):
                        h, g, kt = tasks[p]
                        i = h % 2
                        c0 = 128 * max(kt - 4 * g, 0)
                        sbank = sbk[p % 3]
                        S.op("pe", lambda e: e.matmul(sbank[:, c0:512], lhsT=KT[i][:, kt * 128:(kt + 1) * 128],
                                                      rhs=QT[i][:, g * 512 + c0:(g + 1) * 512], start=True, stop=True),
                             reads=[KT[i], QT[i]], accum=[sbank])

                    def rest_f(p):
                        h, g, kt = tasks[p]
                        i = h % 2
                        j = kt - 4 * g
                        jb = max(j, 0)
                        c0 = 128 * jb
                        sbank = sbk[p % 3]
                        pt = pts[p % 4]
                        ob = obs[g % 2]
                        S.op("act", lambda e: e.activation(out=pt[:, c0:512], in_=sbank[:, c0:512], func=AF.Exp,
                                                           bias=ncum[:, b, kt, h:h + 1]), reads=[sbank, ncum], writes=[pt])
                        if j >= 0:
                            S.op("pool", lambda e: e.affine_select(out=pt[:, c0:c0 + 128], in_=pt[:, c0:c0 + 128], pattern=[[1, 128]],
                                                                   compare_op=ALU.is_ge, fill=0.0, base=0, channel_multiplier=-1),
                                 reads=[pt], writes=[pt])
                        for qb in range(jb, 4):
                            S.op("pe", lambda e, qb=qb: e.matmul(
                                ob[:, qb, :], lhsT=pt[:, qb * 128:(qb + 1) * 128], rhs=VT[i][:, kt, :],
                                start=(kt == 0 and qb == 0), stop=(kt == 4 * g + qb), skip_group_check=True), reads=[pt, VT[i]], accum=[ob])
                        if kt == 4 * g + 3:
                            r = rec[g % 2]
                            S.op("dve", lambda e: e.reciprocal(out=r[:], in_=ob[:, :, 64]), reads=[ob], writes=[r])
                            S.op("dve", lambda e: e.tensor_tensor(out=o_all[:, g * 4:(g + 1) * 4, h * 64:(h + 1) * 64], in0=ob[:, :, 0:64],
                                                                  in1=r[:].unsqueeze(2).to_broadcast([128, 4, 64]), op=ALU.mult),
                                 reads=[ob, r], accum=[o_all])

                    LA = 2
                    load_f(0)
                    for p in range(min(LA, len(tasks))):
                        if tasks[p][1] == 0 and tasks[p][2] == 0 and tasks[p][0] + 1 < 8:
                            pass
                        qk_f(p)
                    for p in range(len(tasks)):
                        h, g, kt = tasks[p]
                        if g == 0 and kt == 0 and h + 1 < 8:
                            load_f(h + 1)
                        if p + LA < len(tasks):
                            qk_f(p + LA)
                        rest_f(p)
                        if p % 5 == 4:
                            bg_cast()
                S.barrier()
                with contextlib.ExitStack() as st2:
                    QT2 = [sb(st2, "QTd%d" % i, [64, 2, TT], BF16) for i in range(2)]
                    KT2 = [sb(st2, "KTd%d" % i, [64, 2, TT], BF16) for i in range(2)]
                    VT2 = [sb(st2, "VTd%d" % i, [128, NT, 129], BF16) for i in range(2)]
                    onorm = sb(st2, "onorm", [128, 8, 128], F32)
                    oo = sb(st2, "oo", [128, 4, 128], F32)
                    sq2 = sb(st2, "sq2", [128, 4, 128], F32)
                    recd = sb(st2, "recd", [128, 8], F32)
                    msd = sb(st2, "msd", [128, 4], F32)

                    def load_d(hd):
                        i = hd % 2
                        S.dma("sp", "QTd%d" % i, lambda e: e.dma_start(
                            out=QT2[i][:], in_=qTd[b, hd * 2:hd * 2 + 2, :, :].rearrange("m r t -> r m t")), writes=[QT2[i]])
                        S.dma("sp", "KTd%d" % i, lambda e: e.dma_start(
                            out=KT2[i][:], in_=kTd[b, hd * 2:hd * 2 + 2, :, :].rearrange("m r t -> r m t")), writes=[KT2[i]])
                        S.dma("sp", "VTd%d" % i, lambda e: e.dma_start(
                            out=VT2[i][:], in_=vD[b].rearrange("(t p) c -> p t c", p=128)[:, :, hd * 129:(hd + 1) * 129]), writes=[VT2[i]])

                    tasks = [(hd, g, kt, m) for hd in range(4) for g in range(NG) for kt in range(4 * g + 4) for m in range(2)]

                    def qk_d(p):
                        hd, g, kt, m = tasks[p]
                        i = hd % 2
                        c0 = 128 * max(kt - 4 * g, 0)
                        sbank = sbk[p % 3]
                        S.op("pe", lambda e: e.matmul(sbank[:, c0:512], lhsT=KT2[i][:, m, kt * 128:(kt + 1) * 128],
                                                      rhs=QT2[i][:, m, g * 512 + c0:(g + 1) * 512], start=True, stop=True),
                             reads=[KT2[i], QT2[i]], accum=[sbank])

                    def rest_d(p):
                        hd, g, kt, m = tasks[p]
                        i = hd % 2
                        j = kt - 4 * g
                        jb = max(j, 0)
                        c0 = 128 * jb
                        sbank = sbk[p % 3]
                        pt = pts[p % 4]
                        S.op("act", lambda e: e.activation(out=pt[:, c0:512], in_=sbank[:, c0:512], func=AF.Exp),
                             reads=[sbank], writes=[pt])
                        if j >= 0:
                            S.op("pool", lambda e: e.memset(pt[64:128, c0:c0 + 64], 0.0), reads=[pt], writes=[pt])
                        for qb in range(jb, 4):
                            idx = m * 4 + qb
                            bank = idx // 3
                            off = (idx % 3) * 129
                            S.op("pe", lambda e, qb=qb, bank=bank, off=off, idx=idx: e.matmul(
                                od[:, bank, off:off + 129], lhsT=pt[:, qb * 128:(qb + 1) * 128], rhs=VT2[i][:, kt, :],
                                start=(kt == 0 and idx in (0, 3, 6)), stop=(kt == 4 * g + qb), skip_group_check=True), reads=[pt, VT2[i]], accum=[od])
                        if kt == 4 * g + 3 and m == 1:
                            for bank in range(3):
                                n = 3 if bank < 2 else 2
                                view = od[:, bank, 0:n * 129].rearrange("p (a c) -> p a c", c=129)
                                S.op("dve", lambda e, view=view, bank=bank, n=n: e.reciprocal(out=recd[:, bank * 3:bank * 3 + n], in_=view[:, :, 128]),
                                     reads=[od], accum=[recd])
                                S.op("dve", lambda e, view=view, bank=bank, n=n: e.tensor_tensor(
                                    out=onorm[:, bank * 3:bank * 3 + n, :], in0=view[:, :, 0:128],
                                    in1=recd[:, bank * 3:bank * 3 + n].unsqueeze(2).to_broadcast([128, n, 128]), op=ALU.mult),
                                     reads=[od, recd], accum=[onorm])
                            S.op("dve", lambda e: e.scalar_tensor_tensor(out=oo[:], in0=onorm[:, 4:8, :], scalar=nlam[:, 0:1], in1=onorm[:, 0:4, :],
                                                                         op0=ALU.mult, op1=ALU.add), reads=[onorm, nlam], writes=[oo])
                            S.op("pool", lambda e: e.tensor_tensor(out=sq2[:], in0=oo[:], in1=oo[:], op=ALU.mult), reads=[oo], writes=[sq2])
                            S.op("dve", lambda e: e.tensor_reduce(out=msd[:], in_=sq2[:], axis=AX.X, op=ALU.add), reads=[sq2], writes=[msd])
                            S.op("act", lambda e: e.activation(out=msd[:], in_=msd[:], func=AF.Ln, scale=1.0 / 128, bias=EPS), reads=[msd], writes=[msd])
                            S.op("act", lambda e: e.activation(out=msd[:], in_=msd[:], func=AF.Exp, scale=-0.5), reads=[msd], writes=[msd])
                            S.op("dve", lambda e: e.tensor_tensor(out=oo[:], in0=oo[:], in1=msd[:].unsqueeze(2).to_broadcast([128, 4, 128]), op=ALU.mult),
                                 reads=[oo, msd], writes=[oo])
                            S.op("pool", lambda e: e.tensor_tensor(out=o_all[:, g * 4:(g + 1) * 4, 512 + hd * 128:512 + (hd + 1) * 128], in0=oo[:],
                                                                   in1=gsub[:].unsqueeze(1).to_broadcast([128, 4, 128]), op=ALU.mult),
                                 reads=[oo, gsub], accum=[o_all])

                    LA = 2
                    load_d(0)
                    for p in range(min(LA, len(tasks))):
                        qk_d(p)
                    for p in range(len(tasks)):
                        hd, g, kt, m = tasks[p]
                        if g == 0 and kt == 0 and m == 0 and hd + 1 < 4:
                            load_d(hd + 1)
                        if p + LA < len(tasks):
                            qk_d(p + LA)
                        rest_d(p)
                        if p % 5 == 4:
                            bg_cast()
            S.barrier()
            if dbg:
                S.dma("sp", "dbgoc", lambda e: e.dma_start(out=dbg_t["oc"][b].rearrange("(t p) d -> p t d", p=128), in_=o_all[:]), reads=[o_all])

        def phase3(b, o_all):
            with contextlib.ExitStack() as st:
                wpf = sb(st, "wpf", [128, 4, D], BF16)
                wpd = sb(st, "wpd", [128, 4, D], BF16)
                wo = sb(st, "wo", [128, 8, D], BF16)
                wr = sb(st, "wr", [128, 8, 36], BF16)
                brt = sb(st, "brt", [128, 36], F32)
                S.dma("pool", "wpf", lambda e: e.dma_start(out=wpf[:], in_=w_pf.rearrange("(c k) n -> k c n", k=128)), writes=[wpf])
                S.dma("pool", "wpd", lambda e: e.dma_start(out=wpd[:], in_=w_pd.rearrange("(c k) n -> k c n", k=128)), writes=[wpd])
                S.dma("pool", "wo", lambda e: e.dma_start(out=wo[:], in_=w_out.rearrange("(c k) n -> k c n", k=128)), writes=[wo])
                S.dma("pool", "wr", lambda e: e.dma_start(out=wr[:, :, 0:4], in_=w_rg.rearrange("(c k) n -> k c n", k=128)), accum=[wr])
                S.dma("pool", "wr", lambda e: e.dma_start(out=wr[:, :, 4:36], in_=w_re.rearrange("(c k) n -> k c n", k=128)), accum=[wr])
                S.dma("sp", "brt", lambda e: e.dma_start(out=brt[:, 0:4], in_=bcast_rows(b_rg[0:1, :], 128)), accum=[brt])
                S.dma("sp", "brt", lambda e: e.dma_start(out=brt[:, 4:36], in_=bcast_rows(b_re[0:1, :], 128)), accum=[brt])
                gt1 = load_mod_row(st, "gt1", b, 2)
                A2 = load_mod_row(st, "A2", b, 3)
                B2 = load_mod_row(st, "B2", b, 4)
                NBUF = 2
                xt = [sb(st, "x3_%d" % i, [128, D], F32) for i in range(3)]
                gls = [sb(st, "gls%d" % i, [128, 2048], BF16) for i in range(3)]
                sgs = [sb(st, "sg0", [128, 2048], BF16)] * NBUF
                oTs = [sb(st, "oT0", [128, KC, 128], BF16)] * NBUF
                m1s = [sb(st, "m1_0", [128, D], F32)] * NBUF
                m2s = [sb(st, "m2_0", [128, D], F32)] * NBUF
                mgs = [sb(st, "mg%d" % i, [128, D], BF16) for i in range(NBUF)]
                mTs = [sb(st, "mT0", [128, KC, 128], BF16)] * NBUF
                x1 = [sb(st, "x1_%d" % i, [128, D], F32) for i in range(NBUF)]
                tmps = [sb(st, "tmp3_0", [128, D], F32)] * NBUF
                sqj = sb(st, "sqj", [128, D], BF16)
                h2 = [sb(st, "h2_%d" % i, [128, D], BF16) for i in range(NBUF)]
                h2T = sb(st, "h2T", [128, KC, 128], BF16)
                ss = sb(st, "ss3", [128, 4], F32)
                Lall = sb(st, "Lall", [128, NT, 36], F32)
                rsm = sb(st, "rsm", [128, 16, NT], F32)
                r4 = sb(st, "r4", [128, 3, NT * 4], F32)
                pT = ps(st, "pT3", [128, KC, 128], BF16)
                pfd = ps(st, "pfd", [128, 4, 512], F32)
                pyy = ps(st, "pyy", [128, 2, 512], F32)
                prt = ps(st, "prt", [128, 128], F32)

                def load3(t):
                    i = t % 3
                    S.dma("sp", "x3_%d" % i, lambda e: e.dma_start(out=xt[i][:], in_=x[b, t * 128:(t + 1) * 128, :]), writes=[xt[i]])
                    S.dma("sp", "gls%d" % i, lambda e: e.dma_start(out=gls[i][:], in_=gl[b, t * 128:(t + 1) * 128, :]), writes=[gls[i]])

                def transp(src_ap_fn, src_t, dstT, eng):
                    for kc in range(KC):
                        S.op("pe", lambda e, kc=kc: e.transpose(out=pT[:, kc, :], in_=src_ap_fn(kc), identity=ident_b[:]),
                             reads=[src_t, ident_b], accum=[pT])
                    if eng == "act":
                        S.op("act", lambda e: e.copy(out=dstT[:], in_=pT[:]), reads=[pT], writes=[dstT])
                    else:
                        S.op("dve", lambda e: e.tensor_copy(out=dstT[:], in_=pT[:]), reads=[pT], writes=[dstT])

                def stageA(t):
                    i = t % NBUF
                    oT, sg, m1, m2, mg = oTs[i], sgs[i], m1s[i], m2s[i], mgs[i]
                    transp(lambda kc: o_all[:, t, kc * 128:(kc + 1) * 128], o_all, oT, "act")
                    for nh in range(2):
                        for c in range(4):
                            S.op("pe", lambda e, nh=nh, c=c: e.matmul(pfd[:, nh, :], lhsT=oT[:, c, :], rhs=wpf[:, c, nh * 512:(nh + 1) * 512],
                                                                      start=(c == 0), stop=(c == 3)), reads=[oT, wpf], accum=[pfd])
                    for nh in range(2):
                        for c in range(4):
                            S.op("pe", lambda e, nh=nh, c=c: e.matmul(pfd[:, 2 + nh, :], lhsT=oT[:, 4 + c, :], rhs=wpd[:, c, nh * 512:(nh + 1) * 512],
                                                                      start=(c == 0), stop=(c == 3)), reads=[oT, wpd], accum=[pfd])
                    S.op("act", lambda e: e.activation(out=sg[:], in_=gls[t % 3][:], func=AF.Sigmoid), reads=[gls[t % 3]], writes=[sg])
                    S.op("dve", lambda e: e.tensor_tensor(out=m1[:], in0=pfd[:, 0:2, :].rearrange("p a n -> p (a n)"), in1=sg[:, 0:1024], op=ALU.mult),
                         reads=[pfd, sg], writes=[m1])
                    S.op("dve", lambda e: e.tensor_tensor(out=m2[:], in0=pfd[:, 2:4, :].rearrange("p a n -> p (a n)"), in1=sg[:, 1024:2048], op=ALU.mult),
                         reads=[pfd, sg], writes=[m2])
                    S.op("pool", lambda e: e.tensor_tensor(out=mg[:], in0=m1[:], in1=m2[:], op=ALU.add), reads=[m1, m2], writes=[mg])

                def stageB(t):
                    i = t % NBUF
                    tt = b * NT + t
                    mg, mT, tmp = mgs[i], mTs[i], tmps[i]
                    transp(lambda kc: mg[:, kc * 128:(kc + 1) * 128], mg, mT, "act")
                    for nh in range(2):
                        for c in range(KC):
                            S.op("pe", lambda e, nh=nh, c=c: e.matmul(pyy[:, nh, :], lhsT=mT[:, c, :], rhs=wo[:, c, nh * 512:(nh + 1) * 512],
                                                                      start=(c == 0), stop=(c == KC - 1)), reads=[mT, wo], accum=[pyy])
                    S.op("dve", lambda e: e.tensor_tensor(out=tmp[:], in0=pyy[:].rearrange("p a n -> p (a n)"), in1=gt1[:], op=ALU.mult),
                         reads=[pyy, gt1], writes=[tmp])
                    S.op("pool", lambda e: e.tensor_tensor(out=x1[i][:], in0=tmp[:], in1=xt[t % 3][:], op=ALU.add), reads=[tmp, xt[t % 3]], writes=[x1[i]])
                    S.dma("sp", "sx1_%d" % i, lambda e: e.dma_start(out=x1s[tt * 128:(tt + 1) * 128, :], in_=x1[i][:]), reads=[x1[i]])
                    S.op("act", lambda e: e.activation(out=sqj[:], in_=x1[i][:], func=AF.Square, accum_out=ss[:, 0:1]), reads=[x1[i]], writes=[sqj, ss])
                    S.op("act", lambda e: e.activation(out=ss[:, 1:2], in_=ss[:, 0:1], func=AF.Ln, scale=1.0 / D, bias=EPS), reads=[ss], accum=[ss])
                    S.op("act", lambda e: e.activation(out=ss[:, 2:3], in_=ss[:, 1:2], func=AF.Exp, scale=-0.5), reads=[ss], accum=[ss])
                    S.op("dve", lambda e: e.scalar_tensor_tensor(out=tmp[:], in0=x1[i][:], scalar=ss[:, 2:3], in1=A2[:], op0=ALU.mult, op1=ALU.mult),
                         reads=[x1[i], ss, A2], writes=[tmp])
                    S.op("pool", lambda e: e.tensor_tensor(out=h2[i][:], in0=tmp[:], in1=B2[:], op=ALU.add), reads=[tmp, B2], writes=[h2[i]])
                    S.dma("sp", "sh2_%d" % i, lambda e: e.dma_start(out=h2s[tt * 128:(tt + 1) * 128, :], in_=h2[i][:]), reads=[h2[i]])

                def stageC(t):
                    i = t % NBUF
                    tt = b * NT + t
                    transp(lambda kc: h2[i][:, kc * 128:(kc + 1) * 128], h2[i], h2T, "dve")
                    for c in range(KC):
                        S.op("pe", lambda e, c=c: e.matmul(prt[:, 0:36], lhsT=h2T[:, c, :], rhs=wr[:, c, :], start=(c == 0), stop=(c == KC - 1)),
                             reads=[h2T, wr], accum=[prt])
                    S.op("dve", lambda e: e.tensor_tensor(out=Lall[:, t, :], in0=prt[:, 0:36], in1=brt[:], op=ALU.add), reads=[prt, brt], accum=[Lall])

                load3(0)
                if NT > 1:
                    load3(1)
                stageA(0)
                for t in range(NT):
                    if t + 2 < NT:
                        load3(t + 2)
                    if t + 1 < NT:
                        stageA(t + 1)
                    if t >= 1:
                        stageC(t - 1)
                    stageB(t)
                stageC(NT - 1)
                TT_ = NT
                tt0 = b * NT
                BT = [m1s[0], m2s[0], tmps[0], xt[0], xt[1], xt[2], x1[0], x1[1]]

                def v32(tl):
                    return tl[:, 0:TT_ * 32].rearrange("p (t e) -> p t e", e=32)

                def v8(tl, k):
                    return tl[:, k * 256:k * 256 + TT_ * 8].rearrange("p (t e) -> p t e", e=8)

                def vec(k):
                    return rsm[:, k, :]

                def g4(k):
                    return r4[:, k, :].rearrange("p (t g) -> p t g", g=4)

                def bc(ap2, n):
                    return ap2.unsqueeze(2).to_broadcast([128, TT_, n])

                lg = Lall[:, :, 0:4]
                le4 = Lall[:, :, 4:36].rearrange("p t (g e) -> p t g e", e=8)
                T48, M1t, M2t, M12t, BASEt, RRt, TAt, SMt = BT
                mg4, zg, gw, m1v, m2v, dm, ex, w1_ = [vec(k) for k in range(8)]
                ohg, d4, eg = g4(0), g4(1), g4(2)
                e8, oh1, e8b, oh2 = v8(SMt, 0), v8(SMt, 1), v8(SMt, 2), v8(SMt, 3)
                R_ = [Lall, rsm, r4]

                def DV(fn, reads, writes, eng="dve"):
                    S.op(eng, fn, reads=reads, writes=writes)

                DV(lambda e: e.tensor_reduce(out=mg4, in_=lg, axis=AX.X, op=ALU.max), [Lall], [rsm])
                DV(lambda e: e.tensor_tensor(out=ohg, in0=lg, in1=bc(mg4, 4), op=ALU.is_equal), [Lall, rsm], [r4])
                DV(lambda e: e.tensor_tensor(out=d4, in0=lg, in1=bc(mg4, 4), op=ALU.subtract), [Lall, rsm, r4], [r4])
                DV(lambda e: e.activation(out=eg, in_=d4, func=AF.Exp), [r4], [r4], eng="act")
                DV(lambda e: e.tensor_reduce(out=zg, in_=eg, axis=AX.X, op=ALU.add), [r4, rsm], [rsm])
                DV(lambda e: e.reciprocal(out=gw, in_=zg), [rsm], [rsm])
                t48v = v32(T48).rearrange("p t (g e) -> p t g e", e=8)
                DV(lambda e: e.tensor_tensor(out=t48v, in0=le4, in1=ohg.unsqueeze(3).to_broadcast([128, TT_, 4, 8]), op=ALU.mult), [Lall, r4], [T48])
                DV(lambda e: e.tensor_reduce(out=e8, in_=t48v.rearrange("p t g e -> p t e g"), axis=AX.X, op=ALU.add), [T48], [SMt])
                DV(lambda e: e.tensor_reduce(out=m1v, in_=e8, axis=AX.X, op=ALU.max), [SMt, rsm], [rsm])
                DV(lambda e: e.tensor_tensor(out=oh1, in0=e8, in1=bc(m1v, 8), op=ALU.is_equal), [SMt, rsm], [SMt])
                DV(lambda e: e.scalar_tensor_tensor(out=e8b, in0=oh1, scalar=-1e30, in1=e8, op0=ALU.mult, op1=ALU.add), [SMt], [SMt])
                DV(lambda e: e.tensor_reduce(out=m2v, in_=e8b, axis=AX.X, op=ALU.max), [SMt, rsm], [rsm])
                DV(lambda e: e.tensor_tensor(out=oh2, in0=e8b, in1=bc(m2v, 8), op=ALU.is_equal), [SMt, rsm], [SMt])
                DV(lambda e: e.tensor_tensor(out=dm, in0=m2v, in1=m1v, op=ALU.subtract), [rsm], [rsm])
                DV(lambda e: e.activation(out=ex, in_=dm, func=AF.Exp), [rsm], [rsm], eng="act")
                DV(lambda e: e.tensor_scalar(out=ex, in0=ex, scalar1=1.0, scalar2=None, op0=ALU.add), [rsm], [rsm])
                DV(lambda e: e.reciprocal(out=w1_, in_=ex), [rsm], [rsm])
                S.op("dve", lambda e: e.tensor_tensor(out=wt_all[:, tt0:tt0 + TT_, 0], in0=w1_, in1=gw, op=ALU.mult), reads=[rsm], accum=[wt_all])
                S.op("dve", lambda e: e.tensor_tensor(out=wt_all[:, tt0:tt0 + TT_, 1], in0=gw, in1=wt_all[:, tt0:tt0 + TT_, 0], op=ALU.subtract),
                     reads=[rsm, wt_all], accum=[wt_all])
                M1v = v32(M1t).rearrange("p t (g e) -> p t g e", e=8)
                M2v = v32(M2t).rearrange("p t (g e) -> p t g e", e=8)
                DV(lambda e: e.tensor_tensor(out=M1v, in0=ohg.unsqueeze(3).to_broadcast([128, TT_, 4, 8]),
                                            in1=oh1.unsqueeze(2).to_broadcast([128, TT_, 4, 8]), op=ALU.mult), [r4, SMt], [M1t])
                DV(lambda e: e.tensor_tensor(out=M2v, in0=ohg.unsqueeze(3).to_broadcast([128, TT_, 4, 8]),
                                            in1=oh2.unsqueeze(2).to_broadcast([128, TT_, 4, 8]), op=ALU.mult), [r4, SMt], [M2t])
                DV(lambda e: e.tensor_tensor(out=v32(M12t), in0=v32(M1t), in1=v32(M2t), op=ALU.add), [M1t, M2t], [M12t])
                io_b = iota32[:].unsqueeze(1).to_broadcast([128, TT_, 32])
                for k, Mt in ((0, M1t), (1, M2t)):
                    DV(lambda e, Mt=Mt: e.tensor_tensor(out=v32(TAt), in0=v32(Mt), in1=io_b, op=ALU.mult), [Mt, iota32], [TAt])
                    S.op("dve", lambda e, k=k: e.tensor_reduce(out=eid_all[:, tt0:tt0 + TT_, k], in_=v32(TAt), axis=AX.X, op=ALU.add),
                         reads=[TAt], accum=[eid_all])
                NCOL = TT_ * 32
                pw = pfd[:, 0:2, :].rearrange("p a n -> p (a n)")
                pc_ = pfd[:, 2:4, :].rearrange("p a n -> p (a n)")
                for c0 in range(0, NCOL, 512):
                    c1 = min(c0 + 512, NCOL)
                    S.op("pe", lambda e, c0=c0, c1=c1: e.matmul(pw[:, c0:c1], lhsT=tri_s[:], rhs=M12t[:, c0:c1], start=True, stop=True),
                         reads=[tri_s, M12t], accum=[pfd])
                    S.op("pe", lambda e, c0=c0, c1=c1: e.matmul(pc_[:, c0:c1], lhsT=ones_f[:], rhs=M12t[:, c0:c1], start=True, stop=True),
                         reads=[ones_f, M12t], accum=[pfd])
                DV(lambda e: e.tensor_copy(out=v32(TAt), in_=pc_[:, 0:NCOL].rearrange("p (t e) -> p t e", e=32)), [pfd], [TAt])
                for t in range(TT_):
                    S.op("pool", lambda e, t=t: e.tensor_copy(out=BASEt[:, t * 32:(t + 1) * 32], in_=ecarry[:]), reads=[ecarry], accum=[BASEt])
                    S.op("pool", lambda e, t=t: e.tensor_tensor(out=ecarry[:], in0=ecarry[:], in1=TAt[:, t * 32:(t + 1) * 32], op=ALU.add),
                         reads=[ecarry, TAt, BASEt], writes=[ecarry])
                DV(lambda e: e.tensor_tensor(out=v32(RRt), in0=pw[:, 0:NCOL].rearrange("p (t e) -> p t e", e=32), in1=v32(BASEt), op=ALU.add),
                  [pfd, BASEt], [RRt])
                for k, Mt in ((0, M1t), (1, M2t)):
                    DV(lambda e, Mt=Mt: e.tensor_tensor(out=v32(TAt), in0=v32(RRt), in1=v32(Mt), op=ALU.mult), [RRt, Mt], [TAt])
                    S.op("dve", lambda e, k=k: e.tensor_reduce(out=rk_all[:, tt0:tt0 + TT_, k], in_=v32(TAt), axis=AX.X, op=ALU.add),
                         reads=[TAt], accum=[rk_all])
            S.barrier()

        def phase3b():
            S.bg_keys.clear()
            with contextlib.ExitStack() as st:
                padf = sb(st, "padf", [128, 32], F32)
                pend = sb(st, "pend", [128, 32], F32)
                pstart = sb(st, "pstart", [128, 32], F32)
                one32 = sb(st, "one32", [128, 32], F32)
                bst_i = sb(st, "bst_i", [128, NBLK], I32)
                bst_f = sb(st, "bst_f", [128, NBLK], F32)
                S.op("pool", lambda e: e.iota(bst_i[:], pattern=[[BS, NBLK]], base=0, channel_multiplier=0), writes=[bst_i])
                S.op("dve", lambda e: e.tensor_copy(out=bst_f[:], in_=bst_i[:]), reads=[bst_i], writes=[bst_f])
                cmpc = sb(st, "cmpc", [128, 32, NBLK], F32)
                S.op("dve", lambda e: e.tensor_tensor(out=cmpc[:], in0=ecarry[:].unsqueeze(2).to_broadcast([128, 32, NBLK]),
                                                      in1=bst_f[:].unsqueeze(1).to_broadcast([128, 32, NBLK]), op=ALU.is_gt),
                     reads=[ecarry, bst_f], writes=[cmpc])
                S.op("dve", lambda e: e.tensor_reduce(out=padf[:], in_=cmpc[:], axis=AX.X, op=ALU.add), reads=[cmpc], writes=[padf])
                S.op("dve", lambda e: e.tensor_scalar(out=padf[:], in0=padf[:], scalar1=float(BS), scalar2=None, op0=ALU.mult), reads=[padf], writes=[padf])
                S.op("pool", lambda e: e.memset(one32[:], 1.0), writes=[one32])
                S.op("dve", lambda e: e.tensor_tensor_scan(out=pend[:], data0=one32[:], data1=padf[:], initial=0.0, op0=ALU.mult, op1=ALU.add),
                     reads=[one32, padf], writes=[pend])
                S.op("dve", lambda e: e.tensor_tensor(out=pstart[:], in0=pend[:], in1=padf[:], op=ALU.subtract), reads=[pend, padf], writes=[pstart])
                big = sb(st, "big", [128, NTT, 32], F32)
                dsf = sb(st, "dsf", [128, NTT, 2], F32)
                for k in range(2):
                    S.op("dve", lambda e, k=k: e.tensor_tensor(out=big[:], in0=iota32[:].unsqueeze(1).to_broadcast([128, NTT, 32]),
                                                               in1=eid_all[:, :, k:k + 1].to_broadcast([128, NTT, 32]), op=ALU.is_equal),
                         reads=[iota32, eid_all], writes=[big])
                    S.op("dve", lambda e: e.tensor_tensor(out=big[:], in0=big[:], in1=pstart[:].unsqueeze(1).to_broadcast([128, NTT, 32]), op=ALU.mult),
                         reads=[big, pstart], writes=[big])
                    S.op("dve", lambda e, k=k: e.tensor_reduce(out=dsf[:, :, k:k + 1], in_=big[:], axis=AX.X, op=ALU.add), reads=[big], accum=[dsf])
                S.op("dve", lambda e: e.tensor_tensor(out=dsf[:], in0=dsf[:], in1=rk_all[:], op=ALU.add), reads=[dsf, rk_all], writes=[dsf])
                S.op("dve", lambda e: e.tensor_copy(out=dest_i[:], in_=dsf[:]), reads=[dsf], writes=[dest_i])
                cmpb = sb(st, "cmpb", [128, NBLK, 32], F32)
                S.op("dve", lambda e: e.tensor_tensor(out=cmpb[:], in0=pend[:].unsqueeze(1).to_broadcast([128, NBLK, 32]),
                                                      in1=bst_f[:].unsqueeze(2).to_broadcast([128, NBLK, 32]), op=ALU.is_le),
                     reads=[pend, bst_f], writes=[cmpb])
                BE = sb(st, "BE", [128, NBLK], F32)
                S.op("dve", lambda e: e.tensor_reduce(out=BE[:], in_=cmpb[:], axis=AX.X, op=ALU.add), reads=[cmpb], writes=[BE])
                S.op("dve", lambda e: e.tensor_scalar(out=BE[:], in0=BE[:], scalar1=float(NEXP - 1), scalar2=None, op0=ALU.min), reads=[BE], writes=[BE])
                bpc_i = sb(st, "bpc_i", [128, 1], I32)
                bpc = sb(st, "bpc", [128, 1], F32)
                S.op("pool", lambda e: e.iota(bpc_i[:], pattern=[[0, 1]], base=0, channel_multiplier=2), writes=[bpc_i])
                S.op("dve", lambda e: e.tensor_copy(out=bpc[:], in_=bpc_i[:]), reads=[bpc_i], writes=[bpc])
                idf = sb(st, "idf", [128, NBLK, 2], F32)
                idx1 = blkidx["idx1"]
                S.op("dve", lambda e: e.tensor_scalar(out=idf[:, :, 0], in0=BE[:], scalar1=256.0, scalar2=bpc[:, 0:1], op0=ALU.mult, op1=ALU.add),
                     reads=[BE, bpc], writes=[idf])
                S.op("dve", lambda e: e.tensor_scalar(out=idf[:, :, 1], in0=idf[:, :, 0], scalar1=1.0, scalar2=None, op0=ALU.add),
                     reads=[idf], accum=[idf])
                S.op("dve", lambda e: e.tensor_copy(out=idx1[:], in_=idf[:]), reads=[idf], writes=[idx1])
                if dbg:
                    S.dma("sp", "dbgbe", lambda e: e.dma_start(out=dbg_t["be"][:, 0:NBLK], in_=BE[:]), reads=[BE])
                    S.dma("sp", "dbgbe", lambda e: e.dma_start(out=dbg_t["be"][:, NBLK:NBLK + 32], in_=pend[:]), reads=[pend])
                    S.dma("sp", "dbgbe", lambda e: e.dma_start(out=dbg_t["be"][:, NBLK + 32:NBLK + 64], in_=ecarry[:]), reads=[ecarry])
                    S.dma("sp", "dbgrt", lambda e: e.dma_start(out=dbg_t["rt"][:, :, 0:2], in_=eid_all[:]), reads=[eid_all])
                    S.dma("sp", "dbgrt", lambda e: e.dma_start(out=dbg_t["rt"][:, :, 2:4], in_=wt_all[:]), reads=[wt_all])
                    S.dma("sp", "dbgrt", lambda e: e.dma_start(out=dbg_t["rt"][:, :, 4:6], in_=dsf[:]), reads=[dsf])
                    S.dma("sp", "dbgrt", lambda e: e.dma_start(out=dbg_t["rt"][:, :, 6:8], in_=rk_all[:]), reads=[rk_all])
                hb_ = [sb(st, "h2l%d" % i, [128, D], BF16) for i in range(2)]
                for tt in range(NTT):
                    i = tt % 2
                    S.dma("sp", "h2l%d" % i, lambda e: e.dma_start(out=hb_[i][:], in_=h2s[tt * 128:(tt + 1) * 128, :]), writes=[hb_[i]])
                    for k in range(2):
                        S.dma("pool", "h2sc%d" % i, lambda e, k=k: e.indirect_dma_start(
                            out=xbuf[:, :], out_offset=bass.IndirectOffsetOnAxis(ap=dest_i[:, tt, k:k + 1], axis=0),
                            in_=hb_[i][:], in_offset=None), reads=[hb_[i], dest_i, xz])
            S.barrier()

        def phase4():
            idx1 = blkidx["idx1"]
            bg_cast(len(castjobs))
            w1v, w3v, w2v = w1c, w3c, w2c
            SUB = BS // 128
            with contextlib.ExitStack() as st:
                w1b = [sb(st, "w1b%d" % i, [128, 8, DEXP], BF16) for i in range(2)]
                w3b = [sb(st, "w3b%d" % i, [128, 8, DEXP], BF16) for i in range(2)]
                w2b = [sb(st, "w2b%d" % i, [128, 4, D], BF16) for i in range(3)]
                xb = [sb(st, "xb%d" % i, [128, SUB, D], BF16) for i in range(2)]
                xTs = [sb(st, "xT%d" % i, [128, KC, BS], BF16) for i in range(2)]
                sact = [sb(st, "sact%d" % i, [128, BS], F32) for i in range(2)]
                gTs = [sb(st, "gT%d" % i, [128, 4, BS], BF16) for i in range(2)]
                yo = [sb(st, "yo%d" % i, [128, SUB, D], BF16) for i in range(2)]
                pX = ps(st, "pX", [128, KC, BS], BF16)
                ph = ps(st, "ph", [128, 4, 512], F32)
                py = ps(st, "py", [128, 2, 512], F32)

                def load_blk(bi):
                    i = bi % 2
                    for hf in range(2):
                        off = bass.IndirectOffsetOnAxis(ap=idx1[:, bi, hf:hf + 1], axis=0)
                        S.dma("pool", "w1b%d" % i, lambda e, hf=hf: e.indirect_dma_start(
                            out=w1b[i][:, hf * 4:(hf + 1) * 4, :].rearrange("p c n -> p (c n)"), out_offset=None,
                            in_=w1v[:, :], in_offset=off), reads=[idx1, wcres], accum=[w1b[i]])
                        S.dma("pool", "w3b%d" % i, lambda e, hf=hf: e.indirect_dma_start(
                            out=w3b[i][:, hf * 4:(hf + 1) * 4, :].rearrange("p c n -> p (c n)"), out_offset=None,
                            in_=w3v[:, :], in_offset=off), reads=[idx1, wcres], accum=[w3b[i]])
                        S.dma("pool", "w2b%d" % (bi % 3), lambda e, hf=hf: e.indirect_dma_start(
                            out=w2b[bi % 3][:, hf * 2:(hf + 1) * 2, :].rearrange("p c n -> p (c n)"), out_offset=None,
                            in_=w2v[:, :], in_offset=off), reads=[idx1, wcres], accum=[w2b[bi % 3]])
                    S.dma("sp", "xb%d" % i, lambda e: e.dma_start(out=xb[i][:], in_=xbuf[bi * BS:(bi + 1) * BS, :].rearrange("(s p) d -> p s d", p=128)),
                          writes=[xb[i]])

                def stX(bi):
                    i = bi % 2
                    xT = xTs[i]
                    for s_ in range(SUB):
                        for c in range(KC):
                            S.op("pe", lambda e, s_=s_, c=c: e.transpose(out=pX[:, c, s_ * 128:(s_ + 1) * 128], in_=xb[i][:, s_, :].rearrange("p (q c) -> p c q", c=8)[:, c, :],
                                                                         identity=ident_b[:]), reads=[xb[i], ident_b], accum=[pX])
                    S.op("act", lambda e: e.copy(out=xT[:], in_=pX[:]), reads=[pX], writes=[xT])

                def stH(bi):
                    i = bi % 2
                    xT, gT = xTs[i], gTs[i]
                    for fc in range(4):
                        for c in range(KC):
                            S.op("pe", lambda e, fc=fc, c=c: e.matmul(ph[:, fc, 0:BS], lhsT=w1b[i][:, c, :].rearrange("p (q f) -> p f q", f=4)[:, fc, :], rhs=xT[:, c, :],
                                                                      start=(c == 0), stop=(c == KC - 1), skip_group_check=True), reads=[w1b[i], xT], accum=[ph])
                        for c in range(KC):
                            S.op("pe", lambda e, fc=fc, c=c: e.matmul(ph[:, fc, 256:256 + BS], lhsT=w3b[i][:, c, :].rearrange("p (q f) -> p f q", f=4)[:, fc, :], rhs=xT[:, c, :],
                                                                      start=(c == 0), stop=(c == KC - 1), skip_group_check=True), reads=[w3b[i], xT], accum=[ph])
                        sa = sact[fc % 2]
                        S.op("act", lambda e, fc=fc, sa=sa: e.activation(out=sa[:], in_=ph[:, fc, 0:BS], func=AF.Silu), reads=[ph], writes=[sa])
                        S.op("dve", lambda e, fc=fc, sa=sa: e.tensor_tensor(out=gT[:, fc, :], in0=sa[:], in1=ph[:, fc, 256:256 + BS], op=ALU.mult),
                             reads=[sa, ph], accum=[gT])

                def stY(bi):
                    i = bi % 2
                    gT = gTs[i]
                    w2t = w2b[bi % 3]
                    for s_ in range(SUB):
                        for nh in range(2):
                            for fc in range(4):
                                S.op("pe", lambda e, s_=s_, nh=nh, fc=fc: e.matmul(py[:, nh, :], lhsT=gT[:, fc, s_ * 128:(s_ + 1) * 128],
                                                                                   rhs=w2t[:, fc, nh * 512:(nh + 1) * 512],
                                                                                   start=(fc == 0), stop=(fc == 3)), reads=[gT, w2t], accum=[py])
                        if s_ % 2 == 0:
                            S.op("act", lambda e, s_=s_: e.copy(out=yo[i][:, s_, :], in_=py[:].rearrange("p a n -> p (a n)")), reads=[py], accum=[yo[i]])
                        else:
                            S.op("dve", lambda e, s_=s_: e.tensor_copy(out=yo[i][:, s_, :], in_=py[:].rearrange("p a n -> p (a n)")), reads=[py], accum=[yo[i]])
                    S.dma("sp", "yo%d" % i, lambda e: e.dma_start(out=ybuf[bi * BS:(bi + 1) * BS, :].rearrange("(s p) d -> p s d", p=128), in_=yo[i][:]),
                          reads=[yo[i]])

                load_blk(0)
                stX(0)
                for bi in range(NBLK):
                    if bi + 1 < NBLK:
                        load_blk(bi + 1)
                    stH(bi)
                    if bi + 1 < NBLK:
                        stX(bi + 1)
                    if bi >= 1:
                        stY(bi - 1)
                stY(NBLK - 1)
            S.barrier()

        def phase5():
            with contextlib.ExitStack() as st:
                gt2 = [load_mod_row(st, "gt2_%d" % bb, bb, 5) for bb in range(NB)]
                ya = [sb(st, "ya%d" % i, [128, D], BF16) for i in range(2)]
                yb = [sb(st, "yb%d" % i, [128, D], BF16) for i in range(2)]
                xl = [sb(st, "xl%d" % i, [128, D], F32) for i in range(2)]
                ma = [sb(st, "ma%d" % i, [128, D], F32) for i in range(2)]
                mb = [sb(st, "mb%d" % i, [128, D], F32) for i in range(2)]
                oo_ = [sb(st, "oo5_%d" % i, [128, D], F32) for i in range(2)]

                def load5(tt):
                    i = tt % 2
                    S.dma("pool", "ya%d" % i, lambda e: e.indirect_dma_start(
                        out=ya[i][:], out_offset=None, in_=ybuf[:, :], in_offset=bass.IndirectOffsetOnAxis(ap=dest_i[:, tt, 0:1], axis=0)),
                        reads=[dest_i], writes=[ya[i]])
                    S.dma("pool", "yb%d" % i, lambda e: e.indirect_dma_start(
                        out=yb[i][:], out_offset=None, in_=ybuf[:, :], in_offset=bass.IndirectOffsetOnAxis(ap=dest_i[:, tt, 1:2], axis=0)),
                        reads=[dest_i], writes=[yb[i]])
                    S.dma("sp", "xl%d" % i, lambda e: e.dma_start(out=xl[i][:], in_=x1s[tt * 128:(tt + 1) * 128, :]), writes=[xl[i]])

                load5(0)
                for tt in range(NTT):
                    i = tt % 2
                    bb = tt // NT
                    t = tt % NT
                    if tt + 1 < NTT:
                        load5(tt + 1)
                    S.op("dve", lambda e: e.tensor_scalar(out=ma[i][:], in0=ya[i][:], scalar1=wt_all[:, tt, 0:1], scalar2=None, op0=ALU.mult),
                         reads=[ya[i], wt_all], writes=[ma[i]])
                    S.op("dve", lambda e: e.scalar_tensor_tensor(out=mb[i][:], in0=yb[i][:], scalar=wt_all[:, tt, 1:2], in1=ma[i][:],
                                                                 op0=ALU.mult, op1=ALU.add), reads=[yb[i], wt_all, ma[i]], writes=[mb[i]])
                    S.op("pool", lambda e: e.tensor_tensor(out=ma[i][:], in0=mb[i][:], in1=gt2[bb][:], op=ALU.mult), reads=[mb[i], gt2[bb]], writes=[ma[i]])
                    S.op("pool", lambda e: e.tensor_tensor(out=oo_[i][:], in0=ma[i][:], in1=xl[i][:], op=ALU.add), reads=[ma[i], xl[i]], writes=[oo_[i]])
                    S.dma("sp", "oo5_%d" % i, lambda e: e.dma_start(out=out[bb, t * 128:(t + 1) * 128, :], in_=oo_[i][:]), reads=[oo_[i]])
            S.barrier()

        for b in range(NB):
            phase1(b)
            if upto >= 2:
                with contextlib.ExitStack() as bst:
                    o_all = sb(bst, "o_all", [128, NT, D], BF16)
                    phase2(b, o_all)
                    if upto >= 3:
                        phase3(b, o_all)
        if upto >= 4:
            phase3b()
            phase4()
        if upto >= 5:
            phase5()
        S.barrier()
        S.final_wait()
    print("instructions:", S.n_ins, "dma sems:", len(S.dsem), "eng sems:", len(S.all_sems))
    return nc


_INVF = (10000.0 ** (-np.arange(0, 64, 2, dtype=np.float32) / np.float32(64))).astype(np.float32).reshape(1, 32)


def make_in_map(inp, core, NB):
    sl = slice(core * NB, (core + 1) * NB)
    f = lambda a: np.ascontiguousarray(a)
    m = {
        "x": f(inp["x"][sl]), "c": f(inp["c"][sl]), "positions": f(inp["positions"][sl]).astype(np.int32),
        "w_ada": f(inp["w_ada"][0]), "b_ada": f(inp["b_ada"][0:1]), "g_norm1": f(inp["g_norm1"][0:1]),
        "w_in": f(inp["w_in"][0]), "b_f": f(inp["b_f"][0:1]),
        "g_q_fox": f(inp["g_q_fox"][0:1]), "g_k_fox": f(inp["g_k_fox"][0:1]),
        "g_q_diff": f(inp["g_q_diff"][0:1]), "g_k_diff": f(inp["g_k_diff"][0:1]),
        "lam_q1": f(inp["lam_q1"][0:1]), "lam_k1": f(inp["lam_k1"][0:1]),
        "lam_q2": f(inp["lam_q2"][0:1]), "lam_k2": f(inp["lam_k2"][0:1]),
        "g_subln": f(inp["g_subln"][0:1]),
        "w_proj_fox": f(inp["w_proj_fox"][0]), "w_proj_diff": f(inp["w_proj_diff"][0]), "w_out": f(inp["w_out"][0]),
        "g_norm2": f(inp["g_norm2"][0:1]),
        "w_router_group": f(inp["w_router_group"][0]), "b_router_group": f(inp["b_router_group"][0:1]),
        "w_router_expert": f(inp["w_router_expert"][0]), "b_router_expert": f(inp["b_router_expert"][0:1]),
        "w1": f(inp["w1"][0]).reshape(NEXP * D, DEXP), "w3": f(inp["w3"][0]).reshape(NEXP * D, DEXP),
        "w2": f(inp["w2"][0]).reshape(NEXP * DEXP, D),
        "invf": _INVF,
    }
    return m


def kernel(**inputs):
    inp = {k: np.asarray(v) for k, v in inputs.items()}
    B, TT, _ = inp["x"].shape
    NB = B // N_CORES
    nc = build_nc(NB, TT)
    in_maps = [make_in_map(inp, c, NB) for c in range(N_CORES)]
    res = run_bass_kernel_spmd(nc, in_maps, core_ids=list(range(N_CORES)))
    return np.concatenate([np.asarray(r["out"]) for r in res.results], axis=0).astype(np.float32)
```

```python
import contextlib
import math
import numpy as np
import concourse.bass as bass
import concourse.mybir as mybir
from concourse.bass_utils import run_bass_kernel_spmd

F32 = mybir.dt.float32
BF16 = mybir.dt.bfloat16
I32 = mybir.dt.int32
U32 = mybir.dt.uint32
AF = mybir.ActivationFunctionType
ALU = mybir.AluOpType
AX = mybir.AxisListType

D = 1024
KC = 8
HD = 64
IN_COLS = 5128
C_FQ, C_FK, C_FV, C_FF, C_DQ, C_DK, C_DV, C_G = 0, 512, 1024, 1536, 1544, 2056, 2568, 3080
NEXP = 32
DEXP = 512
EPS = 1e-6
BS = 256
LAM0 = 0.8 - 0.6 * math.exp(-0.3 * 0)
N_CORES = 8


class Res:
    __slots__ = ("name", "w", "r")

    def __init__(self, name):
        self.name = name
        self.w = {}
        self.r = {}


class T:
    def __init__(self, t, name):
        self.t = t
        self.res = Res(name)

    def __getitem__(self, k):
        return self.t[k]


class Sched:
    EPOCH = 30000

    def __init__(self, nc, es):
        self.nc = nc
        self.es = es
        self.eng = {"pe": nc.tensor, "act": nc.scalar, "dve": nc.vector, "pool": nc.gpsimd, "sp": nc.sync}
        self.sem = {}
        self.cnt = {}
        self.seen = {k: {} for k in self.eng}
        self.all_sems = {}
        for k in self.eng:
            self._new_eng_sem(k)
        self.dsem = {}
        self.dcnt = {}
        self.n_ins = 0
        self.bg_keys = set()

    def _new_eng_sem(self, k):
        s = self.es.enter_context(self.nc.semaphore("s_%s_%d" % (k, len(self.all_sems))))
        self.sem[k] = s
        self.cnt[k] = 0
        self.all_sems[s] = 0

    def _res(self, lst):
        return [x.res if isinstance(x, T) else x for x in lst]

    def _need(self, reads, writes, accum):
        need = {}
        for r in reads:
            for s, v in r.w.items():
                if need.get(s, 0) < v:
                    need[s] = v
        for w in writes:
            for dct in (w.w, w.r):
                for s, v in dct.items():
                    if need.get(s, 0) < v:
                        need[s] = v
        for w in accum:
            for s, v in w.r.items():
                if need.get(s, 0) < v:
                    need[s] = v
        return need

    def _emit_waits(self, e, need):
        eng = self.eng[e]
        seen = self.seen[e]
        waits = [(s, v) for s, v in need.items() if seen.get(s, 0) < v]
        for s, v in waits:
            seen[s] = v
        return eng, waits

    def _record(self, ev_s, ev_v, reads, writes, accum):
        self.all_sems[ev_s] = ev_v
        for r in reads:
            if r.r.get(ev_s, 0) < ev_v:
                r.r[ev_s] = ev_v
        for w in writes:
            w.w = {ev_s: ev_v}
            w.r = {}
        for w in accum:
            if w.w.get(ev_s, 0) < ev_v:
                w.w[ev_s] = ev_v

    def op(self, e, fn, reads=(), writes=(), accum=()):
        reads, writes, accum = self._res(reads), self._res(writes), self._res(accum)
        if self.cnt[e] >= self.EPOCH:
            self._new_eng_sem(e)
        need = self._need(reads, writes, accum)
        eng, waits = self._emit_waits(e, need)
        for s, v in waits:
            eng.wait_ge(s, v)
        ins = fn(eng)
        self.cnt[e] += 1
        ins.then_inc(self.sem[e], 1)
        self._record(self.sem[e], self.cnt[e], reads, writes, accum)
        self.n_ins += 1
        return ins

    def dma(self, q, key, fn, reads=(), writes=(), accum=()):
        reads, writes, accum = self._res(reads), self._res(writes), self._res(accum)
        if key not in self.dsem:
            self.dsem[key] = self.es.enter_context(self.nc.semaphore("d_" + key))
            self.dcnt[key] = 0
        need = self._need(reads, writes, accum)
        eng, waits = self._emit_waits(q, need)
        for s, v in waits:
            eng.wait_ge(s, v)
        ins = fn(eng)
        self.dcnt[key] += 16
        s = self.dsem[key]
        ins.then_inc(s, 16)
        self._record(s, self.dcnt[key], reads, writes, accum)
        self.n_ins += 1
        return ins

    def barrier(self):
        skip = {self.dsem[k] for k in self.bg_keys if k in self.dsem}
        allv = {s: v for s, v in self.all_sems.items() if s not in skip}
        for e in self.eng:
            eng, waits = self._emit_waits(e, allv)
            for s, v in waits:
                if v > 0:
                    eng.wait_ge(s, v)

    def final_wait(self):
        eng, waits = self._emit_waits("sp", dict(self.all_sems))
        for s, v in waits:
            if v > 0:
                eng.wait_ge(s, v)


def build_nc(NB, TT, dbg=False, upto=5):
    NT = TT // 128
    NG = TT // 512
    NTOK = NB * TT
    NTT = NTOK // 128
    NBLK = (NTOK * 2) // BS + NEXP
    PROWS = NBLK * BS
    nc = bass.Bass("TRN2", target_bir_lowering=False)

    def din(name, shape, dt=F32):
        return nc.dram_tensor(name, list(shape), dt, kind="ExternalInput").ap()

    DBG_OUT = ("modv", "qTd", "kTd", "qTf", "x1s", "h2s")

    def dscr(name, shape, dt):
        return nc.dram_tensor(name, list(shape), dt, kind=("ExternalOutput" if (dbg and name in DBG_OUT) else "Internal")).ap()

    x = din("x", [NB, TT, D])
    cin = din("c", [NB, D])
    pos = din("positions", [NB, TT], I32)
    w_ada = din("w_ada", [D, 6 * D])
    b_ada = din("b_ada", [1, 6 * D])
    g_norm1 = din("g_norm1", [1, D])
    w_in = din("w_in", [D, IN_COLS])
    b_f = din("b_f", [1, 8])
    g_q_fox = din("g_q_fox", [1, HD])
    g_k_fox = din("g_k_fox", [1, HD])
    g_q_diff = din("g_q_diff", [1, HD])
    g_k_diff = din("g_k_diff", [1, HD])
    lam_q1 = din("lam_q1", [1, HD])
    lam_k1 = din("lam_k1", [1, HD])
    lam_q2 = din("lam_q2", [1, HD])
    lam_k2 = din("lam_k2", [1, HD])
    g_subln = din("g_subln", [1, 128])
    w_pf = din("w_proj_fox", [512, D])
    w_pd = din("w_proj_diff", [512, D])
    w_out = din("w_out", [D, D])
    g_norm2 = din("g_norm2", [1, D])
    w_rg = din("w_router_group", [D, 4])
    b_rg = din("b_router_group", [1, 4])
    w_re = din("w_router_expert", [D, 32])
    b_re = din("b_router_expert", [1, 32])
    w1 = din("w1", [NEXP * D, DEXP])
    w3 = din("w3", [NEXP * D, DEXP])
    w2 = din("w2", [NEXP * DEXP, D])
    invf = din("invf", [1, 32])
    out = nc.dram_tensor("out", [NB, TT, D], F32, kind="ExternalOutput").ap()

    modv = dscr("modv", [NB, 6, D], F32)
    qTf = dscr("qTf", [NB, 8, 65, TT], BF16)
    kTf = dscr("kTf", [NB, 8, 65, TT], BF16)
    qTd = dscr("qTd", [NB, 8, 64, TT], BF16)
    kTd = dscr("kTd", [NB, 8, 64, TT], BF16)
    vF = dscr("vF", [NB, TT, 8 * 65], BF16)
    vD = dscr("vD", [NB, TT, 4 * 129], BF16)
    gl = dscr("gl", [NB, TT, 2048], BF16)
    x1s = dscr("x1s", [NTOK, D], F32)
    h2s = dscr("h2s", [NTOK, D], BF16)
    w1c = dscr("w1c", [NEXP * D // 4, 2048], BF16)
    w3c = dscr("w3c", [NEXP * D // 4, 2048], BF16)
    w2c = dscr("w2c", [NEXP * DEXP // 2, 2048], BF16)
    xbuf = dscr("xbuf", [PROWS, D], BF16)
    ybuf = dscr("ybuf", [PROWS, D], BF16)
    dbg_t = {}
    if dbg:
        dbg_t["oc"] = nc.dram_tensor("dbg_oc", [NB, TT, D], BF16, kind="ExternalOutput").ap()
        dbg_t["rt"] = nc.dram_tensor("dbg_rt", [128, NTT, 8], F32, kind="ExternalOutput").ap()
        dbg_t["be"] = nc.dram_tensor("dbg_be", [128, NBLK + 64], F32, kind="ExternalOutput").ap()

    es = contextlib.ExitStack()
    with es:
        S = Sched(nc, es)

        uid = [0]

        def sb(stack, name, shape, dt):
            uid[0] += 1
            name = "%s_%d" % (name, uid[0])
            return T(stack.enter_context(nc.sbuf_tensor(name, list(shape), dt)), name)

        def ps(stack, name, shape, dt):
            uid[0] += 1
            name = "%s_%d" % (name, uid[0])
            return T(stack.enter_context(nc.psum_tensor(name, list(shape), dt)), name)

        ident_b = sb(es, "ident_b", [128, 128], BF16)
        ident_f = sb(es, "ident_f", [128, 128], F32)
        tri_i = sb(es, "tri_i", [128, 128], F32)
        tri_s = sb(es, "tri_s", [128, 128], F32)
        ones_f = sb(es, "ones_f", [128, 128], F32)
        G4 = sb(es, "G4", [128, 4, HD], F32)
        bf_rep = sb(es, "bf_rep", [128, 8], F32)
        gsub = sb(es, "gsub", [128, 128], F32)
        nlam = sb(es, "nlam", [128, 1], F32)
        invf_rep = sb(es, "invf_rep", [128, 32], F32)
        ncum = sb(es, "ncum", [128, NB, NT, 8], F32)
        eid_all = sb(es, "eid_all", [128, NTT, 2], F32)
        rk_all = sb(es, "rk_all", [128, NTT, 2], F32)
        wt_all = sb(es, "wt_all", [128, NTT, 2], F32)
        dest_i = sb(es, "dest_i", [128, NTT, 2], U32)
        ecarry = sb(es, "ecarry", [128, 32], F32)
        iota32 = sb(es, "iota32", [128, 32], F32)
        ztile = sb(es, "ztile", [128, D], BF16)
        xz = Res("xbuf_zero")
        S.bg_keys.add("ztile")
        blkidx = {"idx1": sb(es, "idx1", [128, NBLK, 2], U32)}

        def bcast_rows(ap, n):
            return ap.partition_broadcast(n)

        def setup():
            with contextlib.ExitStack() as st:
                zf = sb(st, "zf", [128, 128], F32)
                S.op("pool", lambda e: e.memset(zf[:], 0.0), writes=[zf])
                S.op("pool", lambda e: e.memset(ones_f[:], 1.0), writes=[ones_f])
                S.op("pool", lambda e: e.affine_select(out=ident_f[:], in_=zf[:], pattern=[[-1, 128]],
                                                       compare_op=ALU.not_equal, fill=1.0, base=0,
                                                       channel_multiplier=1), reads=[zf], writes=[ident_f])
                S.op("pool", lambda e: e.affine_select(out=tri_i[:], in_=ones_f[:], pattern=[[1, 128]],
                                                       compare_op=ALU.is_ge, fill=0.0, base=0,
                                                       channel_multiplier=-1), reads=[ones_f], writes=[tri_i])
                S.op("pool", lambda e: e.affine_select(out=tri_s[:], in_=ones_f[:], pattern=[[1, 128]],
                                                       compare_op=ALU.is_ge, fill=0.0, base=-1,
                                                       channel_multiplier=-1), reads=[ones_f], writes=[tri_s])
                S.op("dve", lambda e: e.tensor_copy(out=ident_b[:], in_=ident_f[:]), reads=[ident_f], writes=[ident_b])
                S.op("pool", lambda e: e.memset(ecarry[:], 0.0), writes=[ecarry])
                S.op("pool", lambda e: e.memset(ztile[:], 0.0), writes=[ztile])
                xbv = xbuf.rearrange("(r p) d -> p r d", p=128)
                for r0 in range(PROWS // 128):
                    S.dma("sp", "ztile", lambda e, r0=r0: e.dma_start(out=xbv[:, r0, :], in_=ztile[:]), reads=[ztile], accum=[xz])
                io_i = sb(st, "io_i", [128, 32], I32)
                S.op("pool", lambda e: e.iota(io_i[:], pattern=[[1, 32]], base=0, channel_multiplier=0), writes=[io_i])
                S.op("dve", lambda e: e.tensor_copy(out=iota32[:], in_=io_i[:]), reads=[io_i], writes=[iota32])
                for i, (g, sc) in enumerate([(g_q_fox, 0.125), (g_k_fox, 1.0), (g_q_diff, 0.125), (g_k_diff, 1.0)]):
                    S.dma("sp", "G4", lambda e, g=g, i=i: e.dma_start(out=G4[:, i, :], in_=bcast_rows(g[0:1, :], 128)),
                          accum=[G4])
                S.dma("sp", "bf", lambda e: e.dma_start(out=bf_rep[:], in_=bcast_rows(b_f[0:1, :], 128)), writes=[bf_rep])
                S.dma("sp", "gsub", lambda e: e.dma_start(out=gsub[:], in_=bcast_rows(g_subln[0:1, :], 128)), writes=[gsub])
                S.dma("sp", "invf", lambda e: e.dma_start(out=invf_rep[:], in_=bcast_rows(invf[0:1, :], 128)), writes=[invf_rep])
                S.op("dve", lambda e: e.tensor_scalar(out=G4[:, 0, :], in0=G4[:, 0, :], scalar1=0.125, scalar2=None,
                                                      op0=ALU.mult), reads=[G4], writes=[G4])
                S.op("dve", lambda e: e.tensor_scalar(out=G4[:, 2, :], in0=G4[:, 2, :], scalar1=0.125, scalar2=None,
                                                      op0=ALU.mult), reads=[G4], writes=[G4])
                S.op("dve", lambda e: e.tensor_scalar(out=gsub[:], in0=gsub[:], scalar1=1.0 - LAM0, scalar2=None,
                                                      op0=ALU.mult), reads=[gsub], writes=[gsub])
                lv = sb(st, "lv", [128, 4, HD], F32)
                for i, g in enumerate([lam_q1, lam_k1, lam_q2, lam_k2]):
                    S.dma("sp", "lv", lambda e, g=g, i=i: e.dma_start(out=lv[:, i, :], in_=bcast_rows(g[0:1, :], 128)),
                          accum=[lv])
                lp = sb(st, "lp", [128, 2, HD], F32)
                ls = sb(st, "ls", [128, 2], F32)
                S.op("dve", lambda e: e.tensor_tensor(out=lp[:, 0, :], in0=lv[:, 0, :], in1=lv[:, 1, :], op=ALU.mult),
                     reads=[lv], writes=[lp])
                S.op("dve", lambda e: e.tensor_tensor(out=lp[:, 1, :], in0=lv[:, 2, :], in1=lv[:, 3, :], op=ALU.mult),
                     reads=[lv, lp], accum=[lp])
                S.op("dve", lambda e: e.tensor_reduce(out=ls[:], in_=lp[:], axis=AX.X, op=ALU.add), reads=[lp], writes=[ls])
                S.op("act", lambda e: e.activation(out=ls[:], in_=ls[:], func=AF.Exp), reads=[ls], writes=[ls])
                S.op("dve", lambda e: e.scalar_tensor_tensor(out=nlam[:], in0=ls[:, 1:2], scalar=-LAM0, in1=ls[:, 0:1],
                                                             op0=ALU.add, op1=ALU.subtract), reads=[ls], writes=[nlam])
                cc = sb(st, "cc", [128, 8, NB], F32)
                with nc.allow_non_contiguous_dma(reason="tiny one-time transposed load of c"):
                    for bb in range(NB):
                        S.dma("sp", "cc", lambda e, bb=bb: e.dma_start(out=cc[:, :, bb:bb + 1],
                                                                  in_=cin[bb:bb + 1, :].rearrange("b (kc k) -> k kc b", k=128)),
                              accum=[cc])
                with contextlib.ExitStack() as pst:
                    pc = cc
                    pm = ps(pst, "pm", [128, 512], F32)
                    cact = sb(st, "cact", [128, 8, NB], F32)
                    S.op("act", lambda e: e.activation(out=cact[:], in_=pc[:], func=AF.Silu), reads=[pc], writes=[cact])
                    bad = sb(st, "bad", [128, 6 * D], F32)
                    S.dma("sp", "bad", lambda e: e.dma_start(out=bad[:], in_=bcast_rows(b_ada[0:1, :], 128)), writes=[bad])
                    gg = sb(st, "gg", [128, 2, D], F32)
                    S.dma("sp", "gg", lambda e: e.dma_start(out=gg[:, 0, :], in_=bcast_rows(g_norm1[0:1, :], 128)), accum=[gg])
                    S.dma("sp", "gg", lambda e: e.dma_start(out=gg[:, 1, :], in_=bcast_rows(g_norm2[0:1, :], 128)), accum=[gg])
                    was = [sb(st, "wa%d" % i, [128, 8, 512], BF16) for i in range(2)]
                    creps = [sb(st, "crep%d" % i, [128, 8, 128], BF16) for i in range(NB)]
                    mods = [sb(st, "mod%d" % i, [128, 6 * D], F32) for i in range(NB)]
                    for bb in range(NB):
                        S.op("dve", lambda e, bb=bb: e.tensor_copy(out=creps[bb][:], in_=cact[:, :, bb:bb + 1].to_broadcast([128, 8, 128])),
                             reads=[cact], writes=[creps[bb]])
                    for j in range(12):
                        wa = was[j % 2]
                        S.dma("pool", "wa%d" % (j % 2),
                              lambda e, wa=wa, j=j: e.dma_start(
                                  out=wa[:], in_=w_ada[:, j * 512:(j + 1) * 512].rearrange("(kc k) n -> k kc n", k=128)),
                              writes=[wa])
                        for bb in range(NB):
                            for kc in range(KC):
                                S.op("pe", lambda e, wa=wa, kc=kc, bb=bb: e.matmul(pm[:], lhsT=creps[bb][:, kc, :], rhs=wa[:, kc, :],
                                                                                   start=(kc == 0), stop=(kc == KC - 1)),
                                     reads=[creps[bb], wa], accum=[pm])
                            S.op("dve", lambda e, j=j, bb=bb: e.tensor_tensor(out=mods[bb][:, j * 512:(j + 1) * 512], in0=pm[:],
                                                                              in1=bad[:, j * 512:(j + 1) * 512], op=ALU.add),
                                 reads=[pm, bad], accum=[mods[bb]])
                    for bb in range(NB):
                        mod = mods[bb]
                        for (dst, src, kind) in [(0, 1, "A1"), (1, 0, "c"), (2, 2, "c"), (3, 4, "A2"), (4, 3, "c"), (5, 5, "c")]:
                            if kind == "c":
                                S.dma("sp", "mod%d" % bb, lambda e, dst=dst, src=src, bb=bb, mod=mod: e.dma_start(
                                    out=modv[bb, dst:dst + 1, :], in_=mod[0:1, src * D:(src + 1) * D]), reads=[mod])
                            else:
                                gi = 0 if kind == "A1" else 1
                                S.op("dve", lambda e, src=src, gi=gi, mod=mod: e.scalar_tensor_tensor(
                                    out=mod[:, src * D:(src + 1) * D], in0=mod[:, src * D:(src + 1) * D], scalar=1.0, in1=gg[:, gi, :],
                                    op0=ALU.add, op1=ALU.mult), reads=[mod, gg], writes=[mod])
                                S.dma("sp", "mod%d" % bb, lambda e, dst=dst, src=src, bb=bb, mod=mod: e.dma_start(
                                    out=modv[bb, dst:dst + 1, :], in_=mod[0:1, src * D:(src + 1) * D]), reads=[mod])
                S.barrier()

        setup()

        modv_res = Res("modv")

        def load_mod_row(stack, name, b, i):
            t = sb(stack, name, [128, D], F32)
            S.dma("sp", name, lambda e: e.dma_start(out=t[:], in_=bcast_rows(modv[b, i:i + 1, :], 128)), writes=[t])
            return t

        def phase1(b):
            with contextlib.ExitStack() as st:
                win = sb(st, "win", [128, KC, IN_COLS], BF16)
                for kc in range(KC):
                    for c0 in range(0, IN_COLS, 1024):
                        c1 = min(c0 + 1024, IN_COLS)
                        S.dma("pool", "win", lambda e, kc=kc, c0=c0, c1=c1: e.dma_start(
                            out=win[:, kc, c0:c1], in_=w_in[kc * 128:(kc + 1) * 128, c0:c1]), accum=[win])
                A1 = load_mod_row(st, "A1", b, 0)
                B1 = load_mod_row(st, "B1", b, 1)
                cosT = sb(st, "cosT", [128, NT, 32], F32)
                sinT = sb(st, "sinT", [128, NT, 32], F32)
                with contextlib.ExitStack() as st2:
                    pi_ = sb(st2, "pi_", [128, NT], I32)
                    with nc.allow_non_contiguous_dma(reason="one-time transposed load of positions"):
                        S.dma("sp", "pi", lambda e: e.dma_start(out=pi_[:], in_=pos[b, :].rearrange("(t p) -> p t", p=128)), writes=[pi_])
                    posf = sb(st2, "posf", [128, NT], F32)
                    S.op("dve", lambda e: e.tensor_copy(out=posf[:], in_=pi_[:]), reads=[pi_], writes=[posf])
                    ang = sb(st2, "ang", [128, NT, 32], F32)
                    S.op("dve", lambda e: e.tensor_tensor(out=ang[:], in0=posf[:].unsqueeze(2).to_broadcast([128, NT, 32]),
                                                          in1=invf_rep[:].unsqueeze(1).to_broadcast([128, NT, 32]), op=ALU.mult),
                         reads=[posf, invf_rep], writes=[ang])
                    tq = sb(st2, "tq", [128, NT, 32], F32)
                    nq = sb(st2, "nq", [128, NT, 32], F32)
                    MAGIC = 12582912.0
                    C1 = 6.28125
                    C2 = 2.0 * math.pi - 6.28125
                    for (dst, shift) in [(sinT, 0.0), (cosT, math.pi / 2)]:
                        S.op("dve", lambda e, shift=shift: e.tensor_scalar(out=tq[:], in0=ang[:], scalar1=shift, scalar2=None,
                                                                          op0=ALU.add), reads=[ang], writes=[tq])
                        S.op("dve", lambda e: e.tensor_scalar(out=nq[:], in0=tq[:], scalar1=1.0 / (2 * math.pi), scalar2=MAGIC,
                                                              op0=ALU.mult, op1=ALU.add), reads=[tq], writes=[nq])
                        S.op("dve", lambda e: e.tensor_scalar(out=nq[:], in0=nq[:], scalar1=-MAGIC, scalar2=None,
                                                              op0=ALU.add), reads=[nq], writes=[nq])
                        S.op("dve", lambda e: e.scalar_tensor_tensor(out=tq[:], in0=nq[:], scalar=-C1, in1=tq[:],
                                                                     op0=ALU.mult, op1=ALU.add), reads=[nq, tq], writes=[tq])
                        S.op("dve", lambda e: e.scalar_tensor_tensor(out=tq[:], in0=nq[:], scalar=-C2, in1=tq[:],
                                                                     op0=ALU.mult, op1=ALU.add), reads=[nq, tq], writes=[tq])
                        S.op("dve", lambda e: e.tensor_scalar(out=tq[:], in0=tq[:], scalar1=3.1415925, scalar2=-3.1415925,
                                                              op0=ALU.min, op1=ALU.max), reads=[tq], writes=[tq])
                        S.op("act", lambda e, dst=dst: e.activation(out=dst[:], in_=tq[:], func=AF.Sin), reads=[tq], writes=[dst])
                S.barrier()
                NBUF = 2
                xt = [sb(st, "xt%d" % i, [128, D], F32) for i in range(NBUF)]
                tmp = [sb(st, "tmp%d" % i, [128, D], F32) for i in range(NBUF)]
                hb = [sb(st, "hb%d" % i, [128, D], BF16) for i in range(NBUF)]
                hT = [sb(st, "hT%d" % i, [128, KC, 128], BF16) for i in range(NBUF)]
                sq = sb(st, "sq", [128, D], F32)
                ss = [sb(st, "ss%d" % i, [128, 4], F32) for i in range(NBUF)]
                ms8 = [sb(st, "ms8_%d" % i, [128, 8], F32) for i in range(4)]
                qn = [sb(st, "qn%d" % i, [128, 8, HD], F32) for i in range(2)]
                qg = [sb(st, "qg%d" % i, [128, 8, HD], F32) for i in range(2)]
                ra = [sb(st, "ra%d" % i, [128, 8, 32], F32) for i in range(4)]
                QKF = [sb(st, "QKF%d" % i, [128, 16, 66], BF16) for i in range(NBUF)]
                QKD = [sb(st, "QKD%d" % i, [128, 16, HD], BF16) for i in range(NBUF)]
                QTs = [sb(st, "QTs%d" % i, [65, 32, 128], BF16) for i in range(NBUF)]
                VF = [sb(st, "VF%d" % i, [128, 8, 65], BF16) for i in range(NBUF)]
                VD = [sb(st, "VD%d" % i, [128, 4, 129], BF16) for i in range(NBUF)]
                GT = [sb(st, "GT%d" % i, [128, 2048], BF16) for i in range(NBUF)]
                fz = [sb(st, "fz%d" % i, [128, 8], F32) for i in range(3)]
                carry = sb(st, "carry", [128, 8], F32)
                S.op("pool", lambda e: e.memset(carry[:], 0.0), writes=[carry])
                for i in range(NBUF):
                    S.op("pool", lambda e, i=i: e.memset(QKF[i][:], 0.0), writes=[QKF[i]])
                    S.op("pool", lambda e, i=i: e.memset(QKF[i][:, 8:16, 64:65], 1.0), reads=[QKF[i]], accum=[QKF[i]])
                    S.op("pool", lambda e, i=i: e.memset(VF[i][:, :, 64:65], 1.0), writes=[VF[i]])
                    S.op("pool", lambda e, i=i: e.memset(VD[i][:, :, 128:129], 1.0), writes=[VD[i]])
                pT = ps(st, "pT", [128, KC, 128], BF16)
                zb = [ps(st, "zb%d" % i, [128, 512], F32) for i in range(3)]
                psm = ps(st, "psm", [128, 16], F32)
                qtp = ps(st, "qtp", [65, 16, 128], BF16)
                zbi = [0]

                def zgroup(hTt, col0, ncols):
                    z = zb[zbi[0] % 3]
                    zbi[0] += 1
                    for kc in range(KC):
                        S.op("pe", lambda e, kc=kc: e.matmul(z[:, 0:ncols], lhsT=hTt[:, kc, :], rhs=win[:, kc, col0:col0 + ncols],
                                                             start=(kc == 0), stop=(kc == KC - 1)),
                             reads=[hTt, win], accum=[z])
                    return z

                def headnorm(z, gi, mi, dst_fn, rope_t=None):
                    m = ms8[mi]
                    S.op("act", lambda e: e.activation(out=sq[:, 0:512], in_=z[:], func=AF.Square), reads=[z], writes=[sq])
                    S.op("dve", lambda e: e.tensor_reduce(out=m[:], in_=sq[:, 0:512].rearrange("p (h d) -> p h d", d=HD),
                                                          axis=AX.X, op=ALU.add), reads=[sq], writes=[m])
                    S.op("act", lambda e: e.activation(out=m[:], in_=m[:], func=AF.Ln, scale=1.0 / HD, bias=EPS), reads=[m], writes=[m])
                    S.op("act", lambda e: e.activation(out=m[:], in_=m[:], func=AF.Exp, scale=-0.5), reads=[m], writes=[m])
                    q1 = qn[mi % 2]
                    S.op("dve", lambda e: e.tensor_tensor(out=q1[:], in0=z[:].rearrange("p (h d) -> p h d", d=HD),
                                                          in1=m[:].unsqueeze(2).to_broadcast([128, 8, HD]), op=ALU.mult),
                         reads=[z, m], writes=[q1])
                    gb = G4[:, gi, :].unsqueeze(1).to_broadcast([128, 8, HD])
                    if rope_t is None:
                        S.op("pool", lambda e: e.tensor_tensor(out=dst_fn(0, HD), in0=q1[:], in1=gb, op=ALU.mult),
                             reads=[q1, G4], accum=[dst_fn.tile])
                    else:
                        q2 = qg[mi % 2]
                        S.op("pool", lambda e: e.tensor_tensor(out=q2[:], in0=q1[:], in1=gb, op=ALU.mult),
                             reads=[q1, G4], writes=[q2])
                        cb = cosT[:, rope_t, :].unsqueeze(1).to_broadcast([128, 8, 32])
                        sbb = sinT[:, rope_t, :].unsqueeze(1).to_broadcast([128, 8, 32])
                        x1 = q2[:, :, 0:32]
                        x2 = q2[:, :, 32:64]
                        S.op("dve", lambda e: e.tensor_tensor(out=ra[0][:], in0=x1, in1=cb, op=ALU.mult), reads=[q2, cosT], writes=[ra[0]])
                        S.op("pool", lambda e: e.tensor_tensor(out=ra[1][:], in0=x2, in1=sbb, op=ALU.mult), reads=[q2, sinT], writes=[ra[1]])
                        S.op("dve", lambda e: e.tensor_tensor(out=ra[2][:], in0=x2, in1=cb, op=ALU.mult), reads=[q2, cosT], writes=[ra[2]])
                        S.op("pool", lambda e: e.tensor_tensor(out=ra[3][:], in0=x1, in1=sbb, op=ALU.mult), reads=[q2, sinT], writes=[ra[3]])
                        S.op("dve", lambda e: e.tensor_tensor(out=dst_fn(0, 32), in0=ra[0][:], in1=ra[1][:], op=ALU.subtract),
                             reads=[ra[0], ra[1]], accum=[dst_fn.tile])
                        S.op("pool", lambda e: e.tensor_tensor(out=dst_fn(32, 64), in0=ra[2][:], in1=ra[3][:], op=ALU.add),
                             reads=[ra[2], ra[3]], accum=[dst_fn.tile])

                def mkdst(tile, h0):
                    def f(a, bb):
                        return tile[:, h0:h0 + 8, a:bb]
                    f.tile = tile
                    return f

                def load_x(t):
                    i = t % NBUF
                    S.dma("sp", "xt%d" % i, lambda e: e.dma_start(out=xt[i][:], in_=x[b, t * 128:(t + 1) * 128, :]), writes=[xt[i]])

                def pre_a(t):
                    i = t % NBUF
                    ssi = ss[i]
                    S.op("act", lambda e: e.activation(out=sq[:], in_=xt[i][:], func=AF.Square, accum_out=ssi[:, 0:1]),
                         reads=[xt[i]], writes=[sq, ssi])
                    S.op("act", lambda e: e.activation(out=ssi[:, 1:2], in_=ssi[:, 0:1], func=AF.Ln, scale=1.0 / D, bias=EPS),
                         reads=[ssi], accum=[ssi])
                    S.op("act", lambda e: e.activation(out=ssi[:, 2:3], in_=ssi[:, 1:2], func=AF.Exp, scale=-0.5),
                         reads=[ssi], accum=[ssi])
                    S.op("dve", lambda e: e.scalar_tensor_tensor(out=tmp[i][:], in0=xt[i][:], scalar=ssi[:, 2:3], in1=A1[:],
                                                                 op0=ALU.mult, op1=ALU.mult), reads=[xt[i], ssi, A1], writes=[tmp[i]])
                    S.op("pool", lambda e: e.tensor_tensor(out=hb[i][:], in0=tmp[i][:], in1=B1[:], op=ALU.add),
                         reads=[tmp[i], B1], writes=[hb[i]])

                def pre_b(t):
                    i = t % NBUF
                    for kc in range(KC):
                        S.op("pe", lambda e, kc=kc: e.transpose(out=pT[:, kc, :], in_=hb[i][:, kc * 128:(kc + 1) * 128], identity=ident_b[:]),
                             reads=[hb[i], ident_b], accum=[pT])
                    S.op("act", lambda e: e.copy(out=hT[i][:], in_=pT[:]), reads=[pT], writes=[hT[i]])

                def zstage(t):
                    i = t % NBUF
                    z = zgroup(hT[i], C_FF, 8)
                    f0, f1, f2 = fz
                    S.op("dve", lambda e: e.tensor_tensor(out=f0[:], in0=z[:, 0:8], in1=bf_rep[:], op=ALU.add), reads=[z, bf_rep], writes=[f0])
                    S.op("act", lambda e: e.activation(out=f1[:], in_=f0[:], func=AF.Exp, scale=-1.0), reads=[f0], writes=[f1])
                    S.op("act", lambda e: e.activation(out=f2[:], in_=f1[:], func=AF.Ln, bias=1.0), reads=[f1], writes=[f2])
                    z = zgroup(hT[i], C_FQ, 512)
                    headnorm(z, 0, 0, mkdst(QKF[i], 0))
                    z = zgroup(hT[i], C_FK, 512)
                    headnorm(z, 1, 1, mkdst(QKF[i], 8))
                    z = zgroup(hT[i], C_FV, 512)
                    S.op("act", lambda e, z=z: e.copy(out=VF[i][:, :, 0:64], in_=z[:].rearrange("p (h d) -> p h d", d=HD)),
                         reads=[z], accum=[VF[i]])
                    S.op("pe", lambda e: e.matmul(psm[:, 0:8], lhsT=tri_i[:], rhs=f2[:], start=True, stop=True),
                         reads=[tri_i, f2], accum=[psm])
                    S.op("pe", lambda e: e.matmul(psm[:, 8:16], lhsT=ones_f[:], rhs=f2[:], start=False, stop=True, skip_group_check=True),
                         reads=[ones_f, f2], accum=[psm])
                    S.op("dve", lambda e: e.tensor_tensor(out=ncum[:, b, t, :], in0=psm[:, 0:8], in1=carry[:], op=ALU.add),
                         reads=[psm, carry], accum=[ncum])
                    S.op("dve", lambda e: e.tensor_tensor(out=carry[:], in0=psm[:, 8:16], in1=carry[:], op=ALU.add),
                         reads=[psm, carry], writes=[carry])
                    S.op("dve", lambda e: e.tensor_scalar(out=QKF[i][:, 0:8, 64:65], in0=ncum[:, b, t, :].unsqueeze(2), scalar1=-1.0,
                                                          scalar2=None, op0=ALU.mult), reads=[ncum], accum=[QKF[i]])
                    z = zgroup(hT[i], C_DQ, 512)
                    headnorm(z, 2, 2, mkdst(QKD[i], 0), rope_t=t)
                    z = zgroup(hT[i], C_DK, 512)
                    headnorm(z, 3, 3, mkdst(QKD[i], 8), rope_t=t)
                    z = zgroup(hT[i], C_DV, 512)
                    S.op("act", lambda e, z=z: e.copy(out=VD[i][:, :, 0:128], in_=z[:].rearrange("p (h d) -> p h d", d=128)),
                         reads=[z], accum=[VD[i]])
                    for gi in range(4):
                        z = zgroup(hT[i], C_G + gi * 512, 512)
                        if gi % 2 == 0:
                            S.op("act", lambda e, z=z, gi=gi: e.copy(out=GT[i][:, gi * 512:(gi + 1) * 512], in_=z[:]), reads=[z], accum=[GT[i]])
                        else:
                            S.op("dve", lambda e, z=z, gi=gi: e.tensor_copy(out=GT[i][:, gi * 512:(gi + 1) * 512], in_=z[:]), reads=[z], accum=[GT[i]])
                    for h in range(16):
                        S.op("pe", lambda e, h=h: e.transpose(out=qtp[0:65, h, :], in_=QKF[i][:, h, 0:65], identity=ident_b[:]),
                             reads=[QKF[i], ident_b], accum=[qtp])
                    S.op("act", lambda e: e.copy(out=QTs[i][0:65, 0:16, :], in_=qtp[0:65, :, :]), reads=[qtp], accum=[QTs[i]])
                    for h in range(16):
                        S.op("pe", lambda e, h=h: e.transpose(out=qtp[0:64, h, :], in_=QKD[i][:, h, :], identity=ident_b[:]),
                             reads=[QKD[i], ident_b], accum=[qtp])
                    S.op("dve", lambda e: e.tensor_copy(out=QTs[i][0:64, 16:32, :], in_=qtp[0:64, :, :]), reads=[qtp], accum=[QTs[i]])
                    cs = slice(t * 128, (t + 1) * 128)
                    S.dma("sp", "sQ%d" % i, lambda e: e.dma_start(out=qTf[b].rearrange("h r t -> r h t")[:, :, cs], in_=QTs[i][0:65, 0:8, :]), reads=[QTs[i]])
                    S.dma("sp", "sQ%d" % i, lambda e: e.dma_start(out=kTf[b].rearrange("h r t -> r h t")[:, :, cs], in_=QTs[i][0:65, 8:16, :]), reads=[QTs[i]])
                    S.dma("sp", "sQ%d" % i, lambda e: e.dma_start(out=qTd[b].rearrange("h r t -> r h t")[:, :, cs], in_=QTs[i][0:64, 16:24, :]), reads=[QTs[i]])
                    S.dma("sp", "sQ%d" % i, lambda e: e.dma_start(out=kTd[b].rearrange("h r t -> r h t")[:, :, cs], in_=QTs[i][0:64, 24:32, :]), reads=[QTs[i]])
                    S.dma("sp", "sVF%d" % i, lambda e: e.dma_start(out=vF[b, cs, :], in_=VF[i][:].rearrange("p h d -> p (h d)")), reads=[VF[i]])
                    S.dma("sp", "sVD%d" % i, lambda e: e.dma_start(out=vD[b, cs, :], in_=VD[i][:].rearrange("p h d -> p (h d)")), reads=[VD[i]])
                    S.dma("sp", "sGT%d" % i, lambda e: e.dma_start(out=gl[b, cs, :], in_=GT[i][:]), reads=[GT[i]])

                load_x(0)
                if NT > 1:
                    load_x(1)
                pre_a(0)
                pre_b(0)
                for t in range(NT):
                    if t + 1 < NT:
                        pre_a(t + 1)
                    if t + 2 < NT:
                        load_x(t + 2)
                    zstage(t)
                    if t + 1 < NT:
                        pre_b(t + 1)
                S.barrier()


        wcres = Res("wcast")
        CH = 64
        castjobs = []
        for (src, dst, key, c) in ((w1, w1c, "wc1", 4), (w3, w3c, "wc3", 4), (w2, w2c, "wc2", 2)):
            srcv = src.rearrange("(r c) n -> r (c n)", c=c)
            for r0 in range(0, 8192, CH):
                castjobs.append((srcv, dst, key, r0))
        castpos = [0]
        for key in ("wc1", "wc2", "wc3"):
            S.bg_keys.add(key)

        def bg_cast(n=1):
            for _ in range(n):
                if castpos[0] >= len(castjobs):
                    return
                srcv, dst, key, r0 = castjobs[castpos[0]]
                castpos[0] += 1
                S.dma("pool", key, lambda e: e.dma_start(out=dst[r0:r0 + CH, :], in_=srcv[r0:r0 + CH, :]), accum=[wcres])

        def phase2(b, o_all):
            with contextlib.ExitStack() as st:
                sbk = [ps(st, "sbk%d" % i, [128, 512], F32) for i in range(3)]
                obs = [ps(st, "ob%d" % i, [128, 4, 65], F32) for i in range(2)]
                od = ps(st, "od", [128, 3, 512], F32)
                pts = [sb(st, "pt%d" % i, [128, 512], BF16) for i in range(4)]
                cnt = [0]
                with contextlib.ExitStack() as st2:
                    QT = [sb(st2, "QTf%d" % i, [65, TT], BF16) for i in range(2)]
                    KT = [sb(st2, "KTf%d" % i, [65, TT], BF16) for i in range(2)]
                    VT = [sb(st2, "VTf%d" % i, [128, NT, 65], BF16) for i in range(2)]
                    rec = [sb(st2, "rec%d" % i, [128, 4], F32) for i in range(2)]

                    def load_f(h):
                        i = h % 2
                        S.dma("sp", "QTf%d" % i, lambda e: e.dma_start(out=QT[i][:], in_=qTf[b, h, :, :]), writes=[QT[i]])
                        S.dma("sp", "KTf%d" % i, lambda e: e.dma_start(out=KT[i][:], in_=kTf[b, h, :, :]), writes=[KT[i]])
                        S.dma("sp", "VTf%d" % i, lambda e: e.dma_start(
                            out=VT[i][:], in_=vF[b].rearrange("(t p) c -> p t c", p=128)[:, :, h * 65:(h + 1) * 65]), writes=[VT[i]])

                    tasks = [(h, g, kt) for h in range(8) for g in range(NG) for kt in range(4 * g + 4)]

                    def qk_f(p):
                        h, g, kt = tasks[p]
                        i = h % 2
                        c0 = 128 * max(kt - 4 * g, 0)
                        sbank = sbk[p % 3]
                        S.op("pe", lambda e: e.matmul(sbank[:, c0:512], lhsT=KT[i][:, kt * 128:(kt + 1) * 128],
                                                      rhs=QT[i][:, g * 512 + c0:(g + 1) * 512], start=True, stop=True),
                             reads=[KT[i], QT[i]], accum=[sbank])

                    def rest_f(p):
                        h, g, kt = tasks[p]
                        i = h % 2
                        j = kt - 4 * g
                        jb = max(j, 0)
                        c0 = 128 * jb
                        sbank = sbk[p % 3]
                        pt = pts[p % 4]
                        ob = obs[g % 2]
                        S.op("act", lambda e: e.activation(out=pt[:, c0:512], in_=sbank[:, c0:512], func=AF.Exp,
                                                           bias=ncum[:, b, kt, h:h + 1]), reads=[sbank, ncum], writes=[pt])
                        if j >= 0:
                            S.op("pool", lambda e: e.affine_select(out=pt[:, c0:c0 + 128], in_=pt[:, c0:c0 + 128], pattern=[[1, 128]],
                                                                   compare_op=ALU.is_ge, fill=0.0, base=0, channel_multiplier=-1),
                                 reads=[pt], writes=[pt])
                        for qb in range(jb, 4):
                            S.op("pe", lambda e, qb=qb: e.matmul(
                                ob[:, qb, :], lhsT=pt[:, qb * 128:(qb + 1) * 128], rhs=VT[i][:, kt, :],
                                start=(kt == 0 and qb == 0), stop=(kt == 4 * g + qb), skip_group_check=True), reads=[pt, VT[i]], accum=[ob])
                        if kt == 4 * g + 3:
                            r = rec[g % 2]
                            S.op("dve", lambda e: e.reciprocal(out=r[:], in_=ob[:, :, 64]), reads=[ob], writes=[r])
                            S.op("dve", lambda e: e.tensor_tensor(out=o_all[:, g * 4:(g + 1) * 4, h * 64:(h + 1) * 64], in0=ob[:, :, 0:64],
                                                                  in1=r[:].unsqueeze(2).to_broadcast([128, 4, 64]), op=ALU.mult),
                                 reads=[ob, r], accum=[o_all])

                    LA = 2
                    load_f(0)
                    for p in range(min(LA, len(tasks))):
                        if tasks[p][1] == 0 and tasks[p][2] == 0 and tasks[p][0] + 1 < 8:
                            pass
                        qk_f(p)
                    for p in range(len(tasks)):
                        h, g, kt = tasks[p]
                        if g == 0 and kt == 0 and h + 1 < 8:
                            load_f(h + 1)
                        if p + LA < len(tasks):
                            qk_f(p + LA)
                        rest_f(p)
                        if p % 5 == 4:
                            bg_cast()
                S.barrier()
                with contextlib.ExitStack() as st2:
                    QT2 = [sb(st2, "QTd%d" % i, [64, 2, TT], BF16) for i in range(2)]
                    KT2 = [sb(st2, "KTd%d" % i, [64, 2, TT], BF16) for i in range(2)]
                    VT2 = [sb(st2, "VTd%d" % i, [128, NT, 129], BF16) for i in range(2)]
                    onorm = sb(st2, "onorm", [128, 8, 128], F32)
                    oo = sb(st2, "oo", [128, 4, 128], F32)
                    sq2 = sb(st2, "sq2", [128, 4, 128], F32)
                    recd = sb(st2, "recd", [128, 8], F32)
                    msd = sb(st2, "msd", [128, 4], F32)

                    def load_d(hd):
                        i = hd % 2
                        S.dma("sp", "QTd%d" % i, lambda e: e.dma_start(
                            out=QT2[i][:], in_=qTd[b, hd * 2:hd * 2 + 2, :, :].rearrange("m r t -> r m t")), writes=[QT2[i]])
                        S.dma("sp", "KTd%d" % i, lambda e: e.dma_start(
                            out=KT2[i][:], in_=kTd[b, hd * 2:hd * 2 + 2, :, :].rearrange("m r t -> r m t")), writes=[KT2[i]])
                        S.dma("sp", "VTd%d" % i, lambda e: e.dma_start(
                            out=VT2[i][:], in_=vD[b].rearrange("(t p) c -> p t c", p=128)[:, :, hd * 129:(hd + 1) * 129]), writes=[VT2[i]])

                    tasks = [(hd, g, kt, m) for hd in range(4) for g in range(NG) for kt in range(4 * g + 4) for m in range(2)]

                    def qk_d(p):
                        hd, g, kt, m = tasks[p]
                        i = hd % 2
                        c0 = 128 * max(kt - 4 * g, 0)
                        sbank = sbk[p % 3]
                        S.op("pe", lambda e: e.matmul(sbank[:, c0:512], lhsT=KT2[i][:, m, kt * 128:(kt + 1) * 128],
                                                      rhs=QT2[i][:, m, g * 512 + c0:(g + 1) * 512], start=True, stop=True),
                             reads=[KT2[i], QT2[i]], accum=[sbank])

                    def rest_d(p):
                        hd, g, kt, m = tasks[p]
                        i = hd % 2
                        j = kt - 4 * g
                        jb = max(j, 0)
                        c0 = 128 * jb
                        sbank = sbk[p % 3]
                        pt = pts[p % 4]
                        S.op("act", lambda e: e.activation(out=pt[:, c0:512], in_=sbank[:, c0:512], func=AF.Exp),
                             reads=[sbank], writes=[pt])
                        if j >= 0:
                            S.op("pool", lambda e: e.memset(pt[64:128, c0:c0 + 64], 0.0), reads=[pt], writes=[pt])
                        for qb in range(jb, 4):
                            idx = m * 4 + qb
                            bank = idx // 3
                            off = (idx % 3) * 129
                            S.op("pe", lambda e, qb=qb, bank=bank, off=off, idx=idx: e.matmul(
                                od[:, bank, off:off + 129], lhsT=pt[:, qb * 128:(qb + 1) * 128], rhs=VT2[i][:, kt, :],
                                start=(kt == 0 and idx in (0, 3, 6)), stop=(kt == 4 * g + qb), skip_group_check=True), reads=[pt, VT2[i]], accum=[od])
                        if kt == 4 * g + 3 and m == 1:
                            for bank in range(3):
                                n = 3 if bank < 2 else 2
                                view = od[:, bank, 0:n * 129].rearrange("p (a c) -> p a c", c=129)
                                S.op("dve", lambda e, view=view, bank=bank, n=n: e.reciprocal(out=recd[:, bank * 3:bank * 3 + n], in_=view[:, :, 128]),
                                     reads=[od], accum=[recd])
                                S.op("dve", lambda e, view=view, bank=bank, n=n: e.tensor_tensor(
                                    out=onorm[:, bank * 3:bank * 3 + n, :], in0=view[:, :, 0:128],
                                    in1=recd[:, bank * 3:bank * 3 + n].unsqueeze(2).to_broadcast([128, n, 128]), op=ALU.mult),
                                     reads=[od, recd], accum=[onorm])
                            S.op("dve", lambda e: e.scalar_tensor_tensor(out=oo[:], in0=onorm[:, 4:8, :], scalar=nlam[:, 0:1], in1=onorm[:, 0:4, :],
                                                                         op0=ALU.mult, op1=ALU.add), reads=[onorm, nlam], writes=[oo])
                            S.op("pool", lambda e: e.tensor_tensor(out=sq2[:], in0=oo[:], in1=oo[:], op=ALU.mult), reads=[oo], writes=[sq2])
                            S.op("dve", lambda e: e.tensor_reduce(out=msd[:], in_=sq2[:], axis=AX.X, op=ALU.add), reads=[sq2], writes=[msd])
                            S.op("act", lambda e: e.activation(out=msd[:], in_=msd[:], func=AF.Ln, scale=1.0 / 128, bias=EPS), reads=[msd], writes=[msd])
                            S.op("act", lambda e: e.activation(out=msd[:], in_=msd[:], func=AF.Exp, scale=-0.5), reads=[msd], writes=[msd])
                            S.op("dve", lambda e: e.tensor_tensor(out=oo[:], in0=oo[:], in1=msd[:].unsqueeze(2).to_broadcast([128, 4, 128]), op=ALU.mult),
                                 reads=[oo, msd], writes=[oo])
                            S.op("pool", lambda e: e.tensor_tensor(out=o_all[:, g * 4:(g + 1) * 4, 512 + hd * 128:512 + (hd + 1) * 128], in0=oo[:],
                                                                   in1=gsub[:].unsqueeze(1).to_broadcast([128, 4, 128]), op=ALU.mult),
                                 reads=[oo, gsub], accum=[o_all])

                    LA = 2
                    load_d(0)
                    for p in range(min(LA, len(tasks))):
                        qk_d(p)
                    for p in range(len(tasks)):
                        hd, g, kt, m = tasks[p]
                        if g == 0 and kt == 0 and m == 0 and hd + 1 < 4:
                            load_d(hd + 1)
                        if p + LA < len(tasks):
                            qk_d(p + LA)
                        rest_d(p)
                        if p % 5 == 4:
                            bg_cast()
            S.barrier()
            if dbg:
                S.dma("sp", "dbgoc", lambda e: e.dma_start(out=dbg_t["oc"][b].rearrange("(t p) d -> p t d", p=128), in_=o_all[:]), reads=[o_all])

        def phase3(b, o_all):
            with contextlib.ExitStack() as st:
                wpf = sb(st, "wpf", [128, 4, D], BF16)
                wpd = sb(st, "wpd", [128, 4, D], BF16)
                wo = sb(st, "wo", [128, 8, D], BF16)
                wr = sb(st, "wr", [128, 8, 36], BF16)
                brt = sb(st, "brt", [128, 36], F32)
                S.dma("pool", "wpf", lambda e: e.dma_start(out=wpf[:], in_=w_pf.rearrange("(c k) n -> k c n", k=128)), writes=[wpf])
                S.dma("pool", "wpd", lambda e: e.dma_start(out=wpd[:], in_=w_pd.rearrange("(c k) n -> k c n", k=128)), writes=[wpd])
                S.dma("pool", "wo", lambda e: e.dma_start(out=wo[:], in_=w_out.rearrange("(c k) n -> k c n", k=128)), writes=[wo])
                S.dma("pool", "wr", lambda e: e.dma_start(out=wr[:, :, 0:4], in_=w_rg.rearrange("(c k) n -> k c n", k=128)), accum=[wr])
                S.dma("pool", "wr", lambda e: e.dma_start(out=wr[:, :, 4:36], in_=w_re.rearrange("(c k) n -> k c n", k=128)), accum=[wr])
                S.dma("sp", "brt", lambda e: e.dma_start(out=brt[:, 0:4], in_=bcast_rows(b_rg[0:1, :], 128)), accum=[brt])
                S.dma("sp", "brt", lambda e: e.dma_start(out=brt[:, 4:36], in_=bcast_rows(b_re[0:1, :], 128)), accum=[brt])
                gt1 = load_mod_row(st, "gt1", b, 2)
                A2 = load_mod_row(st, "A2", b, 3)
                B2 = load_mod_row(st, "B2", b, 4)
                NBUF = 2
                xt = [sb(st, "x3_%d" % i, [128, D], F32) for i in range(3)]
                gls = [sb(st, "gls%d" % i, [128, 2048], BF16) for i in range(3)]
                sgs = [sb(st, "sg0", [128, 2048], BF16)] * NBUF
                oTs = [sb(st, "oT0", [128, KC, 128], BF16)] * NBUF
                m1s = [sb(st, "m1_0", [128, D], F32)] * NBUF
                m2s = [sb(st, "m2_0", [128, D], F32)] * NBUF
                mgs = [sb(st, "mg%d" % i, [128, D], BF16) for i in range(NBUF)]
                mTs = [sb(st, "mT0", [128, KC, 128], BF16)] * NBUF
                x1 = [sb(st, "x1_%d" % i, [128, D], F32) for i in range(NBUF)]
                tmps = [sb(st, "tmp3_0", [128, D], F32)] * NBUF
                sqj = sb(st, "sqj", [128, D], BF16)
                h2 = [sb(st, "h2_%d" % i, [128, D], BF16) for i in range(NBUF)]
                h2T = sb(st, "h2T", [128, KC, 128], BF16)
                ss = sb(st, "ss3", [128, 4], F32)
                Lall = sb(st, "Lall", [128, NT, 36], F32)
                rsm = sb(st, "rsm", [128, 16, NT], F32)
                r4 = sb(st, "r4", [128, 3, NT * 4], F32)
                pT = ps(st, "pT3", [128, KC, 128], BF16)
                pfd = ps(st, "pfd", [128, 4, 512], F32)
                pyy = ps(st, "pyy", [128, 2, 512], F32)
                prt = ps(st, "prt", [128, 128], F32)

                def load3(t):
                    i = t % 3
                    S.dma("sp", "x3_%d" % i, lambda e: e.dma_start(out=xt[i][:], in_=x[b, t * 128:(t + 1) * 128, :]), writes=[xt[i]])
                    S.dma("sp", "gls%d" % i, lambda e: e.dma_start(out=gls[i][:], in_=gl[b, t * 128:(t + 1) * 128, :]), writes=[gls[i]])

                def transp(src_ap_fn, src_t, dstT, eng):
                    for kc in range(KC):
                        S.op("pe", lambda e, kc=kc: e.transpose(out=pT[:, kc, :], in_=src_ap_fn(kc), identity=ident_b[:]),
                             reads=[src_t, ident_b], accum=[pT])
                    if eng == "act":
                        S.op("act", lambda e: e.copy(out=dstT[:], in_=pT[:]), reads=[pT], writes=[dstT])
                    else:
                        S.op("dve", lambda e: e.tensor_copy(out=dstT[:], in_=pT[:]), reads=[pT], writes=[dstT])

                def stageA(t):
                    i = t % NBUF
                    oT, sg, m1, m2, mg = oTs[i], sgs[i], m1s[i], m2s[i], mgs[i]
                    transp(lambda kc: o_all[:, t, kc * 128:(kc + 1) * 128], o_all, oT, "act")
                    for nh in range(2):
                        for c in range(4):
                            S.op("pe", lambda e, nh=nh, c=c: e.matmul(pfd[:, nh, :], lhsT=oT[:, c, :], rhs=wpf[:, c, nh * 512:(nh + 1) * 512],
                                                                      start=(c == 0), stop=(c == 3)), reads=[oT, wpf], accum=[pfd])
                    for nh in range(2):
                        for c in range(4):
                            S.op("pe", lambda e, nh=nh, c=c: e.matmul(pfd[:, 2 + nh, :], lhsT=oT[:, 4 + c, :], rhs=wpd[:, c, nh * 512:(nh + 1) * 512],
                                                                      start=(c == 0), stop=(c == 3)), reads=[oT, wpd], accum=[pfd])
                    S.op("act", lambda e: e.activation(out=sg[:], in_=gls[t % 3][:], func=AF.Sigmoid), reads=[gls[t % 3]], writes=[sg])
                    S.op("dve", lambda e: e.tensor_tensor(out=m1[:], in0=pfd[:, 0:2, :].rearrange("p a n -> p (a n)"), in1=sg[:, 0:1024], op=ALU.mult),
                         reads=[pfd, sg], writes=[m1])
                    S.op("dve", lambda e: e.tensor_tensor(out=m2[:], in0=pfd[:, 2:4, :].rearrange("p a n -> p (a n)"), in1=sg[:, 1024:2048], op=ALU.mult),
                         reads=[pfd, sg], writes=[m2])
                    S.op("pool", lambda e: e.tensor_tensor(out=mg[:], in0=m1[:], in1=m2[:], op=ALU.add), reads=[m1, m2], writes=[mg])

                def stageB(t):
                    i = t % NBUF
                    tt = b * NT + t
                    mg, mT, tmp = mgs[i], mTs[i], tmps[i]
                    transp(lambda kc: mg[:, kc * 128:(kc + 1) * 128], mg, mT, "act")
                    for nh in range(2):
                        for c in range(KC):
                            S.op("pe", lambda e, nh=nh, c=c: e.matmul(pyy[:, nh, :], lhsT=mT[:, c, :], rhs=wo[:, c, nh * 512:(nh + 1) * 512],
                                                                      start=(c == 0), stop=(c == KC - 1)), reads=[mT, wo], accum=[pyy])
                    S.op("dve", lambda e: e.tensor_tensor(out=tmp[:], in0=pyy[:].rearrange("p a n -> p (a n)"), in1=gt1[:], op=ALU.mult),
                         reads=[pyy, gt1], writes=[tmp])
                    S.op("pool", lambda e: e.tensor_tensor(out=x1[i][:], in0=tmp[:], in1=xt[t % 3][:], op=ALU.add), reads=[tmp, xt[t % 3]], writes=[x1[i]])
                    S.dma("sp", "sx1_%d" % i, lambda e: e.dma_start(out=x1s[tt * 128:(tt + 1) * 128, :], in_=x1[i][:]), reads=[x1[i]])
                    S.op("act", lambda e: e.activation(out=sqj[:], in_=x1[i][:], func=AF.Square, accum_out=ss[:, 0:1]), reads=[x1[i]], writes=[sqj, ss])
                    S.op("act", lambda e: e.activation(out=ss[:, 1:2], in_=ss[:, 0:1], func=AF.Ln, scale=1.0 / D, bias=EPS), reads=[ss], accum=[ss])
                    S.op("act", lambda e: e.activation(out=ss[:, 2:3], in_=ss[:, 1:2], func=AF.Exp, scale=-0.5), reads=[ss], accum=[ss])
                    S.op("dve", lambda e: e.scalar_tensor_tensor(out=tmp[:], in0=x1[i][:], scalar=ss[:, 2:3], in1=A2[:], op0=ALU.mult, op1=ALU.mult),
                         reads=[x1[i], ss, A2], writes=[tmp])
                    S.op("pool", lambda e: e.tensor_tensor(out=h2[i][:], in0=tmp[:], in1=B2[:], op=ALU.add), reads=[tmp, B2], writes=[h2[i]])
                    S.dma("sp", "sh2_%d" % i, lambda e: e.dma_start(out=h2s[tt * 128:(tt + 1) * 128, :], in_=h2[i][:]), reads=[h2[i]])

                def stageC(t):
                    i = t % NBUF
                    tt = b * NT + t
                    transp(lambda kc: h2[i][:, kc * 128:(kc + 1) * 128], h2[i], h2T, "dve")
                    for c in range(KC):
                        S.op("pe", lambda e, c=c: e.matmul(prt[:, 0:36], lhsT=h2T[:, c, :], rhs=wr[:, c, :], start=(c == 0), stop=(c == KC - 1)),
                             reads=[h2T, wr], accum=[prt])
                    S.op("dve", lambda e: e.tensor_tensor(out=Lall[:, t, :], in0=prt[:, 0:36], in1=brt[:], op=ALU.add), reads=[prt, brt], accum=[Lall])

                load3(0)
                if NT > 1:
                    load3(1)
                stageA(0)
                for t in range(NT):
                    if t + 2 < NT:
                        load3(t + 2)
                    if t + 1 < NT:
                        stageA(t + 1)
                    if t >= 1:
                        stageC(t - 1)
                    stageB(t)
                stageC(NT - 1)
                TT_ = NT
                tt0 = b * NT
                BT = [m1s[0], m2s[0], tmps[0], xt[0], xt[1], xt[2], x1[0], x1[1]]

                def v32(tl):
                    return tl[:, 0:TT_ * 32].rearrange("p (t e) -> p t e", e=32)

                def v8(tl, k):
                    return tl[:, k * 256:k * 256 + TT_ * 8].rearrange("p (t e) -> p t e", e=8)

                def vec(k):
                    return rsm[:, k, :]

                def g4(k):
                    return r4[:, k, :].rearrange("p (t g) -> p t g", g=4)

                def bc(ap2, n):
                    return ap2.unsqueeze(2).to_broadcast([128, TT_, n])

                lg = Lall[:, :, 0:4]
                le4 = Lall[:, :, 4:36].rearrange("p t (g e) -> p t g e", e=8)
                T48, M1t, M2t, M12t, BASEt, RRt, TAt, SMt = BT
                mg4, zg, gw, m1v, m2v, dm, ex, w1_ = [vec(k) for k in range(8)]
                ohg, d4, eg = g4(0), g4(1), g4(2)
                e8, oh1, e8b, oh2 = v8(SMt, 0), v8(SMt, 1), v8(SMt, 2), v8(SMt, 3)
                R_ = [Lall, rsm, r4]

                def DV(fn, reads, writes, eng="dve"):
                    S.op(eng, fn, reads=reads, writes=writes)

                DV(lambda e: e.tensor_reduce(out=mg4, in_=lg, axis=AX.X, op=ALU.max), [Lall], [rsm])
                DV(lambda e: e.tensor_tensor(out=ohg, in0=lg, in1=bc(mg4, 4), op=ALU.is_equal), [Lall, rsm], [r4])
                DV(lambda e: e.tensor_tensor(out=d4, in0=lg, in1=bc(mg4, 4), op=ALU.subtract), [Lall, rsm, r4], [r4])
                DV(lambda e: e.activation(out=eg, in_=d4, func=AF.Exp), [r4], [r4], eng="act")
                DV(lambda e: e.tensor_reduce(out=zg, in_=eg, axis=AX.X, op=ALU.add), [r4, rsm], [rsm])
                DV(lambda e: e.reciprocal(out=gw, in_=zg), [rsm], [rsm])
                t48v = v32(T48).rearrange("p t (g e) -> p t g e", e=8)
                DV(lambda e: e.tensor_tensor(out=t48v, in0=le4, in1=ohg.unsqueeze(3).to_broadcast([128, TT_, 4, 8]), op=ALU.mult), [Lall, r4], [T48])
                DV(lambda e: e.tensor_reduce(out=e8, in_=t48v.rearrange("p t g e -> p t e g"), axis=AX.X, op=ALU.add), [T48], [SMt])
                DV(lambda e: e.tensor_reduce(out=m1v, in_=e8, axis=AX.X, op=ALU.max), [SMt, rsm], [rsm])
                DV(lambda e: e.tensor_tensor(out=oh1, in0=e8, in1=bc(m1v, 8), op=ALU.is_equal), [SMt, rsm], [SMt])
                DV(lambda e: e.scalar_tensor_tensor(out=e8b, in0=oh1, scalar=-1e30, in1=e8, op0=ALU.mult, op1=ALU.add), [SMt], [SMt])
                DV(lambda e: e.tensor_reduce(out=m2v, in_=e8b, axis=AX.X, op=ALU.max), [SMt, rsm], [rsm])
                DV(lambda e: e.tensor_tensor(out=oh2, in0=e8b, in1=bc(m2v, 8), op=ALU.is_equal), [SMt, rsm], [SMt])
                DV(lambda e: e.tensor_tensor(out=dm, in0=m2v, in1=m1v, op=ALU.subtract), [rsm], [rsm])
                DV(lambda e: e.activation(out=ex, in_=dm, func=AF.Exp), [rsm], [rsm], eng="act")
                DV(lambda e: e.tensor_scalar(out=ex, in0=ex, scalar1=1.0, scalar2=None, op0=ALU.add), [rsm], [rsm])
                DV(lambda e: e.reciprocal(out=w1_, in_=ex), [rsm], [rsm])
                S.op("dve", lambda e: e.tensor_tensor(out=wt_all[:, tt0:tt0 + TT_, 0], in0=w1_, in1=gw, op=ALU.mult), reads=[rsm], accum=[wt_all])
                S.op("dve", lambda e: e.tensor_tensor(out=wt_all[:, tt0:tt0 + TT_, 1], in0=gw, in1=wt_all[:, tt0:tt0 + TT_, 0], op=ALU.subtract),
                     reads=[rsm, wt_all], accum=[wt_all])
                M1v = v32(M1t).rearrange("p t (g e) -> p t g e", e=8)
                M2v = v32(M2t).rearrange("p t (g e) -> p t g e", e=8)
                DV(lambda e: e.tensor_tensor(out=M1v, in0=ohg.unsqueeze(3).to_broadcast([128, TT_, 4, 8]),
                                            in1=oh1.unsqueeze(2).to_broadcast([128, TT_, 4, 8]), op=ALU.mult), [r4, SMt], [M1t])
                DV(lambda e: e.tensor_tensor(out=M2v, in0=ohg.unsqueeze(3).to_broadcast([128, TT_, 4, 8]),
                                            in1=oh2.unsqueeze(2).to_broadcast([128, TT_, 4, 8]), op=ALU.mult), [r4, SMt], [M2t])
                DV(lambda e: e.tensor_tensor(out=v32(M12t), in0=v32(M1t), in1=v32(M2t), op=ALU.add), [M1t, M2t], [M12t])
                io_b = iota32[:].unsqueeze(1).to_broadcast([128, TT_, 32])
                for k, Mt in ((0, M1t), (1, M2t)):
                    DV(lambda e, Mt=Mt: e.tensor_tensor(out=v32(TAt), in0=v32(Mt), in1=io_b, op=ALU.mult), [Mt, iota32], [TAt])
                    S.op("dve", lambda e, k=k: e.tensor_reduce(out=eid_all[:, tt0:tt0 + TT_, k], in_=v32(TAt), axis=AX.X, op=ALU.add),
                         reads=[TAt], accum=[eid_all])
                NCOL = TT_ * 32
                pw = pfd[:, 0:2, :].rearrange("p a n -> p (a n)")
                pc_ = pfd[:, 2:4, :].rearrange("p a n -> p (a n)")
                for c0 in range(0, NCOL, 512):
                    c1 = min(c0 + 512, NCOL)
                    S.op("pe", lambda e, c0=c0, c1=c1: e.matmul(pw[:, c0:c1], lhsT=tri_s[:], rhs=M12t[:, c0:c1], start=True, stop=True),
                         reads=[tri_s, M12t], accum=[pfd])
                    S.op("pe", lambda e, c0=c0, c1=c1: e.matmul(pc_[:, c0:c1], lhsT=ones_f[:], rhs=M12t[:, c0:c1], start=True, stop=True),
                         reads=[ones_f, M12t], accum=[pfd])
                DV(lambda e: e.tensor_copy(out=v32(TAt), in_=pc_[:, 0:NCOL].rearrange("p (t e) -> p t e", e=32)), [pfd], [TAt])
                for t in range(TT_):
                    S.op("pool", lambda e, t=t: e.tensor_copy(out=BASEt[:, t * 32:(t + 1) * 32], in_=ecarry[:]), reads=[ecarry], accum=[BASEt])
                    S.op("pool", lambda e, t=t: e.tensor_tensor(out=ecarry[:], in0=ecarry[:], in1=TAt[:, t * 32:(t + 1) * 32], op=ALU.add),
                         reads=[ecarry, TAt, BASEt], writes=[ecarry])
                DV(lambda e: e.tensor_tensor(out=v32(RRt), in0=pw[:, 0:NCOL].rearrange("p (t e) -> p t e", e=32), in1=v32(BASEt), op=ALU.add),
                  [pfd, BASEt], [RRt])
                for k, Mt in ((0, M1t), (1, M2t)):
                    DV(lambda e, Mt=Mt: e.tensor_tensor(out=v32(TAt), in0=v32(RRt), in1=v32(Mt), op=ALU.mult), [RRt, Mt], [TAt])
                    S.op("dve", lambda e, k=k: e.tensor_reduce(out=rk_all[:, tt0:tt0 + TT_, k], in_=v32(TAt), axis=AX.X, op=ALU.add),
                         reads=[TAt], accum=[rk_all])
            S.barrier()

        def phase3b():
            S.bg_keys.clear()
            with contextlib.ExitStack() as st:
                padf = sb(st, "padf", [128, 32], F32)
                pend = sb(st, "pend", [128, 32], F32)
                pstart = sb(st, "pstart", [128, 32], F32)
                one32 = sb(st, "one32", [128, 32], F32)
                bst_i = sb(st, "bst_i", [128, NBLK], I32)
                bst_f = sb(st, "bst_f", [128, NBLK], F32)
                S.op("pool", lambda e: e.iota(bst_i[:], pattern=[[BS, NBLK]], base=0, channel_multiplier=0), writes=[bst_i])
                S.op("dve", lambda e: e.tensor_copy(out=bst_f[:], in_=bst_i[:]), reads=[bst_i], writes=[bst_f])
                cmpc = sb(st, "cmpc", [128, 32, NBLK], F32)
                S.op("dve", lambda e: e.tensor_tensor(out=cmpc[:], in0=ecarry[:].unsqueeze(2).to_broadcast([128, 32, NBLK]),
                                                      in1=bst_f[:].unsqueeze(1).to_broadcast([128, 32, NBLK]), op=ALU.is_gt),
                     reads=[ecarry, bst_f], writes=[cmpc])
                S.op("dve", lambda e: e.tensor_reduce(out=padf[:], in_=cmpc[:], axis=AX.X, op=ALU.add), reads=[cmpc], writes=[padf])
                S.op("dve", lambda e: e.tensor_scalar(out=padf[:], in0=padf[:], scalar1=float(BS), scalar2=None, op0=ALU.mult), reads=[padf], writes=[padf])
                S.op("pool", lambda e: e.memset(one32[:], 1.0), writes=[one32])
                S.op("dve", lambda e: e.tensor_tensor_scan(out=pend[:], data0=one32[:], data1=padf[:], initial=0.0, op0=ALU.mult, op1=ALU.add),
                     reads=[one32, padf], writes=[pend])
                S.op("dve", lambda e: e.tensor_tensor(out=pstart[:], in0=pend[:], in1=padf[:], op=ALU.subtract), reads=[pend, padf], writes=[pstart])
                big = sb(st, "big", [128, NTT, 32], F32)
                dsf = sb(st, "dsf", [128, NTT, 2], F32)
                for k in range(2):
                    S.op("dve", lambda e, k=k: e.tensor_tensor(out=big[:], in0=iota32[:].unsqueeze(1).to_broadcast([128, NTT, 32]),
                                                               in1=eid_all[:, :, k:k + 1].to_broadcast([128, NTT, 32]), op=ALU.is_equal),
                         reads=[iota32, eid_all], writes=[big])
                    S.op("dve", lambda e: e.tensor_tensor(out=big[:], in0=big[:], in1=pstart[:].unsqueeze(1).to_broadcast([128, NTT, 32]), op=ALU.mult),
                         reads=[big, pstart], writes=[big])
                    S.op("dve", lambda e, k=k: e.tensor_reduce(out=dsf[:, :, k:k + 1], in_=big[:], axis=AX.X, op=ALU.add), reads=[big], accum=[dsf])
                S.op("dve", lambda e: e.tensor_tensor(out=dsf[:], in0=dsf[:], in1=rk_all[:], op=ALU.add), reads=[dsf, rk_all], writes=[dsf])
                S.op("dve", lambda e: e.tensor_copy(out=dest_i[:], in_=dsf[:]), reads=[dsf], writes=[dest_i])
                cmpb = sb(st, "cmpb", [128, NBLK, 32], F32)
                S.op("dve", lambda e: e.tensor_tensor(out=cmpb[:], in0=pend[:].unsqueeze(1).to_broadcast([128, NBLK, 32]),
                                                      in1=bst_f[:].unsqueeze(2).to_broadcast([128, NBLK, 32]), op=ALU.is_le),
                     reads=[pend, bst_f], writes=[cmpb])
                BE = sb(st, "BE", [128, NBLK], F32)
                S.op("dve", lambda e: e.tensor_reduce(out=BE[:], in_=cmpb[:], axis=AX.X, op=ALU.add), reads=[cmpb], writes=[BE])
                S.op("dve", lambda e: e.tensor_scalar(out=BE[:], in0=BE[:], scalar1=float(NEXP - 1), scalar2=None, op0=ALU.min), reads=[BE], writes=[BE])
                bpc_i = sb(st, "bpc_i", [128, 1], I32)
                bpc = sb(st, "bpc", [128, 1], F32)
                S.op("pool", lambda e: e.iota(bpc_i[:], pattern=[[0, 1]], base=0, channel_multiplier=2), writes=[bpc_i])
                S.op("dve", lambda e: e.tensor_copy(out=bpc[:], in_=bpc_i[:]), reads=[bpc_i], writes=[bpc])
                idf = sb(st, "idf", [128, NBLK, 2], F32)
                idx1 = blkidx["idx1"]
                S.op("dve", lambda e: e.tensor_scalar(out=idf[:, :, 0], in0=BE[:], scalar1=256.0, scalar2=bpc[:, 0:1], op0=ALU.mult, op1=ALU.add),
                     reads=[BE, bpc], writes=[idf])
                S.op("dve", lambda e: e.tensor_scalar(out=idf[:, :, 1], in0=idf[:, :, 0], scalar1=1.0, scalar2=None, op0=ALU.add),
                     reads=[idf], accum=[idf])
                S.op("dve", lambda e: e.tensor_copy(out=idx1[:], in_=idf[:]), reads=[idf], writes=[idx1])
                if dbg:
                    S.dma("sp", "dbgbe", lambda e: e.dma_start(out=dbg_t["be"][:, 0:NBLK], in_=BE[:]), reads=[BE])
                    S.dma("sp", "dbgbe", lambda e: e.dma_start(out=dbg_t["be"][:, NBLK:NBLK + 32], in_=pend[:]), reads=[pend])
                    S.dma("sp", "dbgbe", lambda e: e.dma_start(out=dbg_t["be"][:, NBLK + 32:NBLK + 64], in_=ecarry[:]), reads=[ecarry])
                    S.dma("sp", "dbgrt", lambda e: e.dma_start(out=dbg_t["rt"][:, :, 0:2], in_=eid_all[:]), reads=[eid_all])
                    S.dma("sp", "dbgrt", lambda e: e.dma_start(out=dbg_t["rt"][:, :, 2:4], in_=wt_all[:]), reads=[wt_all])
                    S.dma("sp", "dbgrt", lambda e: e.dma_start(out=dbg_t["rt"][:, :, 4:6], in_=dsf[:]), reads=[dsf])
                    S.dma("sp", "dbgrt", lambda e: e.dma_start(out=dbg_t["rt"][:, :, 6:8], in_=rk_all[:]), reads=[rk_all])
                hb_ = [sb(st, "h2l%d" % i, [128, D], BF16) for i in range(2)]
                for tt in range(NTT):
                    i = tt % 2
                    S.dma("sp", "h2l%d" % i, lambda e: e.dma_start(out=hb_[i][:], in_=h2s[tt * 128:(tt + 1) * 128, :]), writes=[hb_[i]])
                    for k in range(2):
                        S.dma("pool", "h2sc%d" % i, lambda e, k=k: e.indirect_dma_start(
                            out=xbuf[:, :], out_offset=bass.IndirectOffsetOnAxis(ap=dest_i[:, tt, k:k + 1], axis=0),
                            in_=hb_[i][:], in_offset=None), reads=[hb_[i], dest_i, xz])
            S.barrier()

        def phase4():
            idx1 = blkidx["idx1"]
            bg_cast(len(castjobs))
            w1v, w3v, w2v = w1c, w3c, w2c
            SUB = BS // 128
            with contextlib.ExitStack() as st:
                w1b = [sb(st, "w1b%d" % i, [128, 8, DEXP], BF16) for i in range(3)]
                w3b = [sb(st, "w3b%d" % i, [128, 8, DEXP], BF16) for i in range(3)]
                w2b = [sb(st, "w2b%d" % i, [128, 4, D], BF16) for i in range(4)]
                xb = [sb(st, "xb%d" % i, [128, SUB, D], BF16) for i in range(3)]
                xTs = [sb(st, "xT%d" % i, [128, KC, BS], BF16) for i in range(2)]
                sact = [sb(st, "sact%d" % i, [128, BS], F32) for i in range(2)]
                gTs = [sb(st, "gT%d" % i, [128, 4, BS], BF16) for i in range(2)]
                yo = [sb(st, "yo%d" % i, [128, SUB, D], BF16) for i in range(2)]
                pX = ps(st, "pX", [128, KC, BS], BF16)
                ph = ps(st, "ph", [128, 4, 512], F32)
                py = ps(st, "py", [128, 2, 512], F32)

                def load_blk(bi):
                    i = bi % 3
                    for hf in range(2):
                        off = bass.IndirectOffsetOnAxis(ap=idx1[:, bi, hf:hf + 1], axis=0)
                        S.dma("pool", "w1b%d" % i, lambda e, hf=hf: e.indirect_dma_start(
                            out=w1b[i][:, hf * 4:(hf + 1) * 4, :].rearrange("p c n -> p (c n)"), out_offset=None,
                            in_=w1v[:, :], in_offset=off), reads=[idx1, wcres], accum=[w1b[i]])
                        S.dma("pool", "w3b%d" % i, lambda e, hf=hf: e.indirect_dma_start(
                            out=w3b[i][:, hf * 4:(hf + 1) * 4, :].rearrange("p c n -> p (c n)"), out_offset=None,
                            in_=w3v[:, :], in_offset=off), reads=[idx1, wcres], accum=[w3b[i]])
                        S.dma("pool", "w2b%d" % (bi % 4), lambda e, hf=hf: e.indirect_dma_start(
                            out=w2b[bi % 4][:, hf * 2:(hf + 1) * 2, :].rearrange("p c n -> p (c n)"), out_offset=None,
                            in_=w2v[:, :], in_offset=off), reads=[idx1, wcres], accum=[w2b[bi % 4]])
                    S.dma("sp", "xb%d" % i, lambda e: e.dma_start(out=xb[i][:], in_=xbuf[bi * BS:(bi + 1) * BS, :].rearrange("(s p) d -> p s d", p=128)),
                          writes=[xb[i]])

                def stX(bi):
                    i = bi % 2
                    xT = xTs[i]
                    for s_ in range(SUB):
                        for c in range(KC):
                            S.op("pe", lambda e, s_=s_, c=c: e.transpose(out=pX[:, c, s_ * 128:(s_ + 1) * 128], in_=xb[bi % 3][:, s_, :].rearrange("p (q c) -> p c q", c=8)[:, c, :],
                                                                         identity=ident_b[:]), reads=[xb[bi % 3], ident_b], accum=[pX])
                    S.op("act", lambda e: e.copy(out=xT[:], in_=pX[:]), reads=[pX], writes=[xT])

                def stH(bi):
                    i = bi % 2
                    xT, gT = xTs[i], gTs[i]
                    for fc in range(4):
                        for c in range(KC):
                            S.op("pe", lambda e, fc=fc, c=c: e.matmul(ph[:, fc, 0:BS], lhsT=w1b[bi % 3][:, c, :].rearrange("p (q f) -> p f q", f=4)[:, fc, :], rhs=xT[:, c, :],
                                                                      start=(c == 0), stop=(c == KC - 1), skip_group_check=True), reads=[w1b[bi % 3], xT], accum=[ph])
                        for c in range(KC):
                            S.op("pe", lambda e, fc=fc, c=c: e.matmul(ph[:, fc, 256:256 + BS], lhsT=w3b[bi % 3][:, c, :].rearrange("p (q f) -> p f q", f=4)[:, fc, :], rhs=xT[:, c, :],
                                                                      start=(c == 0), stop=(c == KC - 1), skip_group_check=True), reads=[w3b[bi % 3], xT], accum=[ph])
                        sa = sact[fc % 2]
                        S.op("act", lambda e, fc=fc, sa=sa: e.activation(out=sa[:], in_=ph[:, fc, 0:BS], func=AF.Silu), reads=[ph], writes=[sa])
                        S.op("dve", lambda e, fc=fc, sa=sa: e.tensor_tensor(out=gT[:, fc, :], in0=sa[:], in1=ph[:, fc, 256:256 + BS], op=ALU.mult),
                             reads=[sa, ph], accum=[gT])

                def stY(bi):
                    i = bi % 2
                    gT = gTs[i]
                    w2t = w2b[bi % 4]
                    for s_ in range(SUB):
                        for nh in range(2):
                            for fc in range(4):
                                S.op("pe", lambda e, s_=s_, nh=nh, fc=fc: e.matmul(py[:, nh, :], lhsT=gT[:, fc, s_ * 128:(s_ + 1) * 128],
                                                                                   rhs=w2t[:, fc, nh * 512:(nh + 1) * 512],
                                                                                   start=(fc == 0), stop=(fc == 3)), reads=[gT, w2t], accum=[py])
                        if s_ % 2 == 0:
                            S.op("act", lambda e, s_=s_: e.copy(out=yo[i][:, s_, :], in_=py[:].rearrange("p a n -> p (a n)")), reads=[py], accum=[yo[i]])
                        else:
                            S.op("dve", lambda e, s_=s_: e.tensor_copy(out=yo[i][:, s_, :], in_=py[:].rearrange("p a n -> p (a n)")), reads=[py], accum=[yo[i]])
                    S.dma("sp", "yo%d" % i, lambda e: e.dma_start(out=ybuf[bi * BS:(bi + 1) * BS, :].rearrange("(s p) d -> p s d", p=128), in_=yo[i][:]),
                          reads=[yo[i]])

                load_blk(0)
                if NBLK > 1:
                    load_blk(1)
                stX(0)
                for bi in range(NBLK):
                    if bi + 2 < NBLK:
                        load_blk(bi + 2)
                    stH(bi)
                    if bi + 1 < NBLK:
                        stX(bi + 1)
                    if bi >= 1:
                        stY(bi - 1)
                stY(NBLK - 1)
            S.barrier()

        def phase5():
            with contextlib.ExitStack() as st:
                gt2 = [load_mod_row(st, "gt2_%d" % bb, bb, 5) for bb in range(NB)]
                ya = [sb(st, "ya%d" % i, [128, D], BF16) for i in range(2)]
                yb = [sb(st, "yb%d" % i, [128, D], BF16) for i in range(2)]
                xl = [sb(st, "xl%d" % i, [128, D], F32) for i in range(2)]
                ma = [sb(st, "ma%d" % i, [128, D], F32) for i in range(2)]
                mb = [sb(st, "mb%d" % i, [128, D], F32) for i in range(2)]
                oo_ = [sb(st, "oo5_%d" % i, [128, D], F32) for i in range(2)]

                def load5(tt):
                    i = tt % 2
                    S.dma("pool", "ya%d" % i, lambda e: e.indirect_dma_start(
                        out=ya[i][:], out_offset=None, in_=ybuf[:, :], in_offset=bass.IndirectOffsetOnAxis(ap=dest_i[:, tt, 0:1], axis=0)),
                        reads=[dest_i], writes=[ya[i]])
                    S.dma("pool", "yb%d" % i, lambda e: e.indirect_dma_start(
                        out=yb[i][:], out_offset=None, in_=ybuf[:, :], in_offset=bass.IndirectOffsetOnAxis(ap=dest_i[:, tt, 1:2], axis=0)),
                        reads=[dest_i], writes=[yb[i]])
                    S.dma("sp", "xl%d" % i, lambda e: e.dma_start(out=xl[i][:], in_=x1s[tt * 128:(tt + 1) * 128, :]), writes=[xl[i]])

                load5(0)
                for tt in range(NTT):
                    i = tt % 2
                    bb = tt // NT
                    t = tt % NT
                    if tt + 1 < NTT:
                        load5(tt + 1)
                    S.op("dve", lambda e: e.tensor_scalar(out=ma[i][:], in0=ya[i][:], scalar1=wt_all[:, tt, 0:1], scalar2=None, op0=ALU.mult),
                         reads=[ya[i], wt_all], writes=[ma[i]])
                    S.op("dve", lambda e: e.scalar_tensor_tensor(out=mb[i][:], in0=yb[i][:], scalar=wt_all[:, tt, 1:2], in1=ma[i][:],
                                                                 op0=ALU.mult, op1=ALU.add), reads=[yb[i], wt_all, ma[i]], writes=[mb[i]])
                    S.op("dve", lambda e: e.tensor_tensor(out=ma[i][:], in0=mb[i][:], in1=gt2[bb][:], op=ALU.mult), reads=[mb[i], gt2[bb]], writes=[ma[i]])
                    S.op("pool", lambda e: e.tensor_tensor(out=oo_[i][:], in0=ma[i][:], in1=xl[i][:], op=ALU.add), reads=[ma[i], xl[i]], writes=[oo_[i]])
                    S.dma("sp", "oo5_%d" % i, lambda e: e.dma_start(out=out[bb, t * 128:(t + 1) * 128, :], in_=oo_[i][:]), reads=[oo_[i]])
            S.barrier()

        for b in range(NB):
            phase1(b)
            if upto >= 2:
                with contextlib.ExitStack() as bst:
                    o_all = sb(bst, "o_all", [128, NT, D], BF16)
                    phase2(b, o_all)
                    if upto >= 3:
                        phase3(b, o_all)
        if upto >= 4:
            phase3b()
            phase4()
        if upto >= 5:
            phase5()
        S.barrier()
        S.final_wait()
    print("instructions:", S.n_ins, "dma sems:", len(S.dsem), "eng sems:", len(S.all_sems))
    return nc


_INVF = (10000.0 ** (-np.arange(0, 64, 2, dtype=np.float32) / np.float32(64))).astype(np.float32).reshape(1, 32)


def make_in_map(inp, core, NB):
    sl = slice(core * NB, (core + 1) * NB)
    f = lambda a: np.ascontiguousarray(a)
    m = {
        "x": f(inp["x"][sl]), "c": f(inp["c"][sl]), "positions": f(inp["positions"][sl]).astype(np.int32),
        "w_ada": f(inp["w_ada"][0]), "b_ada": f(inp["b_ada"][0:1]), "g_norm1": f(inp["g_norm1"][0:1]),
        "w_in": f(inp["w_in"][0]), "b_f": f(inp["b_f"][0:1]),
        "g_q_fox": f(inp["g_q_fox"][0:1]), "g_k_fox": f(inp["g_k_fox"][0:1]),
        "g_q_diff": f(inp["g_q_diff"][0:1]), "g_k_diff": f(inp["g_k_diff"][0:1]),
        "lam_q1": f(inp["lam_q1"][0:1]), "lam_k1": f(inp["lam_k1"][0:1]),
        "lam_q2": f(inp["lam_q2"][0:1]), "lam_k2": f(inp["lam_k2"][0:1]),
        "g_subln": f(inp["g_subln"][0:1]),
        "w_proj_fox": f(inp["w_proj_fox"][0]), "w_proj_diff": f(inp["w_proj_diff"][0]), "w_out": f(inp["w_out"][0]),
        "g_norm2": f(inp["g_norm2"][0:1]),
        "w_router_group": f(inp["w_router_group"][0]), "b_router_group": f(inp["b_router_group"][0:1]),
        "w_router_expert": f(inp["w_router_expert"][0]), "b_router_expert": f(inp["b_router_expert"][0:1]),
        "w1": f(inp["w1"][0]).reshape(NEXP * D, DEXP), "w3": f(inp["w3"][0]).reshape(NEXP * D, DEXP),
        "w2": f(inp["w2"][0]).reshape(NEXP * DEXP, D),
        "invf": _INVF,
    }
    return m


def kernel(**inputs):
    inp = {k: np.asarray(v) for k, v in inputs.items()}
    B, TT, _ = inp["x"].shape
    NB = B // N_CORES
    nc = build_nc(NB, TT)
    in_maps = [make_in_map(inp, c, NB) for c in range(N_CORES)]
    res = run_bass_kernel_spmd(nc, in_maps, core_ids=list(range(N_CORES)))
    return np.concatenate([np.asarray(r["out"]) for r in res.results], axis=0).astype(np.float32)
```

```python
import contextlib
import math
import numpy as np
import concourse.bass as bass
import concourse.mybir as mybir
from concourse.bass_utils import run_bass_kernel_spmd

F32 = mybir.dt.float32
BF16 = mybir.dt.bfloat16
I32 = mybir.dt.int32
U32 = mybir.dt.uint32
AF = mybir.ActivationFunctionType
ALU = mybir.AluOpType
AX = mybir.AxisListType

D = 1024
KC = 8
HD = 64
IN_COLS = 5128
C_FQ, C_FK, C_FV, C_FF, C_DQ, C_DK, C_DV, C_G = 0, 512, 1024, 1536, 1544, 2056, 2568, 3080
NEXP = 32
DEXP = 512
EPS = 1e-6
BS = 256
LAM0 = 0.8 - 0.6 * math.exp(-0.3 * 0)
N_CORES = 8


class Res:
    __slots__ = ("name", "w", "r")

    def __init__(self, name):
        self.name = name
        self.w = {}
        self.r = {}


class T:
    def __init__(self, t, name):
        self.t = t
        self.res = Res(name)

    def __getitem__(self, k):
        return self.t[k]


class Sched:
    EPOCH = 30000

    def __init__(self, nc, es):
        self.nc = nc
        self.es = es
        self.eng = {"pe": nc.tensor, "act": nc.scalar, "dve": nc.vector, "pool": nc.gpsimd, "sp": nc.sync}
        self.sem = {}
        self.cnt = {}
        self.seen = {k: {} for k in self.eng}
        self.all_sems = {}
        for k in self.eng:
            self._new_eng_sem(k)
        self.dsem = {}
        self.dcnt = {}
        self.n_ins = 0
        self.bg_keys = set()

    def _new_eng_sem(self, k):
        s = self.es.enter_context(self.nc.semaphore("s_%s_%d" % (k, len(self.all_sems))))
        self.sem[k] = s
        self.cnt[k] = 0
        self.all_sems[s] = 0

    def _res(self, lst):
        return [x.res if isinstance(x, T) else x for x in lst]

    def _need(self, reads, writes, accum):
        need = {}
        for r in reads:
            for s, v in r.w.items():
                if need.get(s, 0) < v:
                    need[s] = v
        for w in writes:
            for dct in (w.w, w.r):
                for s, v in dct.items():
                    if need.get(s, 0) < v:
                        need[s] = v
        for w in accum:
            for s, v in w.r.items():
                if need.get(s, 0) < v:
                    need[s] = v
        return need

    def _emit_waits(self, e, need):
        eng = self.eng[e]
        seen = self.seen[e]
        waits = [(s, v) for s, v in need.items() if seen.get(s, 0) < v]
        for s, v in waits:
            seen[s] = v
        return eng, waits

    def _record(self, ev_s, ev_v, reads, writes, accum):
        self.all_sems[ev_s] = ev_v
        for r in reads:
            if r.r.get(ev_s, 0) < ev_v:
                r.r[ev_s] = ev_v
        for w in writes:
            w.w = {ev_s: ev_v}
            w.r = {}
        for w in accum:
            if w.w.get(ev_s, 0) < ev_v:
                w.w[ev_s] = ev_v

    def op(self, e, fn, reads=(), writes=(), accum=()):
        reads, writes, accum = self._res(reads), self._res(writes), self._res(accum)
        if self.cnt[e] >= self.EPOCH:
            self._new_eng_sem(e)
        need = self._need(reads, writes, accum)
        eng, waits = self._emit_waits(e, need)
        for s, v in waits:
            eng.wait_ge(s, v)
        ins = fn(eng)
        self.cnt[e] += 1
        ins.then_inc(self.sem[e], 1)
        self._record(self.sem[e], self.cnt[e], reads, writes, accum)
        self.n_ins += 1
        return ins

    def dma(self, q, key, fn, reads=(), writes=(), accum=()):
        reads, writes, accum = self._res(reads), self._res(writes), self._res(accum)
        if key not in self.dsem:
            self.dsem[key] = self.es.enter_context(self.nc.semaphore("d_" + key))
            self.dcnt[key] = 0
        need = self._need(reads, writes, accum)
        eng, waits = self._emit_waits(q, need)
        for s, v in waits:
            eng.wait_ge(s, v)
        ins = fn(eng)
        self.dcnt[key] += 16
        s = self.dsem[key]
        ins.then_inc(s, 16)
        self._record(s, self.dcnt[key], reads, writes, accum)
        self.n_ins += 1
        return ins

    def barrier(self):
        skip = {self.dsem[k] for k in self.bg_keys if k in self.dsem}
        allv = {s: v for s, v in self.all_sems.items() if s not in skip}
        for e in self.eng:
            eng, waits = self._emit_waits(e, allv)
            for s, v in waits:
                if v > 0:
                    eng.wait_ge(s, v)

    def final_wait(self):
        eng, waits = self._emit_waits("sp", dict(self.all_sems))
        for s, v in waits:
            if v > 0:
                eng.wait_ge(s, v)


def build_nc(NB, TT, dbg=False, upto=5):
    NT = TT // 128
    NG = TT // 512
    NTOK = NB * TT
    NTT = NTOK // 128
    NBLK = (NTOK * 2) // BS + NEXP
    PROWS = NBLK * BS
    nc = bass.Bass("TRN2", target_bir_lowering=False)

    def din(name, shape, dt=F32):
        return nc.dram_tensor(name, list(shape), dt, kind="ExternalInput").ap()

    DBG_OUT = ("modv", "qTd", "kTd", "qTf", "x1s", "h2s")

    def dscr(name, shape, dt):
        return nc.dram_tensor(name, list(shape), dt, kind=("ExternalOutput" if (dbg and name in DBG_OUT) else "Internal")).ap()

    x = din("x", [NB, TT, D])
    cin = din("c", [NB, D])
    pos = din("positions", [NB, TT], I32)
    w_ada = din("w_ada", [D, 6 * D])
    b_ada = din("b_ada", [1, 6 * D])
    g_norm1 = din("g_norm1", [1, D])
    w_in = din("w_in", [D, IN_COLS])
    b_f = din("b_f", [1, 8])
    g_q_fox = din("g_q_fox", [1, HD])
    g_k_fox = din("g_k_fox", [1, HD])
    g_q_diff = din("g_q_diff", [1, HD])
    g_k_diff = din("g_k_diff", [1, HD])
    lam_q1 = din("lam_q1", [1, HD])
    lam_k1 = din("lam_k1", [1, HD])
    lam_q2 = din("lam_q2", [1, HD])
    lam_k2 = din("lam_k2", [1, HD])
    g_subln = din("g_subln", [1, 128])
    w_pf = din("w_proj_fox", [512, D])
    w_pd = din("w_proj_diff", [512, D])
    w_out = din("w_out", [D, D])
    g_norm2 = din("g_norm2", [1, D])
    w_rg = din("w_router_group", [D, 4])
    b_rg = din("b_router_group", [1, 4])
    w_re = din("w_router_expert", [D, 32])
    b_re = din("b_router_expert", [1, 32])
    w1 = din("w1", [NEXP * D, DEXP])
    w3 = din("w3", [NEXP * D, DEXP])
    w2 = din("w2", [NEXP * DEXP, D])
    invf = din("invf", [1, 32])
    out = nc.dram_tensor("out", [NB, TT, D], F32, kind="ExternalOutput").ap()

    modv = dscr("modv", [NB, 6, D], F32)
    qTf = dscr("qTf", [NB, 8, 65, TT], BF16)
    kTf = dscr("kTf", [NB, 8, 65, TT], BF16)
    qTd = dscr("qTd", [NB, 8, 64, TT], BF16)
    kTd = dscr("kTd", [NB, 8, 64, TT], BF16)
    vF = dscr("vF", [NB, TT, 8 * 65], BF16)
    vD = dscr("vD", [NB, TT, 4 * 129], BF16)
    gl = dscr("gl", [NB, TT, 2048], BF16)
    x1s = dscr("x1s", [NTOK, D], F32)
    h2s = dscr("h2s", [NTOK, D], BF16)
    w1c = dscr("w1c", [NEXP * D // 4, 2048], BF16)
    w3c = dscr("w3c", [NEXP * D // 4, 2048], BF16)
    w2c = dscr("w2c", [NEXP * DEXP // 2, 2048], BF16)
    xbuf = dscr("xbuf", [PROWS, D], BF16)
    ybuf = dscr("ybuf", [PROWS, D], BF16)
    dbg_t = {}
    if dbg:
        dbg_t["oc"] = nc.dram_tensor("dbg_oc", [NB, TT, D], BF16, kind="ExternalOutput").ap()
        dbg_t["rt"] = nc.dram_tensor("dbg_rt", [128, NTT, 8], F32, kind="ExternalOutput").ap()
        dbg_t["be"] = nc.dram_tensor("dbg_be", [128, NBLK + 64], F32, kind="ExternalOutput").ap()

    es = contextlib.ExitStack()
    with es:
        S = Sched(nc, es)

        uid = [0]

        def sb(stack, name, shape, dt):
            uid[0] += 1
            name = "%s_%d" % (name, uid[0])
            return T(stack.enter_context(nc.sbuf_tensor(name, list(shape), dt)), name)

        def ps(stack, name, shape, dt):
            uid[0] += 1
            name = "%s_%d" % (name, uid[0])
            return T(stack.enter_context(nc.psum_tensor(name, list(shape), dt)), name)

        ident_b = sb(es, "ident_b", [128, 128], BF16)
        ident_f = sb(es, "ident_f", [128, 128], F32)
        tri_i = sb(es, "tri_i", [128, 128], F32)
        tri_s = sb(es, "tri_s", [128, 128], F32)
        ones_f = sb(es, "ones_f", [128, 128], F32)
        G4 = sb(es, "G4", [128, 4, HD], F32)
        bf_rep = sb(es, "bf_rep", [128, 8], F32)
        gsub = sb(es, "gsub", [128, 128], F32)
        nlam = sb(es, "nlam", [128, 1], F32)
        invf_rep = sb(es, "invf_rep", [128, 32], F32)
        ncum = sb(es, "ncum", [128, NB, NT, 8], F32)
        eid_all = sb(es, "eid_all", [128, NTT, 2], F32)
        rk_all = sb(es, "rk_all", [128, NTT, 2], F32)
        wt_all = sb(es, "wt_all", [128, NTT, 2], F32)
        dest_i = sb(es, "dest_i", [128, NTT, 2], U32)
        ecarry = sb(es, "ecarry", [128, 32], F32)
        iota32 = sb(es, "iota32", [128, 32], F32)
        ztile = sb(es, "ztile", [128, D], BF16)
        xz = Res("xbuf_zero")
        S.bg_keys.add("ztile")
        blkidx = {"idx1": sb(es, "idx1", [128, NBLK, 2], U32)}

        def bcast_rows(ap, n):
            return ap.partition_broadcast(n)

        def setup():
            with contextlib.ExitStack() as st:
                zf = sb(st, "zf", [128, 128], F32)
                S.op("pool", lambda e: e.memset(zf[:], 0.0), writes=[zf])
                S.op("pool", lambda e: e.memset(ones_f[:], 1.0), writes=[ones_f])
                S.op("pool", lambda e: e.affine_select(out=ident_f[:], in_=zf[:], pattern=[[-1, 128]],
                                                       compare_op=ALU.not_equal, fill=1.0, base=0,
                                                       channel_multiplier=1), reads=[zf], writes=[ident_f])
                S.op("pool", lambda e: e.affine_select(out=tri_i[:], in_=ones_f[:], pattern=[[1, 128]],
                                                       compare_op=ALU.is_ge, fill=0.0, base=0,
                                                       channel_multiplier=-1), reads=[ones_f], writes=[tri_i])
                S.op("pool", lambda e: e.affine_select(out=tri_s[:], in_=ones_f[:], pattern=[[1, 128]],
                                                       compare_op=ALU.is_ge, fill=0.0, base=-1,
                                                       channel_multiplier=-1), reads=[ones_f], writes=[tri_s])
                S.op("dve", lambda e: e.tensor_copy(out=ident_b[:], in_=ident_f[:]), reads=[ident_f], writes=[ident_b])
                S.op("pool", lambda e: e.memset(ecarry[:], 0.0), writes=[ecarry])
                S.op("pool", lambda e: e.memset(ztile[:], 0.0), writes=[ztile])
                xbv = xbuf.rearrange("(r p) d -> p r d", p=128)
                for r0 in range(PROWS // 128):
                    S.dma("sp", "ztile", lambda e, r0=r0: e.dma_start(out=xbv[:, r0, :], in_=ztile[:]), reads=[ztile], accum=[xz])
                io_i = sb(st, "io_i", [128, 32], I32)
                S.op("pool", lambda e: e.iota(io_i[:], pattern=[[1, 32]], base=0, channel_multiplier=0), writes=[io_i])
                S.op("dve", lambda e: e.tensor_copy(out=iota32[:], in_=io_i[:]), reads=[io_i], writes=[iota32])
                for i, (g, sc) in enumerate([(g_q_fox, 0.125), (g_k_fox, 1.0), (g_q_diff, 0.125), (g_k_diff, 1.0)]):
                    S.dma("sp", "G4", lambda e, g=g, i=i: e.dma_start(out=G4[:, i, :], in_=bcast_rows(g[0:1, :], 128)),
                          accum=[G4])
                S.dma("sp", "bf", lambda e: e.dma_start(out=bf_rep[:], in_=bcast_rows(b_f[0:1, :], 128)), writes=[bf_rep])
                S.dma("sp", "gsub", lambda e: e.dma_start(out=gsub[:], in_=bcast_rows(g_subln[0:1, :], 128)), writes=[gsub])
                S.dma("sp", "invf", lambda e: e.dma_start(out=invf_rep[:], in_=bcast_rows(invf[0:1, :], 128)), writes=[invf_rep])
                S.op("dve", lambda e: e.tensor_scalar(out=G4[:, 0, :], in0=G4[:, 0, :], scalar1=0.125, scalar2=None,
                                                      op0=ALU.mult), reads=[G4], writes=[G4])
                S.op("dve", lambda e: e.tensor_scalar(out=G4[:, 2, :], in0=G4[:, 2, :], scalar1=0.125, scalar2=None,
                                                      op0=ALU.mult), reads=[G4], writes=[G4])
                S.op("dve", lambda e: e.tensor_scalar(out=gsub[:], in0=gsub[:], scalar1=1.0 - LAM0, scalar2=None,
                                                      op0=ALU.mult), reads=[gsub], writes=[gsub])
                lv = sb(st, "lv", [128, 4, HD], F32)
                for i, g in enumerate([lam_q1, lam_k1, lam_q2, lam_k2]):
                    S.dma("sp", "lv", lambda e, g=g, i=i: e.dma_start(out=lv[:, i, :], in_=bcast_rows(g[0:1, :], 128)),
                          accum=[lv])
                lp = sb(st, "lp", [128, 2, HD], F32)
                ls = sb(st, "ls", [128, 2], F32)
                S.op("dve", lambda e: e.tensor_tensor(out=lp[:, 0, :], in0=lv[:, 0, :], in1=lv[:, 1, :], op=ALU.mult),
                     reads=[lv], writes=[lp])
                S.op("dve", lambda e: e.tensor_tensor(out=lp[:, 1, :], in0=lv[:, 2, :], in1=lv[:, 3, :], op=ALU.mult),
                     reads=[lv, lp], accum=[lp])
                S.op("dve", lambda e: e.tensor_reduce(out=ls[:], in_=lp[:], axis=AX.X, op=ALU.add), reads=[lp], writes=[ls])
                S.op("act", lambda e: e.activation(out=ls[:], in_=ls[:], func=AF.Exp), reads=[ls], writes=[ls])
                S.op("dve", lambda e: e.scalar_tensor_tensor(out=nlam[:], in0=ls[:, 1:2], scalar=-LAM0, in1=ls[:, 0:1],
                                                             op0=ALU.add, op1=ALU.subtract), reads=[ls], writes=[nlam])
                cc = sb(st, "cc", [128, 8, NB], F32)
                with nc.allow_non_contiguous_dma(reason="tiny one-time transposed load of c"):
                    for bb in range(NB):
                        S.dma("sp", "cc", lambda e, bb=bb: e.dma_start(out=cc[:, :, bb:bb + 1],
                                                                  in_=cin[bb:bb + 1, :].rearrange("b (kc k) -> k kc b", k=128)),
                              accum=[cc])
                with contextlib.ExitStack() as pst:
                    pc = cc
                    pm = ps(pst, "pm", [128, 512], F32)
                    cact = sb(st, "cact", [128, 8, NB], F32)
                    S.op("act", lambda e: e.activation(out=cact[:], in_=pc[:], func=AF.Silu), reads=[pc], writes=[cact])
                    bad = sb(st, "bad", [128, 6 * D], F32)
                    S.dma("sp", "bad", lambda e: e.dma_start(out=bad[:], in_=bcast_rows(b_ada[0:1, :], 128)), writes=[bad])
                    gg = sb(st, "gg", [128, 2, D], F32)
                    S.dma("sp", "gg", lambda e: e.dma_start(out=gg[:, 0, :], in_=bcast_rows(g_norm1[0:1, :], 128)), accum=[gg])
                    S.dma("sp", "gg", lambda e: e.dma_start(out=gg[:, 1, :], in_=bcast_rows(g_norm2[0:1, :], 128)), accum=[gg])
                    was = [sb(st, "wa%d" % i, [128, 8, 512], BF16) for i in range(2)]
                    creps = [sb(st, "crep%d" % i, [128, 8, 128], BF16) for i in range(NB)]
                    mods = [sb(st, "mod%d" % i, [128, 6 * D], F32) for i in range(NB)]
                    for bb in range(NB):
                        S.op("dve", lambda e, bb=bb: e.tensor_copy(out=creps[bb][:], in_=cact[:, :, bb:bb + 1].to_broadcast([128, 8, 128])),
                             reads=[cact], writes=[creps[bb]])
                    for j in range(12):
                        wa = was[j % 2]
                        S.dma("pool", "wa%d" % (j % 2),
                              lambda e, wa=wa, j=j: e.dma_start(
                                  out=wa[:], in_=w_ada[:, j * 512:(j + 1) * 512].rearrange("(kc k) n -> k kc n", k=128)),
                              writes=[wa])
                        for bb in range(NB):
                            for kc in range(KC):
                                S.op("pe", lambda e, wa=wa, kc=kc, bb=bb: e.matmul(pm[:], lhsT=creps[bb][:, kc, :], rhs=wa[:, kc, :],
                                                                                   start=(kc == 0), stop=(kc == KC - 1)),
                                     reads=[creps[bb], wa], accum=[pm])
                            S.op("dve", lambda e, j=j, bb=bb: e.tensor_tensor(out=mods[bb][:, j * 512:(j + 1) * 512], in0=pm[:],
                                                                              in1=bad[:, j * 512:(j + 1) * 512], op=ALU.add),
                                 reads=[pm, bad], accum=[mods[bb]])
                    for bb in range(NB):
                        mod = mods[bb]
                        for (dst, src, kind) in [(0, 1, "A1"), (1, 0, "c"), (2, 2, "c"), (3, 4, "A2"), (4, 3, "c"), (5, 5, "c")]:
                            if kind == "c":
                                S.dma("sp", "mod%d" % bb, lambda e, dst=dst, src=src, bb=bb, mod=mod: e.dma_start(
                                    out=modv[bb, dst:dst + 1, :], in_=mod[0:1, src * D:(src + 1) * D]), reads=[mod])
                            else:
                                gi = 0 if kind == "A1" else 1
                                S.op("dve", lambda e, src=src, gi=gi, mod=mod: e.scalar_tensor_tensor(
                                    out=mod[:, src * D:(src + 1) * D], in0=mod[:, src * D:(src + 1) * D], scalar=1.0, in1=gg[:, gi, :],
                                    op0=ALU.add, op1=ALU.mult), reads=[mod, gg], writes=[mod])
                                S.dma("sp", "mod%d" % bb, lambda e, dst=dst, src=src, bb=bb, mod=mod: e.dma_start(
                                    out=modv[bb, dst:dst + 1, :], in_=mod[0:1, src * D:(src + 1) * D]), reads=[mod])
                S.barrier()

        setup()

        modv_res = Res("modv")

        def load_mod_row(stack, name, b, i):
            t = sb(stack, name, [128, D], F32)
            S.dma("sp", name, lambda e: e.dma_start(out=t[:], in_=bcast_rows(modv[b, i:i + 1, :], 128)), writes=[t])
            return t

        def phase1(b):
            with contextlib.ExitStack() as st:
                win = sb(st, "win", [128, KC, IN_COLS], BF16)
                for kc in range(KC):
                    for c0 in range(0, IN_COLS, 1024):
                        c1 = min(c0 + 1024, IN_COLS)
                        S.dma("pool", "win", lambda e, kc=kc, c0=c0, c1=c1: e.dma_start(
                            out=win[:, kc, c0:c1], in_=w_in[kc * 128:(kc + 1) * 128, c0:c1]), accum=[win])
                A1 = load_mod_row(st, "A1", b, 0)
                B1 = load_mod_row(st, "B1", b, 1)
                cosT = sb(st, "cosT", [128, NT, 32], F32)
                sinT = sb(st, "sinT", [128, NT, 32], F32)
                with contextlib.ExitStack() as st2:
                    pi_ = sb(st2, "pi_", [128, NT], I32)
                    with nc.allow_non_contiguous_dma(reason="one-time transposed load of positions"):
                        S.dma("sp", "pi", lambda e: e.dma_start(out=pi_[:], in_=pos[b, :].rearrange("(t p) -> p t", p=128)), writes=[pi_])
                    posf = sb(st2, "posf", [128, NT], F32)
                    S.op("dve", lambda e: e.tensor_copy(out=posf[:], in_=pi_[:]), reads=[pi_], writes=[posf])
                    ang = sb(st2, "ang", [128, NT, 32], F32)
                    S.op("dve", lambda e: e.tensor_tensor(out=ang[:], in0=posf[:].unsqueeze(2).to_broadcast([128, NT, 32]),
                                                          in1=invf_rep[:].unsqueeze(1).to_broadcast([128, NT, 32]), op=ALU.mult),
                         reads=[posf, invf_rep], writes=[ang])
                    tq = sb(st2, "tq", [128, NT, 32], F32)
                    nq = sb(st2, "nq", [128, NT, 32], F32)
                    MAGIC = 12582912.0
                    C1 = 6.28125
                    C2 = 2.0 * math.pi - 6.28125
                    for (dst, shift) in [(sinT, 0.0), (cosT, math.pi / 2)]:
                        S.op("dve", lambda e, shift=shift: e.tensor_scalar(out=tq[:], in0=ang[:], scalar1=shift, scalar2=None,
                                                                          op0=ALU.add), reads=[ang], writes=[tq])
                        S.op("dve", lambda e: e.tensor_scalar(out=nq[:], in0=tq[:], scalar1=1.0 / (2 * math.pi), scalar2=MAGIC,
                                                              op0=ALU.mult, op1=ALU.add), reads=[tq], writes=[nq])
                        S.op("dve", lambda e: e.tensor_scalar(out=nq[:], in0=nq[:], scalar1=-MAGIC, scalar2=None,
                                                              op0=ALU.add), reads=[nq], writes=[nq])
                        S.op("dve", lambda e: e.scalar_tensor_tensor(out=tq[:], in0=nq[:], scalar=-C1, in1=tq[:],
                                                                     op0=ALU.mult, op1=ALU.add), reads=[nq, tq], writes=[tq])
                        S.op("dve", lambda e: e.scalar_tensor_tensor(out=tq[:], in0=nq[:], scalar=-C2, in1=tq[:],
                                                                     op0=ALU.mult, op1=ALU.add), reads=[nq, tq], writes=[tq])
                        S.op("dve", lambda e: e.tensor_scalar(out=tq[:], in0=tq[:], scalar1=3.1415925, scalar2=-3.1415925,
                                                              op0=ALU.min, op1=ALU.max), reads=[tq], writes=[tq])
                        S.op("act", lambda e, dst=dst: e.activation(out=dst[:], in_=tq[:], func=AF.Sin), reads=[tq], writes=[dst])
                S.barrier()
                NBUF = 2
                xt = [sb(st, "xt%d" % i, [128, D], F32) for i in range(NBUF)]
                tmp = [sb(st, "tmp%d" % i, [128, D], F32) for i in range(NBUF)]
                hb = [sb(st, "hb%d" % i, [128, D], BF16) for i in range(NBUF)]
                hT = [sb(st, "hT%d" % i, [128, KC, 128], BF16) for i in range(NBUF)]
                sq = sb(st, "sq", [128, D], F32)
                ss = [sb(st, "ss%d" % i, [128, 4], F32) for i in range(NBUF)]
                ms8 = [sb(st, "ms8_%d" % i, [128, 8], F32) for i in range(4)]
                qn = [sb(st, "qn%d" % i, [128, 8, HD], F32) for i in range(2)]
                qg = [sb(st, "qg%d" % i, [128, 8, HD], F32) for i in range(2)]
                ra = [sb(st, "ra%d" % i, [128, 8, 32], F32) for i in range(4)]
                QKF = [sb(st, "QKF%d" % i, [128, 16, 66], BF16) for i in range(NBUF)]
                QKD = [sb(st, "QKD%d" % i, [128, 16, HD], BF16) for i in range(NBUF)]
                QTs = [sb(st, "QTs%d" % i, [65, 32, 128], BF16) for i in range(NBUF)]
                VF = [sb(st, "VF%d" % i, [128, 8, 65], BF16) for i in range(NBUF)]
                VD = [sb(st, "VD%d" % i, [128, 4, 129], BF16) for i in range(NBUF)]
                GT = [sb(st, "GT%d" % i, [128, 2048], BF16) for i in range(NBUF)]
                fz = [sb(st, "fz%d" % i, [128, 8], F32) for i in range(3)]
                carry = sb(st, "carry", [128, 8], F32)
                S.op("pool", lambda e: e.memset(carry[:], 0.0), writes=[carry])
                for i in range(NBUF):
                    S.op("pool", lambda e, i=i: e.memset(QKF[i][:], 0.0), writes=[QKF[i]])
                    S.op("pool", lambda e, i=i: e.memset(QKF[i][:, 8:16, 64:65], 1.0), reads=[QKF[i]], accum=[QKF[i]])
                    S.op("pool", lambda e, i=i: e.memset(VF[i][:, :, 64:65], 1.0), writes=[VF[i]])
                    S.op("pool", lambda e, i=i: e.memset(VD[i][:, :, 128:129], 1.0), writes=[VD[i]])
                pT = ps(st, "pT", [128, KC, 128], BF16)
                zb = [ps(st, "zb%d" % i, [128, 512], F32) for i in range(4)]
                psm = ps(st, "psm", [128, 16], F32)
                qtp = ps(st, "qtp", [65, 16, 128], BF16)
                zbi = [0]

                def zgroup(hTt, col0, ncols):
                    z = zb[zbi[0] % 4]
                    zbi[0] += 1
                    for kc in range(KC):
                        S.op("pe", lambda e, kc=kc: e.matmul(z[:, 0:ncols], lhsT=hTt[:, kc, :], rhs=win[:, kc, col0:col0 + ncols],
                                                             start=(kc == 0), stop=(kc == KC - 1)),
                             reads=[hTt, win], accum=[z])
                    return z

                def headnorm(z, gi, mi, dst_fn, rope_t=None):
                    m = ms8[mi]
                    S.op("act", lambda e: e.activation(out=sq[:, 0:512], in_=z[:], func=AF.Square), reads=[z], writes=[sq])
                    S.op("dve", lambda e: e.tensor_reduce(out=m[:], in_=sq[:, 0:512].rearrange("p (h d) -> p h d", d=HD),
                                                          axis=AX.X, op=ALU.add), reads=[sq], writes=[m])
                    S.op("act", lambda e: e.activation(out=m[:], in_=m[:], func=AF.Ln, scale=1.0 / HD, bias=EPS), reads=[m], writes=[m])
                    S.op("act", lambda e: e.activation(out=m[:], in_=m[:], func=AF.Exp, scale=-0.5), reads=[m], writes=[m])
                    q1 = qn[mi % 2]
                    S.op("dve", lambda e: e.tensor_tensor(out=q1[:], in0=z[:].rearrange("p (h d) -> p h d", d=HD),
                                                          in1=m[:].unsqueeze(2).to_broadcast([128, 8, HD]), op=ALU.mult),
                         reads=[z, m], writes=[q1])
                    gb = G4[:, gi, :].unsqueeze(1).to_broadcast([128, 8, HD])
                    if rope_t is None:
                        S.op("pool", lambda e: e.tensor_tensor(out=dst_fn(0, HD), in0=q1[:], in1=gb, op=ALU.mult),
                             reads=[q1, G4], accum=[dst_fn.tile])
                    else:
                        q2 = qg[mi % 2]
                        S.op("pool", lambda e: e.tensor_tensor(out=q2[:], in0=q1[:], in1=gb, op=ALU.mult),
                             reads=[q1, G4], writes=[q2])
                        cb = cosT[:, rope_t, :].unsqueeze(1).to_broadcast([128, 8, 32])
                        sbb = sinT[:, rope_t, :].unsqueeze(1).to_broadcast([128, 8, 32])
                        x1 = q2[:, :, 0:32]
                        x2 = q2[:, :, 32:64]
                        S.op("dve", lambda e: e.tensor_tensor(out=ra[0][:], in0=x1, in1=cb, op=ALU.mult), reads=[q2, cosT], writes=[ra[0]])
                        S.op("pool", lambda e: e.tensor_tensor(out=ra[1][:], in0=x2, in1=sbb, op=ALU.mult), reads=[q2, sinT], writes=[ra[1]])
                        S.op("dve", lambda e: e.tensor_tensor(out=ra[2][:], in0=x2, in1=cb, op=ALU.mult), reads=[q2, cosT], writes=[ra[2]])
                        S.op("pool", lambda e: e.tensor_tensor(out=ra[3][:], in0=x1, in1=sbb, op=ALU.mult), reads=[q2, sinT], writes=[ra[3]])
                        S.op("dve", lambda e: e.tensor_tensor(out=dst_fn(0, 32), in0=ra[0][:], in1=ra[1][:], op=ALU.subtract),
                             reads=[ra[0], ra[1]], accum=[dst_fn.tile])
                        S.op("pool", lambda e: e.tensor_tensor(out=dst_fn(32, 64), in0=ra[2][:], in1=ra[3][:], op=ALU.add),
                             reads=[ra[2], ra[3]], accum=[dst_fn.tile])

                def mkdst(tile, h0):
                    def f(a, bb):
                        return tile[:, h0:h0 + 8, a:bb]
                    f.tile = tile
                    return f

                def load_x(t):
                    i = t % NBUF
                    S.dma("sp", "xt%d" % i, lambda e: e.dma_start(out=xt[i][:], in_=x[b, t * 128:(t + 1) * 128, :]), writes=[xt[i]])

                def pre_a(t):
                    i = t % NBUF
                    ssi = ss[i]
                    S.op("act", lambda e: e.activation(out=sq[:], in_=xt[i][:], func=AF.Square, accum_out=ssi[:, 0:1]),
                         reads=[xt[i]], writes=[sq, ssi])
                    S.op("act", lambda e: e.activation(out=ssi[:, 1:2], in_=ssi[:, 0:1], func=AF.Ln, scale=1.0 / D, bias=EPS),
                         reads=[ssi], accum=[ssi])
                    S.op("act", lambda e: e.activation(out=ssi[:, 2:3], in_=ssi[:, 1:2], func=AF.Exp, scale=-0.5),
                         reads=[ssi], accum=[ssi])
                    S.op("dve", lambda e: e.scalar_tensor_tensor(out=tmp[i][:], in0=xt[i][:], scalar=ssi[:, 2:3], in1=A1[:],
                                                                 op0=ALU.mult, op1=ALU.mult), reads=[xt[i], ssi, A1], writes=[tmp[i]])
                    S.op("pool", lambda e: e.tensor_tensor(out=hb[i][:], in0=tmp[i][:], in1=B1[:], op=ALU.add),
                         reads=[tmp[i], B1], writes=[hb[i]])

                def pre_b(t):
                    i = t % NBUF
                    for kc in range(KC):
                        S.op("pe", lambda e, kc=kc: e.transpose(out=pT[:, kc, :], in_=hb[i][:, kc * 128:(kc + 1) * 128], identity=ident_b[:]),
                             reads=[hb[i], ident_b], accum=[pT])
                    S.op("act", lambda e: e.copy(out=hT[i][:], in_=pT[:]), reads=[pT], writes=[hT[i]])

                def zstage(t):
                    i = t % NBUF
                    z = zgroup(hT[i], C_FF, 8)
                    f0, f1, f2 = fz
                    S.op("dve", lambda e: e.tensor_tensor(out=f0[:], in0=z[:, 0:8], in1=bf_rep[:], op=ALU.add), reads=[z, bf_rep], writes=[f0])
                    S.op("act", lambda e: e.activation(out=f1[:], in_=f0[:], func=AF.Exp, scale=-1.0), reads=[f0], writes=[f1])
                    S.op("act", lambda e: e.activation(out=f2[:], in_=f1[:], func=AF.Ln, bias=1.0), reads=[f1], writes=[f2])
                    z = zgroup(hT[i], C_FQ, 512)
                    headnorm(z, 0, 0, mkdst(QKF[i], 0))
                    z = zgroup(hT[i], C_FK, 512)
                    headnorm(z, 1, 1, mkdst(QKF[i], 8))
                    z = zgroup(hT[i], C_FV, 512)
                    S.op("act", lambda e, z=z: e.copy(out=VF[i][:, :, 0:64], in_=z[:].rearrange("p (h d) -> p h d", d=HD)),
                         reads=[z], accum=[VF[i]])
                    S.op("pe", lambda e: e.matmul(psm[:, 0:8], lhsT=tri_i[:], rhs=f2[:], start=True, stop=True),
                         reads=[tri_i, f2], accum=[psm])
                    S.op("pe", lambda e: e.matmul(psm[:, 8:16], lhsT=ones_f[:], rhs=f2[:], start=False, stop=True, skip_group_check=True),
                         reads=[ones_f, f2], accum=[psm])
                    S.op("dve", lambda e: e.tensor_tensor(out=ncum[:, b, t, :], in0=psm[:, 0:8], in1=carry[:], op=ALU.add),
                         reads=[psm, carry], accum=[ncum])
                    S.op("dve", lambda e: e.tensor_tensor(out=carry[:], in0=psm[:, 8:16], in1=carry[:], op=ALU.add),
                         reads=[psm, carry], writes=[carry])
                    S.op("dve", lambda e: e.tensor_scalar(out=QKF[i][:, 0:8, 64:65], in0=ncum[:, b, t, :].unsqueeze(2), scalar1=-1.0,
                                                          scalar2=None, op0=ALU.mult), reads=[ncum], accum=[QKF[i]])
                    z = zgroup(hT[i], C_DQ, 512)
                    headnorm(z, 2, 2, mkdst(QKD[i], 0), rope_t=t)
                    z = zgroup(hT[i], C_DK, 512)
                    headnorm(z, 3, 3, mkdst(QKD[i], 8), rope_t=t)
                    z = zgroup(hT[i], C_DV, 512)
                    S.op("act", lambda e, z=z: e.copy(out=VD[i][:, :, 0:128], in_=z[:].rearrange("p (h d) -> p h d", d=128)),
                         reads=[z], accum=[VD[i]])
                    for gi in range(4):
                        z = zgroup(hT[i], C_G + gi * 512, 512)
                        if gi % 2 == 0:
                            S.op("act", lambda e, z=z, gi=gi: e.copy(out=GT[i][:, gi * 512:(gi + 1) * 512], in_=z[:]), reads=[z], accum=[GT[i]])
                        else:
                            S.op("dve", lambda e, z=z, gi=gi: e.tensor_copy(out=GT[i][:, gi * 512:(gi + 1) * 512], in_=z[:]), reads=[z], accum=[GT[i]])
                    for h in range(16):
                        S.op("pe", lambda e, h=h: e.transpose(out=qtp[0:65, h, :], in_=QKF[i][:, h, 0:65], identity=ident_b[:]),
                             reads=[QKF[i], ident_b], accum=[qtp])
                    S.op("act", lambda e: e.copy(out=QTs[i][0:65, 0:16, :], in_=qtp[0:65, :, :]), reads=[qtp], accum=[QTs[i]])
                    for h in range(16):
                        S.op("pe", lambda e, h=h: e.transpose(out=qtp[0:64, h, :], in_=QKD[i][:, h, :], identity=ident_b[:]),
                             reads=[QKD[i], ident_b], accum=[qtp])
                    S.op("dve", lambda e: e.tensor_copy(out=QTs[i][0:64, 16:32, :], in_=qtp[0:64, :, :]), reads=[qtp], accum=[QTs[i]])
                    cs = slice(t * 128, (t + 1) * 128)
                    S.dma("sp", "sQ%d" % i, lambda e: e.dma_start(out=qTf[b].rearrange("h r t -> r h t")[:, :, cs], in_=QTs[i][0:65, 0:8, :]), reads=[QTs[i]])
                    S.dma("sp", "sQ%d" % i, lambda e: e.dma_start(out=kTf[b].rearrange("h r t -> r h t")[:, :, cs], in_=QTs[i][0:65, 8:16, :]), reads=[QTs[i]])
                    S.dma("sp", "sQ%d" % i, lambda e: e.dma_start(out=qTd[b].rearrange("h r t -> r h t")[:, :, cs], in_=QTs[i][0:64, 16:24, :]), reads=[QTs[i]])
                    S.dma("sp", "sQ%d" % i, lambda e: e.dma_start(out=kTd[b].rearrange("h r t -> r h t")[:, :, cs], in_=QTs[i][0:64, 24:32, :]), reads=[QTs[i]])
                    S.dma("sp", "sVF%d" % i, lambda e: e.dma_start(out=vF[b, cs, :], in_=VF[i][:].rearrange("p h d -> p (h d)")), reads=[VF[i]])
                    S.dma("sp", "sVD%d" % i, lambda e: e.dma_start(out=vD[b, cs, :], in_=VD[i][:].rearrange("p h d -> p (h d)")), reads=[VD[i]])
                    S.dma("sp", "sGT%d" % i, lambda e: e.dma_start(out=gl[b, cs, :], in_=GT[i][:]), reads=[GT[i]])

                load_x(0)
                if NT > 1:
                    load_x(1)
                pre_a(0)
                pre_b(0)
                for t in range(NT):
                    if t + 1 < NT:
                        pre_a(t + 1)
                    if t + 2 < NT:
                        load_x(t + 2)
                    zstage(t)
                    if t + 1 < NT:
                        pre_b(t + 1)
                S.barrier()


        wcres = Res("wcast")
        CH = 64
        castjobs = []
        for (src, dst, key, c) in ((w1, w1c, "wc1", 4), (w3, w3c, "wc3", 4), (w2, w2c, "wc2", 2)):
            srcv = src.rearrange("(r c) n -> r (c n)", c=c)
            for r0 in range(0, 8192, CH):
                castjobs.append((srcv, dst, key, r0))
        castpos = [0]
        for key in ("wc1", "wc2", "wc3"):
            S.bg_keys.add(key)

        def bg_cast(n=1):
            for _ in range(n):
                if castpos[0] >= len(castjobs):
                    return
                srcv, dst, key, r0 = castjobs[castpos[0]]
                castpos[0] += 1
                S.dma("pool", key, lambda e: e.dma_start(out=dst[r0:r0 + CH, :], in_=srcv[r0:r0 + CH, :]), accum=[wcres])

        def phase2(b, o_all):
            with contextlib.ExitStack() as st:
                sbk = [ps(st, "sbk%d" % i, [128, 512], F32) for i in range(3)]
                obs = [ps(st, "ob%d" % i, [128, 4, 65], F32) for i in range(2)]
                od = ps(st, "od", [128, 3, 512], F32)
                pts = [sb(st, "pt%d" % i, [128, 512], BF16) for i in range(4)]
                cnt = [0]
                with contextlib.ExitStack() as st2:
                    QT = [sb(st2, "QTf%d" % i, [65, TT], BF16) for i in range(2)]
                    KT = [sb(st2, "KTf%d" % i, [65, TT], BF16) for i in range(2)]
                    VT = [sb(st2, "VTf%d" % i, [128, NT, 65], BF16) for i in range(2)]
                    rec = [sb(st2, "rec%d" % i, [128, 4], F32) for i in range(2)]

                    def load_f(h):
                        i = h % 2
                        S.dma("sp", "QTf%d" % i, lambda e: e.dma_start(out=QT[i][:], in_=qTf[b, h, :, :]), writes=[QT[i]])
                        S.dma("sp", "KTf%d" % i, lambda e: e.dma_start(out=KT[i][:], in_=kTf[b, h, :, :]), writes=[KT[i]])
                        S.dma("sp", "VTf%d" % i, lambda e: e.dma_start(
                            out=VT[i][:], in_=vF[b].rearrange("(t p) c -> p t c", p=128)[:, :, h * 65:(h + 1) * 65]), writes=[VT[i]])

                    tasks = [(h, g, kt) for h in range(8) for g in range(NG) for kt in range(4 * g + 4)]

                    def qk_f(p):
                        h, g, kt = tasks[p]
                        i = h % 2
                        c0 = 128 * max(kt - 4 * g, 0)
                        sbank = sbk[p % 3]
                        S.op("pe", lambda e: e.matmul(sbank[:, c0:512], lhsT=KT[i][:, kt * 128:(kt + 1) * 128],
                                                      rhs=QT[i][:, g * 512 + c0:(g + 1) * 512], start=True, stop=True),
                             reads=[KT[i], QT[i]], accum=[sbank])

                    def rest_f(p):
                        h, g, kt = tasks[p]
                        i = h % 2
                        j = kt - 4 * g
                        jb = max(j, 0)
                        c0 = 128 * jb
                        sbank = sbk[p % 3]
                        pt = pts[p % 4]
                        ob = obs[g % 2]
                        S.op("act", lambda e: e.activation(out=pt[:, c0:512], in_=sbank[:, c0:512], func=AF.Exp,
                                                           bias=ncum[:, b, kt, h:h + 1]), reads=[sbank, ncum], writes=[pt])
                        if j >= 0:
                            S.op("pool", lambda e: e.affine_select(out=pt[:, c0:c0 + 128], in_=pt[:, c0:c0 + 128], pattern=[[1, 128]],
                                                                   compare_op=ALU.is_ge, fill=0.0, base=0, channel_multiplier=-1),
                                 reads=[pt], writes=[pt])
                        for qb in range(jb, 4):
                            S.op("pe", lambda e, qb=qb: e.matmul(
                                ob[:, qb, :], lhsT=pt[:, qb * 128:(qb + 1) * 128], rhs=VT[i][:, kt, :],
                                start=(kt == 0 and qb == 0), stop=(kt == 4 * g + qb), skip_group_check=True), reads=[pt, VT[i]], accum=[ob])
                        if kt == 4 * g + 3:
                            r = rec[g % 2]
                            S.op("dve", lambda e: e.reciprocal(out=r[:], in_=ob[:, :, 64]), reads=[ob], writes=[r])
                            S.op("dve", lambda e: e.tensor_tensor(out=o_all[:, g * 4:(g + 1) * 4, h * 64:(h + 1) * 64], in0=ob[:, :, 0:64],
                                                                  in1=r[:].unsqueeze(2).to_broadcast([128, 4, 64]), op=ALU.mult),
                                 reads=[ob, r], accum=[o_all])

                    LA = 2
                    load_f(0)
                    for p in range(min(LA, len(tasks))):
                        if tasks[p][1] == 0 and tasks[p][2] == 0 and tasks[p][0] + 1 < 8:
                            pass
                        qk_f(p)
                    for p in range(len(tasks)):
                        h, g, kt = tasks[p]
                        if g == 0 and kt == 0 and h + 1 < 8:
                            load_f(h + 1)
                        if p + LA < len(tasks):
                            qk_f(p + LA)
                        rest_f(p)
                        if p % 5 == 4:
                            bg_cast()
                S.barrier()
                with contextlib.ExitStack() as st2:
                    QT2 = [sb(st2, "QTd%d" % i, [64, 2, TT], BF16) for i in range(2)]
                    KT2 = [sb(st2, "KTd%d" % i, [64, 2, TT], BF16) for i in range(2)]
                    VT2 = [sb(st2, "VTd%d" % i, [128, NT, 129], BF16) for i in range(2)]
                    onorm = sb(st2, "onorm", [128, 8, 128], F32)
                    oo = sb(st2, "oo", [128, 4, 128], F32)
                    sq2 = sb(st2, "sq2", [128, 4, 128], F32)
                    recd = sb(st2, "recd", [128, 8], F32)
                    msd = sb(st2, "msd", [128, 4], F32)

                    def load_d(hd):
                        i = hd % 2
                        S.dma("sp", "QTd%d" % i, lambda e: e.dma_start(
                            out=QT2[i][:], in_=qTd[b, hd * 2:hd * 2 + 2, :, :].rearrange("m r t -> r m t")), writes=[QT2[i]])
                        S.dma("sp", "KTd%d" % i, lambda e: e.dma_start(
                            out=KT2[i][:], in_=kTd[b, hd * 2:hd * 2 + 2, :, :].rearrange("m r t -> r m t")), writes=[KT2[i]])
                        S.dma("sp", "VTd%d" % i, lambda e: e.dma_start(
                            out=VT2[i][:], in_=vD[b].rearrange("(t p) c -> p t c", p=128)[:, :, hd * 129:(hd + 1) * 129]), writes=[VT2[i]])

                    tasks = [(hd, g, kt, m) for hd in range(4) for g in range(NG) for kt in range(4 * g + 4) for m in range(2)]

                    def qk_d(p):
                        hd, g, kt, m = tasks[p]
                        i = hd % 2
                        c0 = 128 * max(kt - 4 * g, 0)
                        sbank = sbk[p % 3]
                        S.op("pe", lambda e: e.matmul(sbank[:, c0:512], lhsT=KT2[i][:, m, kt * 128:(kt + 1) * 128],
                                                      rhs=QT2[i][:, m, g * 512 + c0:(g + 1) * 512], start=True, stop=True),
                             reads=[KT2[i], QT2[i]], accum=[sbank])

                    def rest_d(p):
                        hd, g, kt, m = tasks[p]
                        i = hd % 2
                        j = kt - 4 * g
                        jb = max(j, 0)
                        c0 = 128 * jb
                        sbank = sbk[p % 3]
                        pt = pts[p % 4]
                        S.op("act", lambda e: e.activation(out=pt[:, c0:512], in_=sbank[:, c0:512], func=AF.Exp),
                             reads=[sbank], writes=[pt])
                        if j >= 0:
                            S.op("pool", lambda e: e.memset(pt[64:128, c0:c0 + 64], 0.0), reads=[pt], writes=[pt])
                        for qb in range(jb, 4):
                            idx = m * 4 + qb
                            bank = idx // 3
                            off = (idx % 3) * 129
                            S.op("pe", lambda e, qb=qb, bank=bank, off=off, idx=idx: e.matmul(
                                od[:, bank, off:off + 129], lhsT=pt[:, qb * 128:(qb + 1) * 128], rhs=VT2[i][:, kt, :],
                                start=(kt == 0 and idx in (0, 3, 6)), stop=(kt == 4 * g + qb), skip_group_check=True), reads=[pt, VT2[i]], accum=[od])
                        if kt == 4 * g + 3 and m == 1:
                            for bank in range(3):
                                n = 3 if bank < 2 else 2
                                view = od[:, bank, 0:n * 129].rearrange("p (a c) -> p a c", c=129)
                                S.op("dve", lambda e, view=view, bank=bank, n=n: e.reciprocal(out=recd[:, bank * 3:bank * 3 + n], in_=view[:, :, 128]),
                                     reads=[od], accum=[recd])
                                S.op("dve", lambda e, view=view, bank=bank, n=n: e.tensor_tensor(
                                    out=onorm[:, bank * 3:bank * 3 + n, :], in0=view[:, :, 0:128],
                                    in1=recd[:, bank * 3:bank * 3 + n].unsqueeze(2).to_broadcast([128, n, 128]), op=ALU.mult),
                                     reads=[od, recd], accum=[onorm])
                            S.op("dve", lambda e: e.scalar_tensor_tensor(out=oo[:], in0=onorm[:, 4:8, :], scalar=nlam[:, 0:1], in1=onorm[:, 0:4, :],
                                                                         op0=ALU.mult, op1=ALU.add), reads=[onorm, nlam], writes=[oo])
                            S.op("pool", lambda e: e.tensor_tensor(out=sq2[:], in0=oo[:], in1=oo[:], op=ALU.mult), reads=[oo], writes=[sq2])
                            S.op("dve", lambda e: e.tensor_reduce(out=msd[:], in_=sq2[:], axis=AX.X, op=ALU.add), reads=[sq2], writes=[msd])
                            S.op("act", lambda e: e.activation(out=msd[:], in_=msd[:], func=AF.Ln, scale=1.0 / 128, bias=EPS), reads=[msd], writes=[msd])
                            S.op("act", lambda e: e.activation(out=msd[:], in_=msd[:], func=AF.Exp, scale=-0.5), reads=[msd], writes=[msd])
                            S.op("dve", lambda e: e.tensor_tensor(out=oo[:], in0=oo[:], in1=msd[:].unsqueeze(2).to_broadcast([128, 4, 128]), op=ALU.mult),
                                 reads=[oo, msd], writes=[oo])
                            S.op("pool", lambda e: e.tensor_tensor(out=o_all[:, g * 4:(g + 1) * 4, 512 + hd * 128:512 + (hd + 1) * 128], in0=oo[:],
                                                                   in1=gsub[:].unsqueeze(1).to_broadcast([128, 4, 128]), op=ALU.mult),
                                 reads=[oo, gsub], accum=[o_all])

                    LA = 2
                    load_d(0)
                    for p in range(min(LA, len(tasks))):
                        qk_d(p)
                    for p in range(len(tasks)):
                        hd, g, kt, m = tasks[p]
                        if g == 0 and kt == 0 and m == 0 and hd + 1 < 4:
                            load_d(hd + 1)
                        if p + LA < len(tasks):
                            qk_d(p + LA)
                        rest_d(p)
                        if p % 5 == 4:
                            bg_cast()
            S.barrier()
            if dbg:
                S.dma("sp", "dbgoc", lambda e: e.dma_start(out=dbg_t["oc"][b].rearrange("(t p) d -> p t d", p=128), in_=o_all[:]), reads=[o_all])

        def phase3(b, o_all):
            with contextlib.ExitStack() as st:
                wpf = sb(st, "wpf", [128, 4, D], BF16)
                wpd = sb(st, "wpd", [128, 4, D], BF16)
                wo = sb(st, "wo", [128, 8, D], BF16)
                wr = sb(st, "wr", [128, 8, 36], BF16)
                brt = sb(st, "brt", [128, 36], F32)
                S.dma("pool", "wpf", lambda e: e.dma_start(out=wpf[:], in_=w_pf.rearrange("(c k) n -> k c n", k=128)), writes=[wpf])
                S.dma("pool", "wpd", lambda e: e.dma_start(out=wpd[:], in_=w_pd.rearrange("(c k) n -> k c n", k=128)), writes=[wpd])
                S.dma("pool", "wo", lambda e: e.dma_start(out=wo[:], in_=w_out.rearrange("(c k) n -> k c n", k=128)), writes=[wo])
                S.dma("pool", "wr", lambda e: e.dma_start(out=wr[:, :, 0:4], in_=w_rg.rearrange("(c k) n -> k c n", k=128)), accum=[wr])
                S.dma("pool", "wr", lambda e: e.dma_start(out=wr[:, :, 4:36], in_=w_re.rearrange("(c k) n -> k c n", k=128)), accum=[wr])
                S.dma("sp", "brt", lambda e: e.dma_start(out=brt[:, 0:4], in_=bcast_rows(b_rg[0:1, :], 128)), accum=[brt])
                S.dma("sp", "brt", lambda e: e.dma_start(out=brt[:, 4:36], in_=bcast_rows(b_re[0:1, :], 128)), accum=[brt])
                gt1 = load_mod_row(st, "gt1", b, 2)
                A2 = load_mod_row(st, "A2", b, 3)
                B2 = load_mod_row(st, "B2", b, 4)
                NBUF = 2
                xt = [sb(st, "x3_%d" % i, [128, D], F32) for i in range(3)]
                gls = [sb(st, "gls%d" % i, [128, 2048], BF16) for i in range(3)]
                sgs = [sb(st, "sg0", [128, 2048], BF16)] * NBUF
                oTs = [sb(st, "oT0", [128, KC, 128], BF16)] * NBUF
                m1s = [sb(st, "m1_0", [128, D], F32)] * NBUF
                m2s = [sb(st, "m2_0", [128, D], F32)] * NBUF
                mgs = [sb(st, "mg%d" % i, [128, D], BF16) for i in range(NBUF)]
                mTs = [sb(st, "mT0", [128, KC, 128], BF16)] * NBUF
                x1 = [sb(st, "x1_%d" % i, [128, D], F32) for i in range(NBUF)]
                tmps = [sb(st, "tmp3_0", [128, D], F32)] * NBUF
                sqj = sb(st, "sqj", [128, D], BF16)
                h2 = [sb(st, "h2_%d" % i, [128, D], BF16) for i in range(NBUF)]
                h2T = sb(st, "h2T", [128, KC, 128], BF16)
                ss = sb(st, "ss3", [128, 4], F32)
                Lall = sb(st, "Lall", [128, NT, 36], F32)
                rsm = sb(st, "rsm", [128, 16, NT], F32)
                r4 = sb(st, "r4", [128, 3, NT * 4], F32)
                pT = ps(st, "pT3", [128, KC, 128], BF16)
                pfd = ps(st, "pfd", [128, 4, 512], F32)
                pyy = ps(st, "pyy", [128, 2, 512], F32)
                prt = ps(st, "prt", [128, 128], F32)

                def load3(t):
                    i = t % 3
                    S.dma("sp", "x3_%d" % i, lambda e: e.dma_start(out=xt[i][:], in_=x[b, t * 128:(t + 1) * 128, :]), writes=[xt[i]])
                    S.dma("sp", "gls%d" % i, lambda e: e.dma_start(out=gls[i][:], in_=gl[b, t * 128:(t + 1) * 128, :]), writes=[gls[i]])

                def transp(src_ap_fn, src_t, dstT, eng):
                    for kc in range(KC):
                        S.op("pe", lambda e, kc=kc: e.transpose(out=pT[:, kc, :], in_=src_ap_fn(kc), identity=ident_b[:]),
                             reads=[src_t, ident_b], accum=[pT])
                    if eng == "act":
                        S.op("act", lambda e: e.copy(out=dstT[:], in_=pT[:]), reads=[pT], writes=[dstT])
                    else:
                        S.op("dve", lambda e: e.tensor_copy(out=dstT[:], in_=pT[:]), reads=[pT], writes=[dstT])

                def stageA(t):
                    i = t % NBUF
                    oT, sg, m1, m2, mg = oTs[i], sgs[i], m1s[i], m2s[i], mgs[i]
                    transp(lambda kc: o_all[:, t, kc * 128:(kc + 1) * 128], o_all, oT, "act")
                    for nh in range(2):
                        for c in range(4):
                            S.op("pe", lambda e, nh=nh, c=c: e.matmul(pfd[:, nh, :], lhsT=oT[:, c, :], rhs=wpf[:, c, nh * 512:(nh + 1) * 512],
                                                                      start=(c == 0), stop=(c == 3)), reads=[oT, wpf], accum=[pfd])
                    for nh in range(2):
                        for c in range(4):
                            S.op("pe", lambda e, nh=nh, c=c: e.matmul(pfd[:, 2 + nh, :], lhsT=oT[:, 4 + c, :], rhs=wpd[:, c, nh * 512:(nh + 1) * 512],
                                                                      start=(c == 0), stop=(c == 3)), reads=[oT, wpd], accum=[pfd])
                    S.op("act", lambda e: e.activation(out=sg[:], in_=gls[t % 3][:], func=AF.Sigmoid), reads=[gls[t % 3]], writes=[sg])
                    S.op("dve", lambda e: e.tensor_tensor(out=m1[:], in0=pfd[:, 0:2, :].rearrange("p a n -> p (a n)"), in1=sg[:, 0:1024], op=ALU.mult),
                         reads=[pfd, sg], writes=[m1])
                    S.op("dve", lambda e: e.tensor_tensor(out=m2[:], in0=pfd[:, 2:4, :].rearrange("p a n -> p (a n)"), in1=sg[:, 1024:2048], op=ALU.mult),
                         reads=[pfd, sg], writes=[m2])
                    S.op("pool", lambda e: e.tensor_tensor(out=mg[:], in0=m1[:], in1=m2[:], op=ALU.add), reads=[m1, m2], writes=[mg])

                def stageB(t):
                    i = t % NBUF
                    tt = b * NT + t
                    mg, mT, tmp = mgs[i], mTs[i], tmps[i]
                    transp(lambda kc: mg[:, kc * 128:(kc + 1) * 128], mg, mT, "act")
                    for nh in range(2):
                        for c in range(KC):
                            S.op("pe", lambda e, nh=nh, c=c: e.matmul(pyy[:, nh, :], lhsT=mT[:, c, :], rhs=wo[:, c, nh * 512:(nh + 1) * 512],
                                                                      start=(c == 0), stop=(c == KC - 1)), reads=[mT, wo], accum=[pyy])
                    S.op("dve", lambda e: e.tensor_tensor(out=tmp[:], in0=pyy[:].rearrange("p a n -> p (a n)"), in1=gt1[:], op=ALU.mult),
                         reads=[pyy, gt1], writes=[tmp])
                    S.op("pool", lambda e: e.tensor_tensor(out=x1[i][:], in0=tmp[:], in1=xt[t % 3][:], op=ALU.add), reads=[tmp, xt[t % 3]], writes=[x1[i]])
                    S.dma("sp", "sx1_%d" % i, lambda e: e.dma_start(out=x1s[tt * 128:(tt + 1) * 128, :], in_=x1[i][:]), reads=[x1[i]])
                    S.op("act", lambda e: e.activation(out=sqj[:], in_=x1[i][:], func=AF.Square, accum_out=ss[:, 0:1]), reads=[x1[i]], writes=[sqj, ss])
                    S.op("act", lambda e: e.activation(out=ss[:, 1:2], in_=ss[:, 0:1], func=AF.Ln, scale=1.0 / D, bias=EPS), reads=[ss], accum=[ss])
                    S.op("act", lambda e: e.activation(out=ss[:, 2:3], in_=ss[:, 1:2], func=AF.Exp, scale=-0.5), reads=[ss], accum=[ss])
                    S.op("dve", lambda e: e.scalar_tensor_tensor(out=tmp[:], in0=x1[i][:], scalar=ss[:, 2:3], in1=A2[:], op0=ALU.mult, op1=ALU.mult),
                         reads=[x1[i], ss, A2], writes=[tmp])
                    S.op("pool", lambda e: e.tensor_tensor(out=h2[i][:], in0=tmp[:], in1=B2[:], op=ALU.add), reads=[tmp, B2], writes=[h2[i]])
                    S.dma("sp", "sh2_%d" % i, lambda e: e.dma_start(out=h2s[tt * 128:(tt + 1) * 128, :], in_=h2[i][:]), reads=[h2[i]])

                def stageC(t):
                    i = t % NBUF
                    tt = b * NT + t
                    transp(lambda kc: h2[i][:, kc * 128:(kc + 1) * 128], h2[i], h2T, "dve")
                    for c in range(KC):
                        S.op("pe", lambda e, c=c: e.matmul(prt[:, 0:36], lhsT=h2T[:, c, :], rhs=wr[:, c, :], start=(c == 0), stop=(c == KC - 1)),
                             reads=[h2T, wr], accum=[prt])
                    S.op("dve", lambda e: e.tensor_tensor(out=Lall[:, t, :], in0=prt[:, 0:36], in1=brt[:], op=ALU.add), reads=[prt, brt], accum=[Lall])

                load3(0)
                if NT > 1:
                    load3(1)
                stageA(0)
                for t in range(NT):
                    if t + 2 < NT:
                        load3(t + 2)
                    if t + 1 < NT:
                        stageA(t + 1)
                    if t >= 1:
                        stageC(t - 1)
                    stageB(t)
                stageC(NT - 1)
                TT_ = NT
                tt0 = b * NT
                BT = [m1s[0], m2s[0], tmps[0], xt[0], xt[1], xt[2], x1[0], x1[1]]

                def v32(tl):
                    return tl[:, 0:TT_ * 32].rearrange("p (t e) -> p t e", e=32)

                def v8(tl, k):
                    return tl[:, k * 256:k * 256 + TT_ * 8].rearrange("p (t e) -> p t e", e=8)

                def vec(k):
                    return rsm[:, k, :]

                def g4(k):
                    return r4[:, k, :].rearrange("p (t g) -> p t g", g=4)

                def bc(ap2, n):
                    return ap2.unsqueeze(2).to_broadcast([128, TT_, n])

                lg = Lall[:, :, 0:4]
                le4 = Lall[:, :, 4:36].rearrange("p t (g e) -> p t g e", e=8)
                T48, M1t, M2t, M12t, BASEt, RRt, TAt, SMt = BT
                mg4, zg, gw, m1v, m2v, dm, ex, w1_ = [vec(k) for k in range(8)]
                ohg, d4, eg = g4(0), g4(1), g4(2)
                e8, oh1, e8b, oh2 = v8(SMt, 0), v8(SMt, 1), v8(SMt, 2), v8(SMt, 3)
                R_ = [Lall, rsm, r4]

                def DV(fn, reads, writes, eng="dve"):
                    S.op(eng, fn, reads=reads, writes=writes)

                DV(lambda e: e.tensor_reduce(out=mg4, in_=lg, axis=AX.X, op=ALU.max), [Lall], [rsm])
                DV(lambda e: e.tensor_tensor(out=ohg, in0=lg, in1=bc(mg4, 4), op=ALU.is_equal), [Lall, rsm], [r4])
                DV(lambda e: e.tensor_tensor(out=d4, in0=lg, in1=bc(mg4, 4), op=ALU.subtract), [Lall, rsm, r4], [r4])
                DV(lambda e: e.activation(out=eg, in_=d4, func=AF.Exp), [r4], [r4], eng="act")
                DV(lambda e: e.tensor_reduce(out=zg, in_=eg, axis=AX.X, op=ALU.add), [r4, rsm], [rsm])
                DV(lambda e: e.reciprocal(out=gw, in_=zg), [rsm], [rsm])
                t48v = v32(T48).rearrange("p t (g e) -> p t g e", e=8)
                DV(lambda e: e.tensor_tensor(out=t48v, in0=le4, in1=ohg.unsqueeze(3).to_broadcast([128, TT_, 4, 8]), op=ALU.mult), [Lall, r4], [T48])
                DV(lambda e: e.tensor_reduce(out=e8, in_=t48v.rearrange("p t g e -> p t e g"), axis=AX.X, op=ALU.add), [T48], [SMt])
                DV(lambda e: e.tensor_reduce(out=m1v, in_=e8, axis=AX.X, op=ALU.max), [SMt, rsm], [rsm])
                DV(lambda e: e.tensor_tensor(out=oh1, in0=e8, in1=bc(m1v, 8), op=ALU.is_equal), [SMt, rsm], [SMt])
                DV(lambda e: e.scalar_tensor_tensor(out=e8b, in0=oh1, scalar=-1e30, in1=e8, op0=ALU.mult, op1=ALU.add), [SMt], [SMt])
                DV(lambda e: e.tensor_reduce(out=m2v, in_=e8b, axis=AX.X, op=ALU.max), [SMt, rsm], [rsm])
                DV(lambda e: e.tensor_tensor(out=oh2, in0=e8b, in1=bc(m2v, 8), op=ALU.is_equal), [SMt, rsm], [SMt])
                DV(lambda e: e.tensor_tensor(out=dm, in0=m2v, in1=m1v, op=ALU.subtract), [rsm], [rsm])
                DV(lambda e: e.activation(out=ex, in_=dm, func=AF.Exp), [rsm], [rsm], eng="act")
                DV(lambda e: e.tensor_scalar(out=ex, in0=ex, scalar1=1.0, scalar2=None, op0=ALU.add), [rsm], [rsm])
                DV(lambda e: e.reciprocal(out=w1_, in_=ex), [rsm], [rsm])
                S.op("dve", lambda e: e.tensor_tensor(out=wt_all[:, tt0:tt0 + TT_, 0], in0=w1_, in1=gw, op=ALU.mult), reads=[rsm], accum=[wt_all])
                S.op("dve", lambda e: e.tensor_tensor(out=wt_all[:, tt0:tt0 + TT_, 1], in0=gw, in1=wt_all[:, tt0:tt0 + TT_, 0], op=ALU.subtract),
                     reads=[rsm, wt_all], accum=[wt_all])
                M1v = v32(M1t).rearrange("p t (g e) -> p t g e", e=8)
                M2v = v32(M2t).rearrange("p t (g e) -> p t g e", e=8)
                DV(lambda e: e.tensor_tensor(out=M1v, in0=ohg.unsqueeze(3).to_broadcast([128, TT_, 4, 8]),
                                            in1=oh1.unsqueeze(2).to_broadcast([128, TT_, 4, 8]), op=ALU.mult), [r4, SMt], [M1t])
                DV(lambda e: e.tensor_tensor(out=M2v, in0=ohg.unsqueeze(3).to_broadcast([128, TT_, 4, 8]),
                                            in1=oh2.unsqueeze(2).to_broadcast([128, TT_, 4, 8]), op=ALU.mult), [r4, SMt], [M2t])
                DV(lambda e: e.tensor_tensor(out=v32(M12t), in0=v32(M1t), in1=v32(M2t), op=ALU.add), [M1t, M2t], [M12t])
                io_b = iota32[:].unsqueeze(1).to_broadcast([128, TT_, 32])
                for k, Mt in ((0, M1t), (1, M2t)):
                    DV(lambda e, Mt=Mt: e.tensor_tensor(out=v32(TAt), in0=v32(Mt), in1=io_b, op=ALU.mult), [Mt, iota32], [TAt])
                    S.op("dve", lambda e, k=k: e.tensor_reduce(out=eid_all[:, tt0:tt0 + TT_, k], in_=v32(TAt), axis=AX.X, op=ALU.add),
                         reads=[TAt], accum=[eid_all])
                NCOL = TT_ * 32
                pw = pfd[:, 0:2, :].rearrange("p a n -> p (a n)")
                pc_ = pfd[:, 2:4, :].rearrange("p a n -> p (a n)")
                for c0 in range(0, NCOL, 512):
                    c1 = min(c0 + 512, NCOL)
                    S.op("pe", lambda e, c0=c0, c1=c1: e.matmul(pw[:, c0:c1], lhsT=tri_s[:], rhs=M12t[:, c0:c1], start=True, stop=True),
                         reads=[tri_s, M12t], accum=[pfd])
                    S.op("pe", lambda e, c0=c0, c1=c1: e.matmul(pc_[:, c0:c1], lhsT=ones_f[:], rhs=M12t[:, c0:c1], start=True, stop=True),
                         reads=[ones_f, M12t], accum=[pfd])
                DV(lambda e: e.tensor_copy(out=v32(TAt), in_=pc_[:, 0:NCOL].rearrange("p (t e) -> p t e", e=32)), [pfd], [TAt])
                for t in range(TT_):
                    S.op("pool", lambda e, t=t: e.tensor_copy(out=BASEt[:, t * 32:(t + 1) * 32], in_=ecarry[:]), reads=[ecarry], accum=[BASEt])
                    S.op("pool", lambda e, t=t: e.tensor_tensor(out=ecarry[:], in0=ecarry[:], in1=TAt[:, t * 32:(t + 1) * 32], op=ALU.add),
                         reads=[ecarry, TAt, BASEt], writes=[ecarry])
                DV(lambda e: e.tensor_tensor(out=v32(RRt), in0=pw[:, 0:NCOL].rearrange("p (t e) -> p t e", e=32), in1=v32(BASEt), op=ALU.add),
                  [pfd, BASEt], [RRt])
                for k, Mt in ((0, M1t), (1, M2t)):
                    DV(lambda e, Mt=Mt: e.tensor_tensor(out=v32(TAt), in0=v32(RRt), in1=v32(Mt), op=ALU.mult), [RRt, Mt], [TAt])
                    S.op("dve", lambda e, k=k: e.tensor_reduce(out=rk_all[:, tt0:tt0 + TT_, k], in_=v32(TAt), axis=AX.X, op=ALU.add),
                         reads=[TAt], accum=[rk_all])
            S.barrier()

        def phase3b():
            S.bg_keys.clear()
            with contextlib.ExitStack() as st:
                padf = sb(st, "padf", [128, 32], F32)
                pend = sb(st, "pend", [128, 32], F32)
                pstart = sb(st, "pstart", [128, 32], F32)
                one32 = sb(st, "one32", [128, 32], F32)
                bst_i = sb(st, "bst_i", [128, NBLK], I32)
                bst_f = sb(st, "bst_f", [128, NBLK], F32)
                S.op("pool", lambda e: e.iota(bst_i[:], pattern=[[BS, NBLK]], base=0, channel_multiplier=0), writes=[bst_i])
                S.op("dve", lambda e: e.tensor_copy(out=bst_f[:], in_=bst_i[:]), reads=[bst_i], writes=[bst_f])
                cmpc = sb(st, "cmpc", [128, 32, NBLK], F32)
                S.op("dve", lambda e: e.tensor_tensor(out=cmpc[:], in0=ecarry[:].unsqueeze(2).to_broadcast([128, 32, NBLK]),
                                                      in1=bst_f[:].unsqueeze(1).to_broadcast([128, 32, NBLK]), op=ALU.is_gt),
                     reads=[ecarry, bst_f], writes=[cmpc])
                S.op("dve", lambda e: e.tensor_reduce(out=padf[:], in_=cmpc[:], axis=AX.X, op=ALU.add), reads=[cmpc], writes=[padf])
                S.op("dve", lambda e: e.tensor_scalar(out=padf[:], in0=padf[:], scalar1=float(BS), scalar2=None, op0=ALU.mult), reads=[padf], writes=[padf])
                S.op("pool", lambda e: e.memset(one32[:], 1.0), writes=[one32])
                S.op("dve", lambda e: e.tensor_tensor_scan(out=pend[:], data0=one32[:], data1=padf[:], initial=0.0, op0=ALU.mult, op1=ALU.add),
                     reads=[one32, padf], writes=[pend])
                S.op("dve", lambda e: e.tensor_tensor(out=pstart[:], in0=pend[:], in1=padf[:], op=ALU.subtract), reads=[pend, padf], writes=[pstart])
                big = sb(st, "big", [128, NTT, 32], F32)
                dsf = sb(st, "dsf", [128, NTT, 2], F32)
                for k in range(2):
                    S.op("dve", lambda e, k=k: e.tensor_tensor(out=big[:], in0=iota32[:].unsqueeze(1).to_broadcast([128, NTT, 32]),
                                                               in1=eid_all[:, :, k:k + 1].to_broadcast([128, NTT, 32]), op=ALU.is_equal),
                         reads=[iota32, eid_all], writes=[big])
                    S.op("dve", lambda e: e.tensor_tensor(out=big[:], in0=big[:], in1=pstart[:].unsqueeze(1).to_broadcast([128, NTT, 32]), op=ALU.mult),
                         reads=[big, pstart], writes=[big])
                    S.op("dve", lambda e, k=k: e.tensor_reduce(out=dsf[:, :, k:k + 1], in_=big[:], axis=AX.X, op=ALU.add), reads=[big], accum=[dsf])
                S.op("dve", lambda e: e.tensor_tensor(out=dsf[:], in0=dsf[:], in1=rk_all[:], op=ALU.add), reads=[dsf, rk_all], writes=[dsf])
                S.op("dve", lambda e: e.tensor_copy(out=dest_i[:], in_=dsf[:]), reads=[dsf], writes=[dest_i])
                cmpb = sb(st, "cmpb", [128, NBLK, 32], F32)
                S.op("dve", lambda e: e.tensor_tensor(out=cmpb[:], in0=pend[:].unsqueeze(1).to_broadcast([128, NBLK, 32]),
                                                      in1=bst_f[:].unsqueeze(2).to_broadcast([128, NBLK, 32]), op=ALU.is_le),
                     reads=[pend, bst_f], writes=[cmpb])
                BE = sb(st, "BE", [128, NBLK], F32)
                S.op("dve", lambda e: e.tensor_reduce(out=BE[:], in_=cmpb[:], axis=AX.X, op=ALU.add), reads=[cmpb], writes=[BE])
                S.op("dve", lambda e: e.tensor_scalar(out=BE[:], in0=BE[:], scalar1=float(NEXP - 1), scalar2=None, op0=ALU.min), reads=[BE], writes=[BE])
                bpc_i = sb(st, "bpc_i", [128, 1], I32)
                bpc = sb(st, "bpc", [128, 1], F32)
                S.op("pool", lambda e: e.iota(bpc_i[:], pattern=[[0, 1]], base=0, channel_multiplier=2), writes=[bpc_i])
                S.op("dve", lambda e: e.tensor_copy(out=bpc[:], in_=bpc_i[:]), reads=[bpc_i], writes=[bpc])
                idf = sb(st, "idf", [128, NBLK, 2], F32)
                idx1 = blkidx["idx1"]
                S.op("dve", lambda e: e.tensor_scalar(out=idf[:, :, 0], in0=BE[:], scalar1=256.0, scalar2=bpc[:, 0:1], op0=ALU.mult, op1=ALU.add),
                     reads=[BE, bpc], writes=[idf])
                S.op("dve", lambda e: e.tensor_scalar(out=idf[:, :, 1], in0=idf[:, :, 0], scalar1=1.0, scalar2=None, op0=ALU.add),
                     reads=[idf], accum=[idf])
                S.op("dve", lambda e: e.tensor_copy(out=idx1[:], in_=idf[:]), reads=[idf], writes=[idx1])
                if dbg:
                    S.dma("sp", "dbgbe", lambda e: e.dma_start(out=dbg_t["be"][:, 0:NBLK], in_=BE[:]), reads=[BE])
                    S.dma("sp", "dbgbe", lambda e: e.dma_start(out=dbg_t["be"][:, NBLK:NBLK + 32], in_=pend[:]), reads=[pend])
                    S.dma("sp", "dbgbe", lambda e: e.dma_start(out=dbg_t["be"][:, NBLK + 32:NBLK + 64], in_=ecarry[:]), reads=[ecarry])
                    S.dma("sp", "dbgrt", lambda e: e.dma_start(out=dbg_t["rt"][:, :, 0:2], in_=eid_all[:]), reads=[eid_all])
                    S.dma("sp", "dbgrt", lambda e: e.dma_start(out=dbg_t["rt"][:, :, 2:4], in_=wt_all[:]), reads=[wt_all])
                    S.dma("sp", "dbgrt", lambda e: e.dma_start(out=dbg_t["rt"][:, :, 4:6], in_=dsf[:]), reads=[dsf])
                    S.dma("sp", "dbgrt", lambda e: e.dma_start(out=dbg_t["rt"][:, :, 6:8], in_=rk_all[:]), reads=[rk_all])
                hb_ = [sb(st, "h2l%d" % i, [128, D], BF16) for i in range(2)]
                for tt in range(NTT):
                    i = tt % 2
                    S.dma("sp", "h2l%d" % i, lambda e: e.dma_start(out=hb_[i][:], in_=h2s[tt * 128:(tt + 1) * 128, :]), writes=[hb_[i]])
                    for k in range(2):
                        S.dma("pool", "h2sc%d" % i, lambda e, k=k: e.indirect_dma_start(
                            out=xbuf[:, :], out_offset=bass.IndirectOffsetOnAxis(ap=dest_i[:, tt, k:k + 1], axis=0),
                            in_=hb_[i][:], in_offset=None), reads=[hb_[i], dest_i, xz])
            S.barrier()

        def phase4():
            idx1 = blkidx["idx1"]
            bg_cast(len(castjobs))
            w1v, w3v, w2v = w1c, w3c, w2c
            SUB = BS // 128
            with contextlib.ExitStack() as st:
                w1b = [sb(st, "w1b%d" % i, [128, 8, DEXP], BF16) for i in range(3)]
                w3b = [sb(st, "w3b%d" % i, [128, 8, DEXP], BF16) for i in range(3)]
                w2b = [sb(st, "w2b%d" % i, [128, 4, D], BF16) for i in range(4)]
                xb = [sb(st, "xb%d" % i, [128, SUB, D], BF16) for i in range(3)]
                xTs = [sb(st, "xT%d" % i, [128, KC, BS], BF16) for i in range(2)]
                sact = [sb(st, "sact%d" % i, [128, BS], F32) for i in range(2)]
                gTs = [sb(st, "gT%d" % i, [128, 4, BS], BF16) for i in range(2)]
                yo = [sb(st, "yo%d" % i, [128, SUB, D], BF16) for i in range(2)]
                pX = ps(st, "pX", [128, KC, BS], BF16)
                ph = ps(st, "ph", [128, 4, 512], F32)
                py = ps(st, "py", [128, 2, 512], F32)

                def load_blk(bi):
                    i = bi % 3
                    for hf in range(2):
                        off = bass.IndirectOffsetOnAxis(ap=idx1[:, bi, hf:hf + 1], axis=0)
                        S.dma("pool", "w1b%d" % i, lambda e, hf=hf: e.indirect_dma_start(
                            out=w1b[i][:, hf * 4:(hf + 1) * 4, :].rearrange("p c n -> p (c n)"), out_offset=None,
                            in_=w1v[:, :], in_offset=off), reads=[idx1, wcres], accum=[w1b[i]])
                        S.dma("pool", "w3b%d" % i, lambda e, hf=hf: e.indirect_dma_start(
                            out=w3b[i][:, hf * 4:(hf + 1) * 4, :].rearrange("p c n -> p (c n)"), out_offset=None,
                            in_=w3v[:, :], in_offset=off), reads=[idx1, wcres], accum=[w3b[i]])
                        S.dma("pool", "w2b%d" % (bi % 4), lambda e, hf=hf: e.indirect_dma_start(
                            out=w2b[bi % 4][:, hf * 2:(hf + 1) * 2, :].rearrange("p c n -> p (c n)"), out_offset=None,
                            in_=w2v[:, :], in_offset=off), reads=[idx1, wcres], accum=[w2b[bi % 4]])
                    S.dma("sp", "xb%d" % i, lambda e: e.dma_start(out=xb[i][:], in_=xbuf[bi * BS:(bi + 1) * BS, :].rearrange("(s p) d -> p s d", p=128)),
                          writes=[xb[i]])

                def stX(bi):
                    i = bi % 2
                    xT = xTs[i]
                    for s_ in range(SUB):
                        for c in range(KC):
                            S.op("pe", lambda e, s_=s_, c=c: e.transpose(out=pX[:, c, s_ * 128:(s_ + 1) * 128], in_=xb[bi % 3][:, s_, :].rearrange("p (q c) -> p c q", c=8)[:, c, :],
                                                                         identity=ident_b[:]), reads=[xb[bi % 3], ident_b], accum=[pX])
                    S.op("act", lambda e: e.copy(out=xT[:], in_=pX[:]), reads=[pX], writes=[xT])

                def stH(bi):
                    i = bi % 2
                    xT, gT = xTs[i], gTs[i]
                    for fc in range(4):
                        for c in range(KC):
                            S.op("pe", lambda e, fc=fc, c=c: e.matmul(ph[:, fc, 0:BS], lhsT=w1b[bi % 3][:, c, :].rearrange("p (q f) -> p f q", f=4)[:, fc, :], rhs=xT[:, c, :],
                                                                      start=(c == 0), stop=(c == KC - 1), skip_group_check=True), reads=[w1b[bi % 3], xT], accum=[ph])
                        for c in range(KC):
                            S.op("pe", lambda e, fc=fc, c=c: e.matmul(ph[:, fc, 256:256 + BS], lhsT=w3b[bi % 3][:, c, :].rearrange("p (q f) -> p f q", f=4)[:, fc, :], rhs=xT[:, c, :],
                                                                      start=(c == 0), stop=(c == KC - 1), skip_group_check=True), reads=[w3b[bi % 3], xT], accum=[ph])
                        sa = sact[fc % 2]
                        S.op("act", lambda e, fc=fc, sa=sa: e.activation(out=sa[:], in_=ph[:, fc, 0:BS], func=AF.Silu), reads=[ph], writes=[sa])
                        S.op("dve", lambda e, fc=fc, sa=sa: e.tensor_tensor(out=gT[:, fc, :], in0=sa[:], in1=ph[:, fc, 256:256 + BS], op=ALU.mult),
                             reads=[sa, ph], accum=[gT])

                def stY(bi):
                    i = bi % 2
                    gT = gTs[i]
                    w2t = w2b[bi % 4]
                    for s_ in range(SUB):
                        for nh in range(2):
                            for fc in range(4):
                                S.op("pe", lambda e, s_=s_, nh=nh, fc=fc: e.matmul(py[:, nh, :], lhsT=gT[:, fc, s_ * 128:(s_ + 1) * 128],
                                                                                   rhs=w2t[:, fc, nh * 512:(nh + 1) * 512],
                                                                                   start=(fc == 0), stop=(fc == 3)), reads=[gT, w2t], accum=[py])
                        if s_ % 2 == 0:
                            S.op("act", lambda e, s_=s_: e.copy(out=yo[i][:, s_, :], in_=py[:].rearrange("p a n -> p (a n)")), reads=[py], accum=[yo[i]])
                        else:
                            S.op("dve", lambda e, s_=s_: e.tensor_copy(out=yo[i][:, s_, :], in_=py[:].rearrange("p a n -> p (a n)")), reads=[py], accum=[yo[i]])
                    S.dma("sp", "yo%d" % i, lambda e: e.dma_start(out=ybuf[bi * BS:(bi + 1) * BS, :].rearrange("(s p) d -> p s d", p=128), in_=yo[i][:]),
                          reads=[yo[i]])

                load_blk(0)
                if NBLK > 1:
                    load_blk(1)
                stX(0)
                for bi in range(NBLK):
                    if bi + 2 < NBLK:
                        load_blk(bi + 2)
                    stH(bi)
                    if bi + 1 < NBLK:
                        stX(bi + 1)
                    if bi >= 1:
                        stY(bi - 1)
                stY(NBLK - 1)
            S.barrier()

        def phase5():
            with contextlib.ExitStack() as st:
                gt2 = [load_mod_row(st, "gt2_%d" % bb, bb, 5) for bb in range(NB)]
                ya = [sb(st, "ya%d" % i, [128, D], BF16) for i in range(2)]
                yb = [sb(st, "yb%d" % i, [128, D], BF16) for i in range(2)]
                xl = [sb(st, "xl%d" % i, [128, D], F32) for i in range(2)]
                ma = [sb(st, "ma%d" % i, [128, D], F32) for i in range(2)]
                mb = [sb(st, "mb%d" % i, [128, D], F32) for i in range(2)]
                oo_ = [sb(st, "oo5_%d" % i, [128, D], F32) for i in range(2)]

                def load5(tt):
                    i = tt % 2
                    S.dma("pool", "ya%d" % i, lambda e: e.indirect_dma_start(
                        out=ya[i][:], out_offset=None, in_=ybuf[:, :], in_offset=bass.IndirectOffsetOnAxis(ap=dest_i[:, tt, 0:1], axis=0)),
                        reads=[dest_i], writes=[ya[i]])
                    S.dma("pool", "yb%d" % i, lambda e: e.indirect_dma_start(
                        out=yb[i][:], out_offset=None, in_=ybuf[:, :], in_offset=bass.IndirectOffsetOnAxis(ap=dest_i[:, tt, 1:2], axis=0)),
                        reads=[dest_i], writes=[yb[i]])
                    S.dma("sp", "xl%d" % i, lambda e: e.dma_start(out=xl[i][:], in_=x1s[tt * 128:(tt + 1) * 128, :]), writes=[xl[i]])

                load5(0)
                for tt in range(NTT):
                    i = tt % 2
                    bb = tt // NT
                    t = tt % NT
                    if tt + 1 < NTT:
                        load5(tt + 1)
                    S.op("dve", lambda e: e.tensor_scalar(out=ma[i][:], in0=ya[i][:], scalar1=wt_all[:, tt, 0:1], scalar2=None, op0=ALU.mult),
                         reads=[ya[i], wt_all], writes=[ma[i]])
                    S.op("dve", lambda e: e.scalar_tensor_tensor(out=mb[i][:], in0=yb[i][:], scalar=wt_all[:, tt, 1:2], in1=ma[i][:],
                                                                 op0=ALU.mult, op1=ALU.add), reads=[yb[i], wt_all, ma[i]], writes=[mb[i]])
                    S.op("dve", lambda e: e.tensor_tensor(out=ma[i][:], in0=mb[i][:], in1=gt2[bb][:], op=ALU.mult), reads=[mb[i], gt2[bb]], writes=[ma[i]])
                    S.op("pool", lambda e: e.tensor_tensor(out=oo_[i][:], in0=ma[i][:], in1=xl[i][:], op=ALU.add), reads=[ma[i], xl[i]], writes=[oo_[i]])
                    S.dma("sp", "oo5_%d" % i, lambda e: e.dma_start(out=out[bb, t * 128:(t + 1) * 128, :], in_=oo_[i][:]), reads=[oo_[i]])
            S.barrier()

        for b in range(NB):
            phase1(b)
            if upto >= 2:
                with contextlib.ExitStack() as bst:
                    o_all = sb(bst, "o_all", [128, NT, D], BF16)
                    phase2(b, o_all)
                    if upto >= 3:
                        phase3(b, o_all)
        if upto >= 4:
            phase3b()
            phase4()
        if upto >= 5:
            phase5()
        S.barrier()
        S.final_wait()
    print("instructions:", S.n_ins, "dma sems:", len(S.dsem), "eng sems:", len(S.all_sems))
    return nc


_INVF = (10000.0 ** (-np.arange(0, 64, 2, dtype=np.float32) / np.float32(64))).astype(np.float32).reshape(1, 32)


def make_in_map(inp, core, NB):
    sl = slice(core * NB, (core + 1) * NB)
    f = lambda a: np.ascontiguousarray(a)
    m = {
        "x": f(inp["x"][sl]), "c": f(inp["c"][sl]), "positions": f(inp["positions"][sl]).astype(np.int32),
        "w_ada": f(inp["w_ada"][0]), "b_ada": f(inp["b_ada"][0:1]), "g_norm1": f(inp["g_norm1"][0:1]),
        "w_in": f(inp["w_in"][0]), "b_f": f(inp["b_f"][0:1]),
        "g_q_fox": f(inp["g_q_fox"][0:1]), "g_k_fox": f(inp["g_k_fox"][0:1]),
        "g_q_diff": f(inp["g_q_diff"][0:1]), "g_k_diff": f(inp["g_k_diff"][0:1]),
        "lam_q1": f(inp["lam_q1"][0:1]), "lam_k1": f(inp["lam_k1"][0:1]),
        "lam_q2": f(inp["lam_q2"][0:1]), "lam_k2": f(inp["lam_k2"][0:1]),
        "g_subln": f(inp["g_subln"][0:1]),
        "w_proj_fox": f(inp["w_proj_fox"][0]), "w_proj_diff": f(inp["w_proj_diff"][0]), "w_out": f(inp["w_out"][0]),
        "g_norm2": f(inp["g_norm2"][0:1]),
        "w_router_group": f(inp["w_router_group"][0]), "b_router_group": f(inp["b_router_group"][0:1]),
        "w_router_expert": f(inp["w_router_expert"][0]), "b_router_expert": f(inp["b_router_expert"][0:1]),
        "w1": f(inp["w1"][0]).reshape(NEXP * D, DEXP), "w3": f(inp["w3"][0]).reshape(NEXP * D, DEXP),
        "w2": f(inp["w2"][0]).reshape(NEXP * DEXP, D),
        "invf": _INVF,
    }
    return m


def kernel(**inputs):
    inp = {k: np.asarray(v) for k, v in inputs.items()}
    B, TT, _ = inp["x"].shape
    NB = B // N_CORES
    nc = build_nc(NB, TT)
    in_maps = [make_in_map(inp, c, NB) for c in range(N_CORES)]
    res = run_bass_kernel_spmd(nc, in_maps, core_ids=list(range(N_CORES)))
    return np.concatenate([np.asarray(r["out"]) for r in res.results], axis=0).astype(np.float32)
```

```python
import contextlib
import math
import numpy as np
import concourse.bass as bass
import concourse.mybir as mybir
from concourse.bass_utils import run_bass_kernel_spmd

F32 = mybir.dt.float32
BF16 = mybir.dt.bfloat16
I32 = mybir.dt.int32
U32 = mybir.dt.uint32
AF = mybir.ActivationFunctionType
ALU = mybir.AluOpType
AX = mybir.AxisListType

D = 1024
KC = 8
HD = 64
IN_COLS = 5128
C_FQ, C_FK, C_FV, C_FF, C_DQ, C_DK, C_DV, C_G = 0, 512, 1024, 1536, 1544, 2056, 2568, 3080
NEXP = 32
DEXP = 512
EPS = 1e-6
BS = 256
LAM0 = 0.8 - 0.6 * math.exp(-0.3 * 0)
N_CORES = 8


class Res:
    __slots__ = ("name", "w", "r")

    def __init__(self, name):
        self.name = name
        self.w = {}
        self.r = {}


class T:
    def __init__(self, t, name):
        self.t = t
        self.res = Res(name)

    def __getitem__(self, k):
        return self.t[k]


class Sched:
    EPOCH = 30000

    def __init__(self, nc, es):
        self.nc = nc
        self.es = es
        self.eng = {"pe": nc.tensor, "act": nc.scalar, "dve": nc.vector, "pool": nc.gpsimd, "sp": nc.sync}
        self.sem = {}
        self.cnt = {}
        self.seen = {k: {} for k in self.eng}
        self.all_sems = {}
        for k in self.eng:
            self._new_eng_sem(k)
        self.dsem = {}
        self.dcnt = {}
        self.n_ins = 0
        self.bg_keys = set()

    def _new_eng_sem(self, k):
        s = self.es.enter_context(self.nc.semaphore("s_%s_%d" % (k, len(self.all_sems))))
        self.sem[k] = s
        self.cnt[k] = 0
        self.all_sems[s] = 0

    def _res(self, lst):
        return [x.res if isinstance(x, T) else x for x in lst]

    def _need(self, reads, writes, accum):
        need = {}
        for r in reads:
            for s, v in r.w.items():
                if need.get(s, 0) < v:
                    need[s] = v
        for w in writes:
            for dct in (w.w, w.r):
                for s, v in dct.items():
                    if need.get(s, 0) < v:
                        need[s] = v
        for w in accum:
            for s, v in w.r.items():
                if need.get(s, 0) < v:
                    need[s] = v
        return need

    def _emit_waits(self, e, need):
        eng = self.eng[e]
        seen = self.seen[e]
        waits = [(s, v) for s, v in need.items() if seen.get(s, 0) < v]
        for s, v in waits:
            seen[s] = v
        return eng, waits

    def _record(self, ev_s, ev_v, reads, writes, accum):
        self.all_sems[ev_s] = ev_v
        for r in reads:
            if r.r.get(ev_s, 0) < ev_v:
                r.r[ev_s] = ev_v
        for w in writes:
            w.w = {ev_s: ev_v}
            w.r = {}
        for w in accum:
            if w.w.get(ev_s, 0) < ev_v:
                w.w[ev_s] = ev_v

    def op(self, e, fn, reads=(), writes=(), accum=()):
        reads, writes, accum = self._res(reads), self._res(writes), self._res(accum)
        if self.cnt[e] >= self.EPOCH:
            self._new_eng_sem(e)
        need = self._need(reads, writes, accum)
        eng, waits = self._emit_waits(e, need)
        for s, v in waits:
            eng.wait_ge(s, v)
        ins = fn(eng)
        self.cnt[e] += 1
        ins.then_inc(self.sem[e], 1)
        self._record(self.sem[e], self.cnt[e], reads, writes, accum)
        self.n_ins += 1
        return ins

    def dma(self, q, key, fn, reads=(), writes=(), accum=()):
        reads, writes, accum = self._res(reads), self._res(writes), self._res(accum)
        if key not in self.dsem:
            self.dsem[key] = self.es.enter_context(self.nc.semaphore("d_" + key))
            self.dcnt[key] = 0
        need = self._need(reads, writes, accum)
        eng, waits = self._emit_waits(q, need)
        for s, v in waits:
            eng.wait_ge(s, v)
        ins = fn(eng)
        self.dcnt[key] += 16
        s = self.dsem[key]
        ins.then_inc(s, 16)
        self._record(s, self.dcnt[key], reads, writes, accum)
        self.n_ins += 1
        return ins

    def barrier(self):
        skip = {self.dsem[k] for k in self.bg_keys if k in self.dsem}
        allv = {s: v for s, v in self.all_sems.items() if s not in skip}
        for e in self.eng:
            eng, waits = self._emit_waits(e, allv)
            for s, v in waits:
                if v > 0:
                    eng.wait_ge(s, v)

    def final_wait(self):
        eng, waits = self._emit_waits("sp", dict(self.all_sems))
        for s, v in waits:
            if v > 0:
                eng.wait_ge(s, v)


def build_nc(NB, TT, dbg=False, upto=5):
    NT = TT // 128
    NG = TT // 512
    NTOK = NB * TT
    NTT = NTOK // 128
    NBLK = (NTOK * 2) // BS + NEXP
    PROWS = NBLK * BS
    nc = bass.Bass("TRN2", target_bir_lowering=False)

    def din(name, shape, dt=F32):
        return nc.dram_tensor(name, list(shape), dt, kind="ExternalInput").ap()

    DBG_OUT = ("modv", "qTd", "kTd", "qTf", "x1s", "h2s")

    def dscr(name, shape, dt):
        return nc.dram_tensor(name, list(shape), dt, kind=("ExternalOutput" if (dbg and name in DBG_OUT) else "Internal")).ap()

    x = din("x", [NB, TT, D])
    cin = din("c", [NB, D])
    pos = din("positions", [NB, TT], I32)
    w_ada = din("w_ada", [D, 6 * D])
    b_ada = din("b_ada", [1, 6 * D])
    g_norm1 = din("g_norm1", [1, D])
    w_in = din("w_in", [D, IN_COLS])
    b_f = din("b_f", [1, 8])
    g_q_fox = din("g_q_fox", [1, HD])
    g_k_fox = din("g_k_fox", [1, HD])
    g_q_diff = din("g_q_diff", [1, HD])
    g_k_diff = din("g_k_diff", [1, HD])
    lam_q1 = din("lam_q1", [1, HD])
    lam_k1 = din("lam_k1", [1, HD])
    lam_q2 = din("lam_q2", [1, HD])
    lam_k2 = din("lam_k2", [1, HD])
    g_subln = din("g_subln", [1, 128])
    w_pf = din("w_proj_fox", [512, D])
    w_pd = din("w_proj_diff", [512, D])
    w_out = din("w_out", [D, D])
    g_norm2 = din("g_norm2", [1, D])
    w_rg = din("w_router_group", [D, 4])
    b_rg = din("b_router_group", [1, 4])
    w_re = din("w_router_expert", [D, 32])
    b_re = din("b_router_expert", [1, 32])
    w1 = din("w1", [NEXP * D, DEXP])
    w3 = din("w3", [NEXP * D, DEXP])
    w2 = din("w2", [NEXP * DEXP, D])
    invf = din("invf", [1, 32])
    out = nc.dram_tensor("out", [NB, TT, D], F32, kind="ExternalOutput").ap()

    modv = dscr("modv", [NB, 6, D], F32)
    qTf = dscr("qTf", [NB, 8, 65, TT], BF16)
    kTf = dscr("kTf", [NB, 8, 65, TT], BF16)
    qTd = dscr("qTd", [NB, 8, 64, TT], BF16)
    kTd = dscr("kTd", [NB, 8, 64, TT], BF16)
    vF = dscr("vF", [NB, TT, 8 * 65], BF16)
    vD = dscr("vD", [NB, TT, 4 * 129], BF16)
    gl = dscr("gl", [NB, TT, 2048], BF16)
    x1s = dscr("x1s", [NTOK, D], F32)
    h2s = dscr("h2s", [NTOK, D], BF16)
    w1c = dscr("w1c", [NEXP * D // 4, 2048], BF16)
    w3c = dscr("w3c", [NEXP * D // 4, 2048], BF16)
    w2c = dscr("w2c", [NEXP * DEXP // 2, 2048], BF16)
    xbuf = dscr("xbuf", [PROWS, D], BF16)
    ybuf = dscr("ybuf", [PROWS, D], BF16)
    dbg_t = {}
    if dbg:
        dbg_t["oc"] = nc.dram_tensor("dbg_oc", [NB, TT, D], BF16, kind="ExternalOutput").ap()
        dbg_t["rt"] = nc.dram_tensor("dbg_rt", [128, NTT, 8], F32, kind="ExternalOutput").ap()
        dbg_t["be"] = nc.dram_tensor("dbg_be", [128, NBLK + 64], F32, kind="ExternalOutput").ap()

    es = contextlib.ExitStack()
    with es:
        S = Sched(nc, es)

        uid = [0]

        def sb(stack, name, shape, dt):
            uid[0] += 1
            name = "%s_%d" % (name, uid[0])
            return T(stack.enter_context(nc.sbuf_tensor(name, list(shape), dt)), name)

        def ps(stack, name, shape, dt):
            uid[0] += 1
            name = "%s_%d" % (name, uid[0])
            return T(stack.enter_context(nc.psum_tensor(name, list(shape), dt)), name)

        ident_b = sb(es, "ident_b", [128, 128], BF16)
        ident_f = sb(es, "ident_f", [128, 128], F32)
        tri_i = sb(es, "tri_i", [128, 128], F32)
        tri_s = sb(es, "tri_s", [128, 128], F32)
        ones_f = sb(es, "ones_f", [128, 128], F32)
        G4 = sb(es, "G4", [128, 4, HD], F32)
        bf_rep = sb(es, "bf_rep", [128, 8], F32)
        gsub = sb(es, "gsub", [128, 128], F32)
        nlam = sb(es, "nlam", [128, 1], F32)
        invf_rep = sb(es, "invf_rep", [128, 32], F32)
        ncum = sb(es, "ncum", [128, NB, NT, 8], F32)
        eid_all = sb(es, "eid_all", [128, NTT, 2], F32)
        rk_all = sb(es, "rk_all", [128, NTT, 2], F32)
        wt_all = sb(es, "wt_all", [128, NTT, 2], F32)
        dest_i = sb(es, "dest_i", [128, NTT, 2], U32)
        ecarry = sb(es, "ecarry", [128, 32], F32)
        iota32 = sb(es, "iota32", [128, 32], F32)
        ztile = sb(es, "ztile", [128, D], BF16)
        xz = Res("xbuf_zero")
        S.bg_keys.add("ztile")
        blkidx = {"idx1": sb(es, "idx1", [128, NBLK, 2], U32)}

        def bcast_rows(ap, n):
            return ap.partition_broadcast(n)

        def setup():
            with contextlib.ExitStack() as st:
                zf = sb(st, "zf", [128, 128], F32)
                S.op("pool", lambda e: e.memset(zf[:], 0.0), writes=[zf])
                S.op("pool", lambda e: e.memset(ones_f[:], 1.0), writes=[ones_f])
                S.op("pool", lambda e: e.affine_select(out=ident_f[:], in_=zf[:], pattern=[[-1, 128]],
                                                       compare_op=ALU.not_equal, fill=1.0, base=0,
                                                       channel_multiplier=1), reads=[zf], writes=[ident_f])
                S.op("pool", lambda e: e.affine_select(out=tri_i[:], in_=ones_f[:], pattern=[[1, 128]],
                                                       compare_op=ALU.is_ge, fill=0.0, base=0,
                                                       channel_multiplier=-1), reads=[ones_f], writes=[tri_i])
                S.op("pool", lambda e: e.affine_select(out=tri_s[:], in_=ones_f[:], pattern=[[1, 128]],
                                                       compare_op=ALU.is_ge, fill=0.0, base=-1,
                                                       channel_multiplier=-1), reads=[ones_f], writes=[tri_s])
                S.op("dve", lambda e: e.tensor_copy(out=ident_b[:], in_=ident_f[:]), reads=[ident_f], writes=[ident_b])
                S.op("pool", lambda e: e.memset(ecarry[:], 0.0), writes=[ecarry])
                S.op("pool", lambda e: e.memset(ztile[:], 0.0), writes=[ztile])
                xbv = xbuf.rearrange("(r p) d -> p r d", p=128)
                for r0 in range(PROWS // 128):
                    S.dma("sp", "ztile", lambda e, r0=r0: e.dma_start(out=xbv[:, r0, :], in_=ztile[:]), reads=[ztile], accum=[xz])
                io_i = sb(st, "io_i", [128, 32], I32)
                S.op("pool", lambda e: e.iota(io_i[:], pattern=[[1, 32]], base=0, channel_multiplier=0), writes=[io_i])
                S.op("dve", lambda e: e.tensor_copy(out=iota32[:], in_=io_i[:]), reads=[io_i], writes=[iota32])
                for i, (g, sc) in enumerate([(g_q_fox, 0.125), (g_k_fox, 1.0), (g_q_diff, 0.125), (g_k_diff, 1.0)]):
                    S.dma("sp", "G4", lambda e, g=g, i=i: e.dma_start(out=G4[:, i, :], in_=bcast_rows(g[0:1, :], 128)),
                          accum=[G4])
                S.dma("sp", "bf", lambda e: e.dma_start(out=bf_rep[:], in_=bcast_rows(b_f[0:1, :], 128)), writes=[bf_rep])
                S.dma("sp", "gsub", lambda e: e.dma_start(out=gsub[:], in_=bcast_rows(g_subln[0:1, :], 128)), writes=[gsub])
                S.dma("sp", "invf", lambda e: e.dma_start(out=invf_rep[:], in_=bcast_rows(invf[0:1, :], 128)), writes=[invf_rep])
                S.op("dve", lambda e: e.tensor_scalar(out=G4[:, 0, :], in0=G4[:, 0, :], scalar1=0.125, scalar2=None,
                                                      op0=ALU.mult), reads=[G4], writes=[G4])
                S.op("dve", lambda e: e.tensor_scalar(out=G4[:, 2, :], in0=G4[:, 2, :], scalar1=0.125, scalar2=None,
                                                      op0=ALU.mult), reads=[G4], writes=[G4])
                S.op("dve", lambda e: e.tensor_scalar(out=gsub[:], in0=gsub[:], scalar1=1.0 - LAM0, scalar2=None,
                                                      op0=ALU.mult), reads=[gsub], writes=[gsub])
                lv = sb(st, "lv", [128, 4, HD], F32)
                for i, g in enumerate([lam_q1, lam_k1, lam_q2, lam_k2]):
                    S.dma("sp", "lv", lambda e, g=g, i=i: e.dma_start(out=lv[:, i, :], in_=bcast_rows(g[0:1, :], 128)),
                          accum=[lv])
                lp = sb(st, "lp", [128, 2, HD], F32)
                ls = sb(st, "ls", [128, 2], F32)
                S.op("dve", lambda e: e.tensor_tensor(out=lp[:, 0, :], in0=lv[:, 0, :], in1=lv[:, 1, :], op=ALU.mult),
                     reads=[lv], writes=[lp])
                S.op("dve", lambda e: e.tensor_tensor(out=lp[:, 1, :], in0=lv[:, 2, :], in1=lv[:, 3, :], op=ALU.mult),
                     reads=[lv, lp], accum=[lp])
                S.op("dve", lambda e: e.tensor_reduce(out=ls[:], in_=lp[:], axis=AX.X, op=ALU.add), reads=[lp], writes=[ls])
                S.op("act", lambda e: e.activation(out=ls[:], in_=ls[:], func=AF.Exp), reads=[ls], writes=[ls])
                S.op("dve", lambda e: e.scalar_tensor_tensor(out=nlam[:], in0=ls[:, 1:2], scalar=-LAM0, in1=ls[:, 0:1],
                                                             op0=ALU.add, op1=ALU.subtract), reads=[ls], writes=[nlam])
                cc = sb(st, "cc", [128, 8, NB], F32)
                with nc.allow_non_contiguous_dma(reason="tiny one-time transposed load of c"):
                    for bb in range(NB):
                        S.dma("sp", "cc", lambda e, bb=bb: e.dma_start(out=cc[:, :, bb:bb + 1],
                                                                  in_=cin[bb:bb + 1, :].rearrange("b (kc k) -> k kc b", k=128)),
                              accum=[cc])
                with contextlib.ExitStack() as pst:
                    pc = cc
                    pm = ps(pst, "pm", [128, 512], F32)
                    cact = sb(st, "cact", [128, 8, NB], F32)
                    S.op("act", lambda e: e.activation(out=cact[:], in_=pc[:], func=AF.Silu), reads=[pc], writes=[cact])
                    bad = sb(st, "bad", [128, 6 * D], F32)
                    S.dma("sp", "bad", lambda e: e.dma_start(out=bad[:], in_=bcast_rows(b_ada[0:1, :], 128)), writes=[bad])
                    gg = sb(st, "gg", [128, 2, D], F32)
                    S.dma("sp", "gg", lambda e: e.dma_start(out=gg[:, 0, :], in_=bcast_rows(g_norm1[0:1, :], 128)), accum=[gg])
                    S.dma("sp", "gg", lambda e: e.dma_start(out=gg[:, 1, :], in_=bcast_rows(g_norm2[0:1, :], 128)), accum=[gg])
                    was = [sb(st, "wa%d" % i, [128, 8, 512], BF16) for i in range(2)]
                    creps = [sb(st, "crep%d" % i, [128, 8, 128], BF16) for i in range(NB)]
                    mods = [sb(st, "mod%d" % i, [128, 6 * D], F32) for i in range(NB)]
                    for bb in range(NB):
                        S.op("dve", lambda e, bb=bb: e.tensor_copy(out=creps[bb][:], in_=cact[:, :, bb:bb + 1].to_broadcast([128, 8, 128])),
                             reads=[cact], writes=[creps[bb]])
                    for j in range(12):
                        wa = was[j % 2]
                        S.dma("pool", "wa%d" % (j % 2),
                              lambda e, wa=wa, j=j: e.dma_start(
                                  out=wa[:], in_=w_ada[:, j * 512:(j + 1) * 512].rearrange("(kc k) n -> k kc n", k=128)),
                              writes=[wa])
                        for bb in range(NB):
                            for kc in range(KC):
                                S.op("pe", lambda e, wa=wa, kc=kc, bb=bb: e.matmul(pm[:], lhsT=creps[bb][:, kc, :], rhs=wa[:, kc, :],
                                                                                   start=(kc == 0), stop=(kc == KC - 1)),
                                     reads=[creps[bb], wa], accum=[pm])
                            S.op("dve", lambda e, j=j, bb=bb: e.tensor_tensor(out=mods[bb][:, j * 512:(j + 1) * 512], in0=pm[:],
                                                                              in1=bad[:, j * 512:(j + 1) * 512], op=ALU.add),
                                 reads=[pm, bad], accum=[mods[bb]])
                    for bb in range(NB):
                        mod = mods[bb]
                        for (dst, src, kind) in [(0, 1, "A1"), (1, 0, "c"), (2, 2, "c"), (3, 4, "A2"), (4, 3, "c"), (5, 5, "c")]:
                            if kind == "c":
                                S.dma("sp", "mod%d" % bb, lambda e, dst=dst, src=src, bb=bb, mod=mod: e.dma_start(
                                    out=modv[bb, dst:dst + 1, :], in_=mod[0:1, src * D:(src + 1) * D]), reads=[mod])
                            else:
                                gi = 0 if kind == "A1" else 1
                                S.op("dve", lambda e, src=src, gi=gi, mod=mod: e.scalar_tensor_tensor(
                                    out=mod[:, src * D:(src + 1) * D], in0=mod[:, src * D:(src + 1) * D], scalar=1.0, in1=gg[:, gi, :],
                                    op0=ALU.add, op1=ALU.mult), reads=[mod, gg], writes=[mod])
                                S.dma("sp", "mod%d" % bb, lambda e, dst=dst, src=src, bb=bb, mod=mod: e.dma_start(
                                    out=modv[bb, dst:dst + 1, :], in_=mod[0:1, src * D:(src + 1) * D]), reads=[mod])
                S.barrier()

        setup()

        modv_res = Res("modv")

        def load_mod_row(stack, name, b, i):
            t = sb(stack, name, [128, D], F32)
            S.dma("sp", name, lambda e: e.dma_start(out=t[:], in_=bcast_rows(modv[b, i:i + 1, :], 128)), writes=[t])
            return t

        def phase1(b):
            with contextlib.ExitStack() as st:
                win = sb(st, "win", [128, KC, IN_COLS], BF16)
                for kc in range(KC):
                    for c0 in range(0, IN_COLS, 1024):
                        c1 = min(c0 + 1024, IN_COLS)
                        S.dma("pool", "win", lambda e, kc=kc, c0=c0, c1=c1: e.dma_start(
                            out=win[:, kc, c0:c1], in_=w_in[kc * 128:(kc + 1) * 128, c0:c1]), accum=[win])
                A1 = load_mod_row(st, "A1", b, 0)
                B1 = load_mod_row(st, "B1", b, 1)
                cosT = sb(st, "cosT", [128, NT, 32], F32)
                sinT = sb(st, "sinT", [128, NT, 32], F32)
                with contextlib.ExitStack() as st2:
                    pi_ = sb(st2, "pi_", [128, NT], I32)
                    with nc.allow_non_contiguous_dma(reason="one-time transposed load of positions"):
                        S.dma("sp", "pi", lambda e: e.dma_start(out=pi_[:], in_=pos[b, :].rearrange("(t p) -> p t", p=128)), writes=[pi_])
                    posf = sb(st2, "posf", [128, NT], F32)
                    S.op("dve", lambda e: e.tensor_copy(out=posf[:], in_=pi_[:]), reads=[pi_], writes=[posf])
                    ang = sb(st2, "ang", [128, NT, 32], F32)
                    S.op("dve", lambda e: e.tensor_tensor(out=ang[:], in0=posf[:].unsqueeze(2).to_broadcast([128, NT, 32]),
                                                          in1=invf_rep[:].unsqueeze(1).to_broadcast([128, NT, 32]), op=ALU.mult),
                         reads=[posf, invf_rep], writes=[ang])
                    tq = sb(st2, "tq", [128, NT, 32], F32)
                    nq = sb(st2, "nq", [128, NT, 32], F32)
                    MAGIC = 12582912.0
                    C1 = 6.28125
                    C2 = 2.0 * math.pi - 6.28125
                    for (dst, shift) in [(sinT, 0.0), (cosT, math.pi / 2)]:
                        S.op("dve", lambda e, shift=shift: e.tensor_scalar(out=tq[:], in0=ang[:], scalar1=shift, scalar2=None,
                                                                          op0=ALU.add), reads=[ang], writes=[tq])
                        S.op("dve", lambda e: e.tensor_scalar(out=nq[:], in0=tq[:], scalar1=1.0 / (2 * math.pi), scalar2=MAGIC,
                                                              op0=ALU.mult, op1=ALU.add), reads=[tq], writes=[nq])
                        S.op("dve", lambda e: e.tensor_scalar(out=nq[:], in0=nq[:], scalar1=-MAGIC, scalar2=None,
                                                              op0=ALU.add), reads=[nq], writes=[nq])
                        S.op("dve", lambda e: e.scalar_tensor_tensor(out=tq[:], in0=nq[:], scalar=-C1, in1=tq[:],
                                                                     op0=ALU.mult, op1=ALU.add), reads=[nq, tq], writes=[tq])
                        S.op("dve", lambda e: e.scalar_tensor_tensor(out=tq[:], in0=nq[:], scalar=-C2, in1=tq[:],
                                                                     op0=ALU.mult, op1=ALU.add), reads=[nq, tq], writes=[tq])
                        S.op("dve", lambda e: e.tensor_scalar(out=tq[:], in0=tq[:], scalar1=3.1415925, scalar2=-3.1415925,
                                                              op0=ALU.min, op1=ALU.max), reads=[tq], writes=[tq])
                        S.op("act", lambda e, dst=dst: e.activation(out=dst[:], in_=tq[:], func=AF.Sin), reads=[tq], writes=[dst])
                S.barrier()
                NBUF = 2
                xt = [sb(st, "xt%d" % i, [128, D], F32) for i in range(NBUF)]
                tmp = [sb(st, "tmp%d" % i, [128, D], F32) for i in range(NBUF)]
                hb = [sb(st, "hb%d" % i, [128, D], BF16) for i in range(NBUF)]
                hT = [sb(st, "hT%d" % i, [128, KC, 128], BF16) for i in range(NBUF)]
                sq = sb(st, "sq", [128, D], F32)
                ss = [sb(st, "ss%d" % i, [128, 4], F32) for i in range(NBUF)]
                ms8 = [sb(st, "ms8_%d" % i, [128, 8], F32) for i in range(4)]
                qn = [sb(st, "qn%d" % i, [128, 8, HD], F32) for i in range(2)]
                qg = [sb(st, "qg%d" % i, [128, 8, HD], F32) for i in range(2)]
                ra = [sb(st, "ra%d" % i, [128, 8, 32], F32) for i in range(4)]
                QKF = [sb(st, "QKF%d" % i, [128, 16, 66], BF16) for i in range(NBUF)]
                QKD = [sb(st, "QKD%d" % i, [128, 16, HD], BF16) for i in range(NBUF)]
                QTs = [sb(st, "QTs%d" % i, [65, 32, 128], BF16) for i in range(NBUF)]
                VF = [sb(st, "VF%d" % i, [128, 8, 65], BF16) for i in range(NBUF)]
                VD = [sb(st, "VD%d" % i, [128, 4, 129], BF16) for i in range(NBUF)]
                GT = [sb(st, "GT%d" % i, [128, 2048], BF16) for i in range(NBUF)]
                fz = [sb(st, "fz%d" % i, [128, 8], F32) for i in range(3)]
                carry = sb(st, "carry", [128, 8], F32)
                S.op("pool", lambda e: e.memset(carry[:], 0.0), writes=[carry])
                for i in range(NBUF):
                    S.op("pool", lambda e, i=i: e.memset(QKF[i][:], 0.0), writes=[QKF[i]])
                    S.op("pool", lambda e, i=i: e.memset(QKF[i][:, 8:16, 64:65], 1.0), reads=[QKF[i]], accum=[QKF[i]])
                    S.op("pool", lambda e, i=i: e.memset(VF[i][:, :, 64:65], 1.0), writes=[VF[i]])
                    S.op("pool", lambda e, i=i: e.memset(VD[i][:, :, 128:129], 1.0), writes=[VD[i]])
                pT = ps(st, "pT", [128, KC, 128], BF16)
                zb = [ps(st, "zb%d" % i, [128, 512], F32) for i in range(4)]
                psm = ps(st, "psm", [128, 16], F32)
                qtp = ps(st, "qtp", [65, 16, 128], BF16)
                zbi = [0]

                def zgroup(hTt, col0, ncols):
                    z = zb[zbi[0] % 4]
                    zbi[0] += 1
                    for kc in range(KC):
                        S.op("pe", lambda e, kc=kc: e.matmul(z[:, 0:ncols], lhsT=hTt[:, kc, :], rhs=win[:, kc, col0:col0 + ncols],
                                                             start=(kc == 0), stop=(kc == KC - 1)),
                             reads=[hTt, win], accum=[z])
                    return z

                def headnorm(z, gi, mi, dst_fn, rope_t=None):
                    m = ms8[mi]
                    S.op("act", lambda e: e.activation(out=sq[:, 0:512], in_=z[:], func=AF.Square), reads=[z], writes=[sq])
                    S.op("dve", lambda e: e.tensor_reduce(out=m[:], in_=sq[:, 0:512].rearrange("p (h d) -> p h d", d=HD),
                                                          axis=AX.X, op=ALU.add), reads=[sq], writes=[m])
                    S.op("act", lambda e: e.activation(out=m[:], in_=m[:], func=AF.Ln, scale=1.0 / HD, bias=EPS), reads=[m], writes=[m])
                    S.op("act", lambda e: e.activation(out=m[:], in_=m[:], func=AF.Exp, scale=-0.5), reads=[m], writes=[m])
                    q1 = qn[mi % 2]
                    S.op("dve", lambda e: e.tensor_tensor(out=q1[:], in0=z[:].rearrange("p (h d) -> p h d", d=HD),
                                                          in1=m[:].unsqueeze(2).to_broadcast([128, 8, HD]), op=ALU.mult),
                         reads=[z, m], writes=[q1])
                    gb = G4[:, gi, :].unsqueeze(1).to_broadcast([128, 8, HD])
                    if rope_t is None:
                        S.op("pool", lambda e: e.tensor_tensor(out=dst_fn(0, HD), in0=q1[:], in1=gb, op=ALU.mult),
                             reads=[q1, G4], accum=[dst_fn.tile])
                    else:
                        q2 = qg[mi % 2]
                        S.op("pool", lambda e: e.tensor_tensor(out=q2[:], in0=q1[:], in1=gb, op=ALU.mult),
                             reads=[q1, G4], writes=[q2])
                        cb = cosT[:, rope_t, :].unsqueeze(1).to_broadcast([128, 8, 32])
                        sbb = sinT[:, rope_t, :].unsqueeze(1).to_broadcast([128, 8, 32])
                        x1 = q2[:, :, 0:32]
                        x2 = q2[:, :, 32:64]
                        S.op("dve", lambda e: e.tensor_tensor(out=ra[0][:], in0=x1, in1=cb, op=ALU.mult), reads=[q2, cosT], writes=[ra[0]])
                        S.op("pool", lambda e: e.tensor_tensor(out=ra[1][:], in0=x2, in1=sbb, op=ALU.mult), reads=[q2, sinT], writes=[ra[1]])
                        S.op("dve", lambda e: e.tensor_tensor(out=ra[2][:], in0=x2, in1=cb, op=ALU.mult), reads=[q2, cosT], writes=[ra[2]])
                        S.op("pool", lambda e: e.tensor_tensor(out=ra[3][:], in0=x1, in1=sbb, op=ALU.mult), reads=[q2, sinT], writes=[ra[3]])
                        S.op("dve", lambda e: e.tensor_tensor(out=dst_fn(0, 32), in0=ra[0][:], in1=ra[1][:], op=ALU.subtract),
                             reads=[ra[0], ra[1]], accum=[dst_fn.tile])
                        S.op("pool", lambda e: e.tensor_tensor(out=dst_fn(32, 64), in0=ra[2][:], in1=ra[3][:], op=ALU.add),
                             reads=[ra[2], ra[3]], accum=[dst_fn.tile])

                def mkdst(tile, h0):
                    def f(a, bb):
                        return tile[:, h0:h0 + 8, a:bb]
                    f.tile = tile
                    return f

                def load_x(t):
                    i = t % NBUF
                    S.dma("sp", "xt%d" % i, lambda e: e.dma_start(out=xt[i][:], in_=x[b, t * 128:(t + 1) * 128, :]), writes=[xt[i]])

                def pre_a(t):
                    i = t % NBUF
                    ssi = ss[i]
                    S.op("act", lambda e: e.activation(out=sq[:], in_=xt[i][:], func=AF.Square, accum_out=ssi[:, 0:1]),
                         reads=[xt[i]], writes=[sq, ssi])
                    S.op("act", lambda e: e.activation(out=ssi[:, 1:2], in_=ssi[:, 0:1], func=AF.Ln, scale=1.0 / D, bias=EPS),
                         reads=[ssi], accum=[ssi])
                    S.op("act", lambda e: e.activation(out=ssi[:, 2:3], in_=ssi[:, 1:2], func=AF.Exp, scale=-0.5),
                         reads=[ssi], accum=[ssi])
                    S.op("dve", lambda e: e.scalar_tensor_tensor(out=tmp[i][:], in0=xt[i][:], scalar=ssi[:, 2:3], in1=A1[:],
                                                                 op0=ALU.mult, op1=ALU.mult), reads=[xt[i], ssi, A1], writes=[tmp[i]])
                    S.op("pool", lambda e: e.tensor_tensor(out=hb[i][:], in0=tmp[i][:], in1=B1[:], op=ALU.add),
                         reads=[tmp[i], B1], writes=[hb[i]])

                def pre_b(t):
                    i = t % NBUF
                    for kc in range(KC):
                        S.op("pe", lambda e, kc=kc: e.transpose(out=pT[:, kc, :], in_=hb[i][:, kc * 128:(kc + 1) * 128], identity=ident_b[:]),
                             reads=[hb[i], ident_b], accum=[pT])
                    S.op("act", lambda e: e.copy(out=hT[i][:], in_=pT[:]), reads=[pT], writes=[hT[i]])

                def zstage(t):
                    i = t % NBUF
                    z = zgroup(hT[i], C_FF, 8)
                    f0, f1, f2 = fz
                    S.op("dve", lambda e: e.tensor_tensor(out=f0[:], in0=z[:, 0:8], in1=bf_rep[:], op=ALU.add), reads=[z, bf_rep], writes=[f0])
                    S.op("act", lambda e: e.activation(out=f1[:], in_=f0[:], func=AF.Exp, scale=-1.0), reads=[f0], writes=[f1])
                    S.op("act", lambda e: e.activation(out=f2[:], in_=f1[:], func=AF.Ln, bias=1.0), reads=[f1], writes=[f2])
                    z = zgroup(hT[i], C_FQ, 512)
                    headnorm(z, 0, 0, mkdst(QKF[i], 0))
                    z = zgroup(hT[i], C_FK, 512)
                    headnorm(z, 1, 1, mkdst(QKF[i], 8))
                    z = zgroup(hT[i], C_FV, 512)
                    S.op("act", lambda e, z=z: e.copy(out=VF[i][:, :, 0:64], in_=z[:].rearrange("p (h d) -> p h d", d=HD)),
                         reads=[z], accum=[VF[i]])
                    S.op("pe", lambda e: e.matmul(psm[:, 0:8], lhsT=tri_i[:], rhs=f2[:], start=True, stop=True),
                         reads=[tri_i, f2], accum=[psm])
                    S.op("pe", lambda e: e.matmul(psm[:, 8:16], lhsT=ones_f[:], rhs=f2[:], start=False, stop=True, skip_group_check=True),
                         reads=[ones_f, f2], accum=[psm])
                    S.op("dve", lambda e: e.tensor_tensor(out=ncum[:, b, t, :], in0=psm[:, 0:8], in1=carry[:], op=ALU.add),
                         reads=[psm, carry], accum=[ncum])
                    S.op("dve", lambda e: e.tensor_tensor(out=carry[:], in0=psm[:, 8:16], in1=carry[:], op=ALU.add),
                         reads=[psm, carry], writes=[carry])
                    S.op("dve", lambda e: e.tensor_scalar(out=QKF[i][:, 0:8, 64:65], in0=ncum[:, b, t, :].unsqueeze(2), scalar1=-1.0,
                                                          scalar2=None, op0=ALU.mult), reads=[ncum], accum=[QKF[i]])
                    z = zgroup(hT[i], C_DQ, 512)
                    headnorm(z, 2, 2, mkdst(QKD[i], 0), rope_t=t)
                    z = zgroup(hT[i], C_DK, 512)
                    headnorm(z, 3, 3, mkdst(QKD[i], 8), rope_t=t)
                    z = zgroup(hT[i], C_DV, 512)
                    S.op("act", lambda e, z=z: e.copy(out=VD[i][:, :, 0:128], in_=z[:].rearrange("p (h d) -> p h d", d=128)),
                         reads=[z], accum=[VD[i]])
                    for gi in range(4):
                        z = zgroup(hT[i], C_G + gi * 512, 512)
                        if gi % 2 == 0:
                            S.op("act", lambda e, z=z, gi=gi: e.copy(out=GT[i][:, gi * 512:(gi + 1) * 512], in_=z[:]), reads=[z], accum=[GT[i]])
                        else:
                            S.op("dve", lambda e, z=z, gi=gi: e.tensor_copy(out=GT[i][:, gi * 512:(gi + 1) * 512], in_=z[:]), reads=[z], accum=[GT[i]])
                    for h in range(16):
                        S.op("pe", lambda e, h=h: e.transpose(out=qtp[0:65, h, :], in_=QKF[i][:, h, 0:65], identity=ident_b[:]),
                             reads=[QKF[i], ident_b], accum=[qtp])
                    S.op("act", lambda e: e.copy(out=QTs[i][0:65, 0:16, :], in_=qtp[0:65, :, :]), reads=[qtp], accum=[QTs[i]])
                    for h in range(16):
                        S.op("pe", lambda e, h=h: e.transpose(out=qtp[0:64, h, :], in_=QKD[i][:, h, :], identity=ident_b[:]),
                             reads=[QKD[i], ident_b], accum=[qtp])
                    S.op("dve", lambda e: e.tensor_copy(out=QTs[i][0:64, 16:32, :], in_=qtp[0:64, :, :]), reads=[qtp], accum=[QTs[i]])
                    cs = slice(t * 128, (t + 1) * 128)
                    S.dma("sp", "sQ%d" % i, lambda e: e.dma_start(out=qTf[b].rearrange("h r t -> r h t")[:, :, cs], in_=QTs[i][0:65, 0:8, :]), reads=[QTs[i]])
                    S.dma("sp", "sQ%d" % i, lambda e: e.dma_start(out=kTf[b].rearrange("h r t -> r h t")[:, :, cs], in_=QTs[i][0:65, 8:16, :]), reads=[QTs[i]])
                    S.dma("sp", "sQ%d" % i, lambda e: e.dma_start(out=qTd[b].rearrange("h r t -> r h t")[:, :, cs], in_=QTs[i][0:64, 16:24, :]), reads=[QTs[i]])
                    S.dma("sp", "sQ%d" % i, lambda e: e.dma_start(out=kTd[b].rearrange("h r t -> r h t")[:, :, cs], in_=QTs[i][0:64, 24:32, :]), reads=[QTs[i]])
                    S.dma("sp", "sVF%d" % i, lambda e: e.dma_start(out=vF[b, cs, :], in_=VF[i][:].rearrange("p h d -> p (h d)")), reads=[VF[i]])
                    S.dma("sp", "sVD%d" % i, lambda e: e.dma_start(out=vD[b, cs, :], in_=VD[i][:].rearrange("p h d -> p (h d)")), reads=[VD[i]])
                    S.dma("sp", "sGT%d" % i, lambda e: e.dma_start(out=gl[b, cs, :], in_=GT[i][:]), reads=[GT[i]])

                load_x(0)
                if NT > 1:
                    load_x(1)
                pre_a(0)
                pre_b(0)
                for t in range(NT):
                    if t + 1 < NT:
                        pre_a(t + 1)
                    if t + 2 < NT:
                        load_x(t + 2)
                    zstage(t)
                    if t + 1 < NT:
                        pre_b(t + 1)
                S.barrier()


        wcres = Res("wcast")
        CH = 64
        castjobs = []
        for (src, dst, key, c) in ((w1, w1c, "wc1", 4), (w3, w3c, "wc3", 4), (w2, w2c, "wc2", 2)):
            srcv = src.rearrange("(r c) n -> r (c n)", c=c)
            for r0 in range(0, 8192, CH):
                castjobs.append((srcv, dst, key, r0))
        castpos = [0]
        for key in ("wc1", "wc2", "wc3"):
            S.bg_keys.add(key)

        def bg_cast(n=1):
            for _ in range(n):
                if castpos[0] >= len(castjobs):
                    return
                srcv, dst, key, r0 = castjobs[castpos[0]]
                castpos[0] += 1
                S.dma("pool", key, lambda e: e.dma_start(out=dst[r0:r0 + CH, :], in_=srcv[r0:r0 + CH, :]), accum=[wcres])

        def phase2(b, o_all):
            with contextlib.ExitStack() as st:
                sbk = [ps(st, "sbk%d" % i, [128, 512], F32) for i in range(3)]
                obs = [ps(st, "ob%d" % i, [128, 4, 65], F32) for i in range(2)]
                od = ps(st, "od", [128, 3, 512], F32)
                pts = [sb(st, "pt%d" % i, [128, 512], BF16) for i in range(4)]
                cnt = [0]
                with contextlib.ExitStack() as st2:
                    QT = [sb(st2, "QTf%d" % i, [65, TT], BF16) for i in range(2)]
                    KT = [sb(st2, "KTf%d" % i, [65, TT], BF16) for i in range(2)]
                    VT = [sb(st2, "VTf%d" % i, [128, NT, 65], BF16) for i in range(2)]
                    rec = [sb(st2, "rec%d" % i, [128, 4], F32) for i in range(2)]

                    def load_f(h):
                        i = h % 2
                        S.dma("sp", "QTf%d" % i, lambda e: e.dma_start(out=QT[i][:], in_=qTf[b, h, :, :]), writes=[QT[i]])
                        S.dma("sp", "KTf%d" % i, lambda e: e.dma_start(out=KT[i][:], in_=kTf[b, h, :, :]), writes=[KT[i]])
                        S.dma("sp", "VTf%d" % i, lambda e: e.dma_start(
                            out=VT[i][:], in_=vF[b].rearrange("(t p) c -> p t c", p=128)[:, :, h * 65:(h + 1) * 65]), writes=[VT[i]])

                    tasks = [(h, g, kt) for h in range(8) for g in range(NG) for kt in range(4 * g + 4)]

                    def qk_f(p):
                        h, g, kt = tasks[p]
                        i = h % 2
                        c0 = 128 * max(kt - 4 * g, 0)
                        sbank = sbk[p % 3]
                        S.op("pe", lambda e: e.matmul(sbank[:, c0:512], lhsT=KT[i][:, kt * 128:(kt + 1) * 128],
                                                      rhs=QT[i][:, g * 512 + c0:(g + 1) * 512], start=True, stop=True),
                             reads=[KT[i], QT[i]], accum=[sbank])

                    def rest_f(p):
                        h, g, kt = tasks[p]
                        i = h % 2
                        j = kt - 4 * g
                        jb = max(j, 0)
                        c0 = 128 * jb
                        sbank = sbk[p % 3]
                        pt = pts[p % 4]
                        ob = obs[g % 2]
                        S.op("act", lambda e: e.activation(out=pt[:, c0:512], in_=sbank[:, c0:512], func=AF.Exp,
                                                           bias=ncum[:, b, kt, h:h + 1]), reads=[sbank, ncum], writes=[pt])
                        if j >= 0:
                            S.op("pool", lambda e: e.affine_select(out=pt[:, c0:c0 + 128], in_=pt[:, c0:c0 + 128], pattern=[[1, 128]],
                                                                   compare_op=ALU.is_ge, fill=0.0, base=0, channel_multiplier=-1),
                                 reads=[pt], writes=[pt])
                        for qb in range(jb, 4):
                            S.op("pe", lambda e, qb=qb: e.matmul(
                                ob[:, qb, :], lhsT=pt[:, qb * 128:(qb + 1) * 128], rhs=VT[i][:, kt, :],
                                start=(kt == 0 and qb == 0), stop=(kt == 4 * g + qb), skip_group_check=True), reads=[pt, VT[i]], accum=[ob])
                        if kt == 4 * g + 3:
                            r = rec[g % 2]
                            S.op("dve", lambda e: e.reciprocal(out=r[:], in_=ob[:, :, 64]), reads=[ob], writes=[r])
                            S.op("dve", lambda e: e.tensor_tensor(out=o_all[:, g * 4:(g + 1) * 4, h * 64:(h + 1) * 64], in0=ob[:, :, 0:64],
                                                                  in1=r[:].unsqueeze(2).to_broadcast([128, 4, 64]), op=ALU.mult),
                                 reads=[ob, r], accum=[o_all])

                    LA = 2
                    load_f(0)
                    for p in range(min(LA, len(tasks))):
                        if tasks[p][1] == 0 and tasks[p][2] == 0 and tasks[p][0] + 1 < 8:
                            pass
                        qk_f(p)
                    for p in range(len(tasks)):
                        h, g, kt = tasks[p]
                        if g == 0 and kt == 0 and h + 1 < 8:
                            load_f(h + 1)
                        if p + LA < len(tasks):
                            qk_f(p + LA)
                        rest_f(p)
                        if p % 5 == 4:
                            bg_cast()
                S.barrier()
                with contextlib.ExitStack() as st2:
                    QT2 = [sb(st2, "QTd%d" % i, [64, 2, TT], BF16) for i in range(2)]
                    KT2 = [sb(st2, "KTd%d" % i, [64, 2, TT], BF16) for i in range(2)]
                    VT2 = [sb(st2, "VTd%d" % i, [128, NT, 129], BF16) for i in range(2)]
                    onorm = sb(st2, "onorm", [128, 8, 128], F32)
                    oo = sb(st2, "oo", [128, 4, 128], F32)
                    sq2 = sb(st2, "sq2", [128, 4, 128], F32)
                    recd = sb(st2, "recd", [128, 8], F32)
                    msd = sb(st2, "msd", [128, 4], F32)

                    def load_d(hd):
                        i = hd % 2
                        S.dma("sp", "QTd%d" % i, lambda e: e.dma_start(
                            out=QT2[i][:], in_=qTd[b, hd * 2:hd * 2 + 2, :, :].rearrange("m r t -> r m t")), writes=[QT2[i]])
                        S.dma("sp", "KTd%d" % i, lambda e: e.dma_start(
                            out=KT2[i][:], in_=kTd[b, hd * 2:hd * 2 + 2, :, :].rearrange("m r t -> r m t")), writes=[KT2[i]])
                        S.dma("sp", "VTd%d" % i, lambda e: e.dma_start(
                            out=VT2[i][:], in_=vD[b].rearrange("(t p) c -> p t c", p=128)[:, :, hd * 129:(hd + 1) * 129]), writes=[VT2[i]])

                    tasks = [(hd, g, kt, m) for hd in range(4) for g in range(NG) for kt in range(4 * g + 4) for m in range(2)]

                    def qk_d(p):
                        hd, g, kt, m = tasks[p]
                        i = hd % 2
                        c0 = 128 * max(kt - 4 * g, 0)
                        sbank = sbk[p % 3]
                        S.op("pe", lambda e: e.matmul(sbank[:, c0:512], lhsT=KT2[i][:, m, kt * 128:(kt + 1) * 128],
                                                      rhs=QT2[i][:, m, g * 512 + c0:(g + 1) * 512], start=True, stop=True),
                             reads=[KT2[i], QT2[i]], accum=[sbank])

                    def rest_d(p):
                        hd, g, kt, m = tasks[p]
                        i = hd % 2
                        j = kt - 4 * g
                        jb = max(j, 0)
                        c0 = 128 * jb
                        sbank = sbk[p % 3]
                        pt = pts[p % 4]
                        S.op("act", lambda e: e.activation(out=pt[:, c0:512], in_=sbank[:, c0:512], func=AF.Exp),
                             reads=[sbank], writes=[pt])
                        if j >= 0:
                            S.op("pool", lambda e: e.memset(pt[64:128, c0:c0 + 64], 0.0), reads=[pt], writes=[pt])
                        for qb in range(jb, 4):
                            idx = m * 4 + qb
                            bank = idx // 3
                            off = (idx % 3) * 129
                            S.op("pe", lambda e, qb=qb, bank=bank, off=off, idx=idx: e.matmul(
                                od[:, bank, off:off + 129], lhsT=pt[:, qb * 128:(qb + 1) * 128], rhs=VT2[i][:, kt, :],
                                start=(kt == 0 and idx in (0, 3, 6)), stop=(kt == 4 * g + qb), skip_group_check=True), reads=[pt, VT2[i]], accum=[od])
                        if kt == 4 * g + 3 and m == 1:
                            for bank in range(3):
                                n = 3 if bank < 2 else 2
                                view = od[:, bank, 0:n * 129].rearrange("p (a c) -> p a c", c=129)
                                S.op("dve", lambda e, view=view, bank=bank, n=n: e.reciprocal(out=recd[:, bank * 3:bank * 3 + n], in_=view[:, :, 128]),
                                     reads=[od], accum=[recd])
                                S.op("dve", lambda e, view=view, bank=bank, n=n: e.tensor_tensor(
                                    out=onorm[:, bank * 3:bank * 3 + n, :], in0=view[:, :, 0:128],
                                    in1=recd[:, bank * 3:bank * 3 + n].unsqueeze(2).to_broadcast([128, n, 128]), op=ALU.mult),
                                     reads=[od, recd], accum=[onorm])
                            S.op("dve", lambda e: e.scalar_tensor_tensor(out=oo[:], in0=onorm[:, 4:8, :], scalar=nlam[:, 0:1], in1=onorm[:, 0:4, :],
                                                                         op0=ALU.mult, op1=ALU.add), reads=[onorm, nlam], writes=[oo])
                            S.op("pool", lambda e: e.tensor_tensor(out=sq2[:], in0=oo[:], in1=oo[:], op=ALU.mult), reads=[oo], writes=[sq2])
                            S.op("dve", lambda e: e.tensor_reduce(out=msd[:], in_=sq2[:], axis=AX.X, op=ALU.add), reads=[sq2], writes=[msd])
                            S.op("act", lambda e: e.activation(out=msd[:], in_=msd[:], func=AF.Ln, scale=1.0 / 128, bias=EPS), reads=[msd], writes=[msd])
                            S.op("act", lambda e: e.activation(out=msd[:], in_=msd[:], func=AF.Exp, scale=-0.5), reads=[msd], writes=[msd])
                            S.op("dve", lambda e: e.tensor_tensor(out=oo[:], in0=oo[:], in1=msd[:].unsqueeze(2).to_broadcast([128, 4, 128]), op=ALU.mult),
                                 reads=[oo, msd], writes=[oo])
                            S.op("pool", lambda e: e.tensor_tensor(out=o_all[:, g * 4:(g + 1) * 4, 512 + hd * 128:512 + (hd + 1) * 128], in0=oo[:],
                                                                   in1=gsub[:].unsqueeze(1).to_broadcast([128, 4, 128]), op=ALU.mult),
                                 reads=[oo, gsub], accum=[o_all])

                    LA = 2
                    load_d(0)
                    for p in range(min(LA, len(tasks))):
                        qk_d(p)
                    for p in range(len(tasks)):
                        hd, g, kt, m = tasks[p]
                        if g == 0 and kt == 0 and m == 0 and hd + 1 < 4:
                            load_d(hd + 1)
                        if p + LA < len(tasks):
                            qk_d(p + LA)
                        rest_d(p)
                        if p % 5 == 4:
                            bg_cast()
            S.barrier()
            if dbg:
                S.dma("sp", "dbgoc", lambda e: e.dma_start(out=dbg_t["oc"][b].rearrange("(t p) d -> p t d", p=128), in_=o_all[:]), reads=[o_all])

        def phase3(b, o_all):
            with contextlib.ExitStack() as st:
                wpf = sb(st, "wpf", [128, 4, D], BF16)
                wpd = sb(st, "wpd", [128, 4, D], BF16)
                wo = sb(st, "wo", [128, 8, D], BF16)
                wr = sb(st, "wr", [128, 8, 36], BF16)
                brt = sb(st, "brt", [128, 36], F32)
                S.dma("pool", "wpf", lambda e: e.dma_start(out=wpf[:], in_=w_pf.rearrange("(c k) n -> k c n", k=128)), writes=[wpf])
                S.dma("pool", "wpd", lambda e: e.dma_start(out=wpd[:], in_=w_pd.rearrange("(c k) n -> k c n", k=128)), writes=[wpd])
                S.dma("pool", "wo", lambda e: e.dma_start(out=wo[:], in_=w_out.rearrange("(c k) n -> k c n", k=128)), writes=[wo])
                S.dma("pool", "wr", lambda e: e.dma_start(out=wr[:, :, 0:4], in_=w_rg.rearrange("(c k) n -> k c n", k=128)), accum=[wr])
                S.dma("pool", "wr", lambda e: e.dma_start(out=wr[:, :, 4:36], in_=w_re.rearrange("(c k) n -> k c n", k=128)), accum=[wr])
                S.dma("sp", "brt", lambda e: e.dma_start(out=brt[:, 0:4], in_=bcast_rows(b_rg[0:1, :], 128)), accum=[brt])
                S.dma("sp", "brt", lambda e: e.dma_start(out=brt[:, 4:36], in_=bcast_rows(b_re[0:1, :], 128)), accum=[brt])
                gt1 = load_mod_row(st, "gt1", b, 2)
                A2 = load_mod_row(st, "A2", b, 3)
                B2 = load_mod_row(st, "B2", b, 4)
                NBUF = 2
                xt = [sb(st, "x3_%d" % i, [128, D], F32) for i in range(3)]
                gls = [sb(st, "gls%d" % i, [128, 2048], BF16) for i in range(3)]
                sgs = [sb(st, "sg0", [128, 2048], BF16)] * NBUF
                oTs = [sb(st, "oT0", [128, KC, 128], BF16)] * NBUF
                m1s = [sb(st, "m1_0", [128, D], F32)] * NBUF
                m2s = [sb(st, "m2_0", [128, D], F32)] * NBUF
                mgs = [sb(st, "mg%d" % i, [128, D], BF16) for i in range(NBUF)]
                mTs = [sb(st, "mT0", [128, KC, 128], BF16)] * NBUF
                x1 = [sb(st, "x1_%d" % i, [128, D], F32) for i in range(NBUF)]
                tmps = [sb(st, "tmp3_0", [128, D], F32)] * NBUF
                sqj = sb(st, "sqj", [128, D], BF16)
                h2 = [sb(st, "h2_%d" % i, [128, D], BF16) for i in range(NBUF)]
                h2T = sb(st, "h2T", [128, KC, 128], BF16)
                ss = sb(st, "ss3", [128, 4], F32)
                Lall = sb(st, "Lall", [128, NT, 36], F32)
                rsm = sb(st, "rsm", [128, 16, NT], F32)
                r4 = sb(st, "r4", [128, 3, NT * 4], F32)
                pT = ps(st, "pT3", [128, KC, 128], BF16)
                pfd = ps(st, "pfd", [128, 4, 512], F32)
                pyy = ps(st, "pyy", [128, 2, 512], F32)
                prt = ps(st, "prt", [128, 128], F32)

                def load3(t):
                    i = t % 3
                    S.dma("sp", "x3_%d" % i, lambda e: e.dma_start(out=xt[i][:], in_=x[b, t * 128:(t + 1) * 128, :]), writes=[xt[i]])
                    S.dma("sp", "gls%d" % i, lambda e: e.dma_start(out=gls[i][:], in_=gl[b, t * 128:(t + 1) * 128, :]), writes=[gls[i]])

                def transp(src_ap_fn, src_t, dstT, eng):
                    for kc in range(KC):
                        S.op("pe", lambda e, kc=kc: e.transpose(out=pT[:, kc, :], in_=src_ap_fn(kc), identity=ident_b[:]),
                             reads=[src_t, ident_b], accum=[pT])
                    if eng == "act":
                        S.op("act", lambda e: e.copy(out=dstT[:], in_=pT[:]), reads=[pT], writes=[dstT])
                    else:
                        S.op("dve", lambda e: e.tensor_copy(out=dstT[:], in_=pT[:]), reads=[pT], writes=[dstT])

                def stageA(t):
                    i = t % NBUF
                    oT, sg, m1, m2, mg = oTs[i], sgs[i], m1s[i], m2s[i], mgs[i]
                    transp(lambda kc: o_all[:, t, kc * 128:(kc + 1) * 128], o_all, oT, "act")
                    for nh in range(2):
                        for c in range(4):
                            S.op("pe", lambda e, nh=nh, c=c: e.matmul(pfd[:, nh, :], lhsT=oT[:, c, :], rhs=wpf[:, c, nh * 512:(nh + 1) * 512],
                                                                      start=(c == 0), stop=(c == 3)), reads=[oT, wpf], accum=[pfd])
                    for nh in range(2):
                        for c in range(4):
                            S.op("pe", lambda e, nh=nh, c=c: e.matmul(pfd[:, 2 + nh, :], lhsT=oT[:, 4 + c, :], rhs=wpd[:, c, nh * 512:(nh + 1) * 512],
                                                                      start=(c == 0), stop=(c == 3)), reads=[oT, wpd], accum=[pfd])
                    S.op("act", lambda e: e.activation(out=sg[:], in_=gls[t % 3][:], func=AF.Sigmoid), reads=[gls[t % 3]], writes=[sg])
                    S.op("dve", lambda e: e.tensor_tensor(out=m1[:], in0=pfd[:, 0:2, :].rearrange("p a n -> p (a n)"), in1=sg[:, 0:1024], op=ALU.mult),
                         reads=[pfd, sg], writes=[m1])
                    S.op("dve", lambda e: e.tensor_tensor(out=m2[:], in0=pfd[:, 2:4, :].rearrange("p a n -> p (a n)"), in1=sg[:, 1024:2048], op=ALU.mult),
                         reads=[pfd, sg], writes=[m2])
                    S.op("pool", lambda e: e.tensor_tensor(out=mg[:], in0=m1[:], in1=m2[:], op=ALU.add), reads=[m1, m2], writes=[mg])

                def stageB(t):
                    i = t % NBUF
                    tt = b * NT + t
                    mg, mT, tmp = mgs[i], mTs[i], tmps[i]
                    transp(lambda kc: mg[:, kc * 128:(kc + 1) * 128], mg, mT, "act")
                    for nh in range(2):
                        for c in range(KC):
                            S.op("pe", lambda e, nh=nh, c=c: e.matmul(pyy[:, nh, :], lhsT=mT[:, c, :], rhs=wo[:, c, nh * 512:(nh + 1) * 512],
                                                                      start=(c == 0), stop=(c == KC - 1)), reads=[mT, wo], accum=[pyy])
                    S.op("dve", lambda e: e.tensor_tensor(out=tmp[:], in0=pyy[:].rearrange("p a n -> p (a n)"), in1=gt1[:], op=ALU.mult),
                         reads=[pyy, gt1], writes=[tmp])
                    S.op("pool", lambda e: e.tensor_tensor(out=x1[i][:], in0=tmp[:], in1=xt[t % 3][:], op=ALU.add), reads=[tmp, xt[t % 3]], writes=[x1[i]])
                    S.dma("sp", "sx1_%d" % i, lambda e: e.dma_start(out=x1s[tt * 128:(tt + 1) * 128, :], in_=x1[i][:]), reads=[x1[i]])
                    S.op("act", lambda e: e.activation(out=sqj[:], in_=x1[i][:], func=AF.Square, accum_out=ss[:, 0:1]), reads=[x1[i]], writes=[sqj, ss])
                    S.op("act", lambda e: e.activation(out=ss[:, 1:2], in_=ss[:, 0:1], func=AF.Ln, scale=1.0 / D, bias=EPS), reads=[ss], accum=[ss])
                    S.op("act", lambda e: e.activation(out=ss[:, 2:3], in_=ss[:, 1:2], func=AF.Exp, scale=-0.5), reads=[ss], accum=[ss])
                    S.op("dve", lambda e: e.scalar_tensor_tensor(out=tmp[:], in0=x1[i][:], scalar=ss[:, 2:3], in1=A2[:], op0=ALU.mult, op1=ALU.mult),
                         reads=[x1[i], ss, A2], writes=[tmp])
                    S.op("pool", lambda e: e.tensor_tensor(out=h2[i][:], in0=tmp[:], in1=B2[:], op=ALU.add), reads=[tmp, B2], writes=[h2[i]])
                    S.dma("sp", "sh2_%d" % i, lambda e: e.dma_start(out=h2s[tt * 128:(tt + 1) * 128, :], in_=h2[i][:]), reads=[h2[i]])

                def stageC(t):
                    i = t % NBUF
                    tt = b * NT + t
                    transp(lambda kc: h2[i][:, kc * 128:(kc + 1) * 128], h2[i], h2T, "dve")
                    for c in range(KC):
                        S.op("pe", lambda e, c=c: e.matmul(prt[:, 0:36], lhsT=h2T[:, c, :], rhs=wr[:, c, :], start=(c == 0), stop=(c == KC - 1)),
                             reads=[h2T, wr], accum=[prt])
                    S.op("dve", lambda e: e.tensor_tensor(out=Lall[:, t, :], in0=prt[:, 0:36], in1=brt[:], op=ALU.add), reads=[prt, brt], accum=[Lall])

                load3(0)
                if NT > 1:
                    load3(1)
                stageA(0)
                for t in range(NT):
                    if t + 2 < NT:
                        load3(t + 2)
                    if t + 1 < NT:
                        stageA(t + 1)
                    if t >= 1:
                        stageC(t - 1)
                    stageB(t)
                stageC(NT - 1)
                TT_ = NT
                tt0 = b * NT
                BT = [m1s[0], m2s[0], tmps[0], xt[0], xt[1], xt[2], x1[0], x1[1]]

                def v32(tl):
                    return tl[:, 0:TT_ * 32].rearrange("p (t e) -> p t e", e=32)

                def v8(tl, k):
                    return tl[:, k * 256:k * 256 + TT_ * 8].rearrange("p (t e) -> p t e", e=8)

                def vec(k):
                    return rsm[:, k, :]

                def g4(k):
                    return r4[:, k, :].rearrange("p (t g) -> p t g", g=4)

                def bc(ap2, n):
                    return ap2.unsqueeze(2).to_broadcast([128, TT_, n])

                lg = Lall[:, :, 0:4]
                le4 = Lall[:, :, 4:36].rearrange("p t (g e) -> p t g e", e=8)
                T48, M1t, M2t, M12t, BASEt, RRt, TAt, SMt = BT
                mg4, zg, gw, m1v, m2v, dm, ex, w1_ = [vec(k) for k in range(8)]
                ohg, d4, eg = g4(0), g4(1), g4(2)
                e8, oh1, e8b, oh2 = v8(SMt, 0), v8(SMt, 1), v8(SMt, 2), v8(SMt, 3)
                R_ = [Lall, rsm, r4]

                def DV(fn, reads, writes, eng="dve"):
                    S.op(eng, fn, reads=reads, writes=writes)

                DV(lambda e: e.tensor_reduce(out=mg4, in_=lg, axis=AX.X, op=ALU.max), [Lall], [rsm])
                DV(lambda e: e.tensor_tensor(out=ohg, in0=lg, in1=bc(mg4, 4), op=ALU.is_equal), [Lall, rsm], [r4])
                DV(lambda e: e.tensor_tensor(out=d4, in0=lg, in1=bc(mg4, 4), op=ALU.subtract), [Lall, rsm, r4], [r4])
                DV(lambda e: e.activation(out=eg, in_=d4, func=AF.Exp), [r4], [r4], eng="act")
                DV(lambda e: e.tensor_reduce(out=zg, in_=eg, axis=AX.X, op=ALU.add), [r4, rsm], [rsm])
                DV(lambda e: e.reciprocal(out=gw, in_=zg), [rsm], [rsm])
                t48v = v32(T48).rearrange("p t (g e) -> p t g e", e=8)
                DV(lambda e: e.tensor_tensor(out=t48v, in0=le4, in1=ohg.unsqueeze(3).to_broadcast([128, TT_, 4, 8]), op=ALU.mult), [Lall, r4], [T48])
                DV(lambda e: e.tensor_reduce(out=e8, in_=t48v.rearrange("p t g e -> p t e g"), axis=AX.X, op=ALU.add), [T48], [SMt])
                DV(lambda e: e.tensor_reduce(out=m1v, in_=e8, axis=AX.X, op=ALU.max), [SMt, rsm], [rsm])
                DV(lambda e: e.tensor_tensor(out=oh1, in0=e8, in1=bc(m1v, 8), op=ALU.is_equal), [SMt, rsm], [SMt])
                DV(lambda e: e.scalar_tensor_tensor(out=e8b, in0=oh1, scalar=-1e30, in1=e8, op0=ALU.mult, op1=ALU.add), [SMt], [SMt])
                DV(lambda e: e.tensor_reduce(out=m2v, in_=e8b, axis=AX.X, op=ALU.max), [SMt, rsm], [rsm])
                DV(lambda e: e.tensor_tensor(out=oh2, in0=e8b, in1=bc(m2v, 8), op=ALU.is_equal), [SMt, rsm], [SMt])
                DV(lambda e: e.tensor_tensor(out=dm, in0=m2v, in1=m1v, op=ALU.subtract), [rsm], [rsm])
                DV(lambda e: e.activation(out=ex, in_=dm, func=AF.Exp), [rsm], [rsm], eng="act")
                DV(lambda e: e.tensor_scalar(out=ex, in0=ex, scalar1=1.0, scalar2=None, op0=ALU.add), [rsm], [rsm])
                DV(lambda e: e.reciprocal(out=w1_, in_=ex), [rsm], [rsm])
                S.op("dve", lambda e: e.tensor_tensor(out=wt_all[:, tt0:tt0 + TT_, 0], in0=w1_, in1=gw, op=ALU.mult), reads=[rsm], accum=[wt_all])
                S.op("dve", lambda e: e.tensor_tensor(out=wt_all[:, tt0:tt0 + TT_, 1], in0=gw, in1=wt_all[:, tt0:tt0 + TT_, 0], op=ALU.subtract),
                     reads=[rsm, wt_all], accum=[wt_all])
                M1v = v32(M1t).rearrange("p t (g e) -> p t g e", e=8)
                M2v = v32(M2t).rearrange("p t (g e) -> p t g e", e=8)
                DV(lambda e: e.tensor_tensor(out=M1v, in0=ohg.unsqueeze(3).to_broadcast([128, TT_, 4, 8]),
                                            in1=oh1.unsqueeze(2).to_broadcast([128, TT_, 4, 8]), op=ALU.mult), [r4, SMt], [M1t])
                DV(lambda e: e.tensor_tensor(out=M2v, in0=ohg.unsqueeze(3).to_broadcast([128, TT_, 4, 8]),
                                            in1=oh2.unsqueeze(2).to_broadcast([128, TT_, 4, 8]), op=ALU.mult), [r4, SMt], [M2t])
                DV(lambda e: e.tensor_tensor(out=v32(M12t), in0=v32(M1t), in1=v32(M2t), op=ALU.add), [M1t, M2t], [M12t])
                io_b = iota32[:].unsqueeze(1).to_broadcast([128, TT_, 32])
                for k, Mt in ((0, M1t), (1, M2t)):
                    DV(lambda e, Mt=Mt: e.tensor_tensor(out=v32(TAt), in0=v32(Mt), in1=io_b, op=ALU.mult), [Mt, iota32], [TAt])
                    S.op("dve", lambda e, k=k: e.tensor_reduce(out=eid_all[:, tt0:tt0 + TT_, k], in_=v32(TAt), axis=AX.X, op=ALU.add),
                         reads=[TAt], accum=[eid_all])
                NCOL = TT_ * 32
                pw = pfd[:, 0:2, :].rearrange("p a n -> p (a n)")
                pc_ = pfd[:, 2:4, :].rearrange("p a n -> p (a n)")
                for c0 in range(0, NCOL, 512):
                    c1 = min(c0 + 512, NCOL)
                    S.op("pe", lambda e, c0=c0, c1=c1: e.matmul(pw[:, c0:c1], lhsT=tri_s[:], rhs=M12t[:, c0:c1], start=True, stop=True),
                         reads=[tri_s, M12t], accum=[pfd])
                    S.op("pe", lambda e, c0=c0, c1=c1: e.matmul(pc_[:, c0:c1], lhsT=ones_f[:], rhs=M12t[:, c0:c1], start=True, stop=True),
                         reads=[ones_f, M12t], accum=[pfd])
                DV(lambda e: e.tensor_copy(out=v32(TAt), in_=pc_[:, 0:NCOL].rearrange("p (t e) -> p t e", e=32)), [pfd], [TAt])
                for t in range(TT_):
                    S.op("pool", lambda e, t=t: e.tensor_copy(out=BASEt[:, t * 32:(t + 1) * 32], in_=ecarry[:]), reads=[ecarry], accum=[BASEt])
                    S.op("pool", lambda e, t=t: e.tensor_tensor(out=ecarry[:], in0=ecarry[:], in1=TAt[:, t * 32:(t + 1) * 32], op=ALU.add),
                         reads=[ecarry, TAt, BASEt], writes=[ecarry])
                DV(lambda e: e.tensor_tensor(out=v32(RRt), in0=pw[:, 0:NCOL].rearrange("p (t e) -> p t e", e=32), in1=v32(BASEt), op=ALU.add),
                  [pfd, BASEt], [RRt])
                for k, Mt in ((0, M1t), (1, M2t)):
                    DV(lambda e, Mt=Mt: e.tensor_tensor(out=v32(TAt), in0=v32(RRt), in1=v32(Mt), op=ALU.mult), [RRt, Mt], [TAt])
                    S.op("dve", lambda e, k=k: e.tensor_reduce(out=rk_all[:, tt0:tt0 + TT_, k], in_=v32(TAt), axis=AX.X, op=ALU.add),
                         reads=[TAt], accum=[rk_all])
            S.barrier()

        def phase3b():
            S.bg_keys.clear()
            with contextlib.ExitStack() as st:
                padf = sb(st, "padf", [128, 32], F32)
                pend = sb(st, "pend", [128, 32], F32)
                pstart = sb(st, "pstart", [128, 32], F32)
                one32 = sb(st, "one32", [128, 32], F32)
                bst_i = sb(st, "bst_i", [128, NBLK], I32)
                bst_f = sb(st, "bst_f", [128, NBLK], F32)
                S.op("pool", lambda e: e.iota(bst_i[:], pattern=[[BS, NBLK]], base=0, channel_multiplier=0), writes=[bst_i])
                S.op("dve", lambda e: e.tensor_copy(out=bst_f[:], in_=bst_i[:]), reads=[bst_i], writes=[bst_f])
                cmpc = sb(st, "cmpc", [128, 32, NBLK], F32)
                S.op("dve", lambda e: e.tensor_tensor(out=cmpc[:], in0=ecarry[:].unsqueeze(2).to_broadcast([128, 32, NBLK]),
                                                      in1=bst_f[:].unsqueeze(1).to_broadcast([128, 32, NBLK]), op=ALU.is_gt),
                     reads=[ecarry, bst_f], writes=[cmpc])
                S.op("dve", lambda e: e.tensor_reduce(out=padf[:], in_=cmpc[:], axis=AX.X, op=ALU.add), reads=[cmpc], writes=[padf])
                S.op("dve", lambda e: e.tensor_scalar(out=padf[:], in0=padf[:], scalar1=float(BS), scalar2=None, op0=ALU.mult), reads=[padf], writes=[padf])
                S.op("pool", lambda e: e.memset(one32[:], 1.0), writes=[one32])
                S.op("dve", lambda e: e.tensor_tensor_scan(out=pend[:], data0=one32[:], data1=padf[:], initial=0.0, op0=ALU.mult, op1=ALU.add),
                     reads=[one32, padf], writes=[pend])
                S.op("dve", lambda e: e.tensor_tensor(out=pstart[:], in0=pend[:], in1=padf[:], op=ALU.subtract), reads=[pend, padf], writes=[pstart])
                big = sb(st, "big", [128, NTT, 32], F32)
                dsf = sb(st, "dsf", [128, NTT, 2], F32)
                for k in range(2):
                    S.op("dve", lambda e, k=k: e.tensor_tensor(out=big[:], in0=iota32[:].unsqueeze(1).to_broadcast([128, NTT, 32]),
                                                               in1=eid_all[:, :, k:k + 1].to_broadcast([128, NTT, 32]), op=ALU.is_equal),
                         reads=[iota32, eid_all], writes=[big])
                    S.op("dve", lambda e: e.tensor_tensor(out=big[:], in0=big[:], in1=pstart[:].unsqueeze(1).to_broadcast([128, NTT, 32]), op=ALU.mult),
                         reads=[big, pstart], writes=[big])
                    S.op("dve", lambda e, k=k: e.tensor_reduce(out=dsf[:, :, k:k + 1], in_=big[:], axis=AX.X, op=ALU.add), reads=[big], accum=[dsf])
                S.op("dve", lambda e: e.tensor_tensor(out=dsf[:], in0=dsf[:], in1=rk_all[:], op=ALU.add), reads=[dsf, rk_all], writes=[dsf])
                S.op("dve", lambda e: e.tensor_copy(out=dest_i[:], in_=dsf[:]), reads=[dsf], writes=[dest_i])
                cmpb = sb(st, "cmpb", [128, NBLK, 32], F32)
                S.op("dve", lambda e: e.tensor_tensor(out=cmpb[:], in0=pend[:].unsqueeze(1).to_broadcast([128, NBLK, 32]),
                                                      in1=bst_f[:].unsqueeze(2).to_broadcast([128, NBLK, 32]), op=ALU.is_le),
                     reads=[pend, bst_f], writes=[cmpb])
                BE = sb(st, "BE", [128, NBLK], F32)
                S.op("dve", lambda e: e.tensor_reduce(out=BE[:], in_=cmpb[:], axis=AX.X, op=ALU.add), reads=[cmpb], writes=[BE])
                S.op("dve", lambda e: e.tensor_scalar(out=BE[:], in0=BE[:], scalar1=float(NEXP - 1), scalar2=None, op0=ALU.min), reads=[BE], writes=[BE])
                bpc_i = sb(st, "bpc_i", [128, 1], I32)
                bpc = sb(st, "bpc", [128, 1], F32)
                S.op("pool", lambda e: e.iota(bpc_i[:], pattern=[[0, 1]], base=0, channel_multiplier=2), writes=[bpc_i])
                S.op("dve", lambda e: e.tensor_copy(out=bpc[:], in_=bpc_i[:]), reads=[bpc_i], writes=[bpc])
                idf = sb(st, "idf", [128, NBLK, 2], F32)
                idx1 = blkidx["idx1"]
                S.op("dve", lambda e: e.tensor_scalar(out=idf[:, :, 0], in0=BE[:], scalar1=256.0, scalar2=bpc[:, 0:1], op0=ALU.mult, op1=ALU.add),
                     reads=[BE, bpc], writes=[idf])
                S.op("dve", lambda e: e.tensor_scalar(out=idf[:, :, 1], in0=idf[:, :, 0], scalar1=1.0, scalar2=None, op0=ALU.add),
                     reads=[idf], accum=[idf])
                S.op("dve", lambda e: e.tensor_copy(out=idx1[:], in_=idf[:]), reads=[idf], writes=[idx1])
                if dbg:
                    S.dma("sp", "dbgbe", lambda e: e.dma_start(out=dbg_t["be"][:, 0:NBLK], in_=BE[:]), reads=[BE])
                    S.dma("sp", "dbgbe", lambda e: e.dma_start(out=dbg_t["be"][:, NBLK:NBLK + 32], in_=pend[:]), reads=[pend])
                    S.dma("sp", "dbgbe", lambda e: e.dma_start(out=dbg_t["be"][:, NBLK + 32:NBLK + 64], in_=ecarry[:]), reads=[ecarry])
                    S.dma("sp", "dbgrt", lambda e: e.dma_start(out=dbg_t["rt"][:, :, 0:2], in_=eid_all[:]), reads=[eid_all])
                    S.dma("sp", "dbgrt", lambda e: e.dma_start(out=dbg_t["rt"][:, :, 2:4], in_=wt_all[:]), reads=[wt_all])
                    S.dma("sp", "dbgrt", lambda e: e.dma_start(out=dbg_t["rt"][:, :, 4:6], in_=dsf[:]), reads=[dsf])
                    S.dma("sp", "dbgrt", lambda e: e.dma_start(out=dbg_t["rt"][:, :, 6:8], in_=rk_all[:]), reads=[rk_all])
                hb_ = [sb(st, "h2l%d" % i, [128, D], BF16) for i in range(2)]
                for tt in range(NTT):
                    i = tt % 2
                    S.dma("sp", "h2l%d" % i, lambda e: e.dma_start(out=hb_[i][:], in_=h2s[tt * 128:(tt + 1) * 128, :]), writes=[hb_[i]])
                    for k in range(2):
                        S.dma("pool", "h2sc%d" % i, lambda e, k=k: e.indirect_dma_start(
                            out=xbuf[:, :], out_offset=bass.IndirectOffsetOnAxis(ap=dest_i[:, tt, k:k + 1], axis=0),
                            in_=hb_[i][:], in_offset=None), reads=[hb_[i], dest_i, xz])
            S.barrier()

        def phase4():
            idx1 = blkidx["idx1"]
            bg_cast(len(castjobs))
            w1v, w3v, w2v = w1c, w3c, w2c
            SUB = BS // 128
            with contextlib.ExitStack() as st:
                w1b = [sb(st, "w1b%d" % i, [128, 8, DEXP], BF16) for i in range(3)]
                w3b = [sb(st, "w3b%d" % i, [128, 8, DEXP], BF16) for i in range(3)]
                w2b = [sb(st, "w2b%d" % i, [128, 4, D], BF16) for i in range(4)]
                xb = [sb(st, "xb%d" % i, [128, SUB, D], BF16) for i in range(3)]
                xTs = [sb(st, "xT%d" % i, [128, KC, BS], BF16) for i in range(2)]
                sact = [sb(st, "sact%d" % i, [128, BS], F32) for i in range(2)]
                gTs = [sb(st, "gT%d" % i, [128, 4, BS], BF16) for i in range(2)]
                yo = [sb(st, "yo%d" % i, [128, SUB, D], BF16) for i in range(2)]
                pX = ps(st, "pX", [128, KC, BS], BF16)
                ph = ps(st, "ph", [128, 4, 512], F32)
                py = ps(st, "py", [128, 2, 512], F32)

                def load_blk(bi):
                    i = bi % 3
                    for hf in range(2):
                        off = bass.IndirectOffsetOnAxis(ap=idx1[:, bi, hf:hf + 1], axis=0)
                        S.dma("pool", "w1b%d" % i, lambda e, hf=hf: e.indirect_dma_start(
                            out=w1b[i][:, hf * 4:(hf + 1) * 4, :].rearrange("p c n -> p (c n)"), out_offset=None,
                            in_=w1v[:, :], in_offset=off), reads=[idx1, wcres], accum=[w1b[i]])
                        S.dma("pool", "w3b%d" % i, lambda e, hf=hf: e.indirect_dma_start(
                            out=w3b[i][:, hf * 4:(hf + 1) * 4, :].rearrange("p c n -> p (c n)"), out_offset=None,
                            in_=w3v[:, :], in_offset=off), reads=[idx1, wcres], accum=[w3b[i]])
                        S.dma("pool", "w2b%d" % (bi % 4), lambda e, hf=hf: e.indirect_dma_start(
                            out=w2b[bi % 4][:, hf * 2:(hf + 1) * 2, :].rearrange("p c n -> p (c n)"), out_offset=None,
                            in_=w2v[:, :], in_offset=off), reads=[idx1, wcres], accum=[w2b[bi % 4]])
                    S.dma("sp", "xb%d" % i, lambda e: e.dma_start(out=xb[i][:], in_=xbuf[bi * BS:(bi + 1) * BS, :].rearrange("(s p) d -> p s d", p=128)),
                          writes=[xb[i]])

                def stX(bi):
                    i = bi % 2
                    xT = xTs[i]
                    for s_ in range(SUB):
                        for c in range(KC):
                            S.op("pe", lambda e, s_=s_, c=c: e.transpose(out=pX[:, c, s_ * 128:(s_ + 1) * 128], in_=xb[bi % 3][:, s_, :].rearrange("p (q c) -> p c q", c=8)[:, c, :],
                                                                         identity=ident_b[:]), reads=[xb[bi % 3], ident_b], accum=[pX])
                    S.op("act", lambda e: e.copy(out=xT[:], in_=pX[:]), reads=[pX], writes=[xT])

                def stH(bi):
                    i = bi % 2
                    xT, gT = xTs[i], gTs[i]
                    for fc in range(4):
                        for c in range(KC):
                            S.op("pe", lambda e, fc=fc, c=c: e.matmul(ph[:, fc, 0:BS], lhsT=w1b[bi % 3][:, c, :].rearrange("p (q f) -> p f q", f=4)[:, fc, :], rhs=xT[:, c, :],
                                                                      start=(c == 0), stop=(c == KC - 1), skip_group_check=True), reads=[w1b[bi % 3], xT], accum=[ph])
                        for c in range(KC):
                            S.op("pe", lambda e, fc=fc, c=c: e.matmul(ph[:, fc, 256:256 + BS], lhsT=w3b[bi % 3][:, c, :].rearrange("p (q f) -> p f q", f=4)[:, fc, :], rhs=xT[:, c, :],
                                                                      start=(c == 0), stop=(c == KC - 1), skip_group_check=True), reads=[w3b[bi % 3], xT], accum=[ph])
                        sa = sact[fc % 2]
                        S.op("act", lambda e, fc=fc, sa=sa: e.activation(out=sa[:], in_=ph[:, fc, 0:BS], func=AF.Silu), reads=[ph], writes=[sa])
                        S.op("dve", lambda e, fc=fc, sa=sa: e.tensor_tensor(out=gT[:, fc, :], in0=sa[:], in1=ph[:, fc, 256:256 + BS], op=ALU.mult),
                             reads=[sa, ph], accum=[gT])

                def stY(bi):
                    i = bi % 2
                    gT = gTs[i]
                    w2t = w2b[bi % 4]
                    for s_ in range(SUB):
                        for nh in range(2):
                            for fc in range(4):
                                S.op("pe", lambda e, s_=s_, nh=nh, fc=fc: e.matmul(py[:, nh, :], lhsT=gT[:, fc, s_ * 128:(s_ + 1) * 128],
                                                                                   rhs=w2t[:, fc, nh * 512:(nh + 1) * 512],
                                                                                   start=(fc == 0), stop=(fc == 3)), reads=[gT, w2t], accum=[py])
                        if s_ % 2 == 0:
                            S.op("act", lambda e, s_=s_: e.copy(out=yo[i][:, s_, :], in_=py[:].rearrange("p a n -> p (a n)")), reads=[py], accum=[yo[i]])
                        else:
                            S.op("dve", lambda e, s_=s_: e.tensor_copy(out=yo[i][:, s_, :], in_=py[:].rearrange("p a n -> p (a n)")), reads=[py], accum=[yo[i]])
                    S.dma("sp", "yo%d" % i, lambda e: e.dma_start(out=ybuf[bi * BS:(bi + 1) * BS, :].rearrange("(s p) d -> p s d", p=128), in_=yo[i][:]),
                          reads=[yo[i]])

                load_blk(0)
                if NBLK > 1:
                    load_blk(1)
                stX(0)
                for bi in range(NBLK):
                    if bi + 2 < NBLK:
                        load_blk(bi + 2)
                    stH(bi)
                    if bi + 1 < NBLK:
                        stX(bi + 1)
                    if bi >= 1:
                        stY(bi - 1)
                stY(NBLK - 1)
            S.barrier()

        def phase5():
            with contextlib.ExitStack() as st:
                gt2 = [load_mod_row(st, "gt2_%d" % bb, bb, 5) for bb in range(NB)]
                ya = [sb(st, "ya%d" % i, [128, D], BF16) for i in range(3)]
                yb = [sb(st, "yb%d" % i, [128, D], BF16) for i in range(3)]
                xl = [sb(st, "xl%d" % i, [128, D], F32) for i in range(3)]
                ma = [sb(st, "ma%d" % i, [128, D], F32) for i in range(2)]
                mb = [sb(st, "mb%d" % i, [128, D], F32) for i in range(2)]
                oo_ = [sb(st, "oo5_%d" % i, [128, D], F32) for i in range(2)]

                def load5(tt):
                    i = tt % 3
                    S.dma("pool", "ya%d" % i, lambda e: e.indirect_dma_start(
                        out=ya[i][:], out_offset=None, in_=ybuf[:, :], in_offset=bass.IndirectOffsetOnAxis(ap=dest_i[:, tt, 0:1], axis=0)),
                        reads=[dest_i], writes=[ya[i]])
                    S.dma("pool", "yb%d" % i, lambda e: e.indirect_dma_start(
                        out=yb[i][:], out_offset=None, in_=ybuf[:, :], in_offset=bass.IndirectOffsetOnAxis(ap=dest_i[:, tt, 1:2], axis=0)),
                        reads=[dest_i], writes=[yb[i]])
                    S.dma("sp", "xl%d" % i, lambda e: e.dma_start(out=xl[i][:], in_=x1s[tt * 128:(tt + 1) * 128, :]), writes=[xl[i]])

                load5(0)
                if NTT > 1:
                    load5(1)
                for tt in range(NTT):
                    i = tt % 2
                    j = tt % 3
                    bb = tt // NT
                    t = tt % NT
                    if tt + 2 < NTT:
                        load5(tt + 2)
                    S.op("dve", lambda e: e.tensor_scalar(out=ma[i][:], in0=ya[j][:], scalar1=wt_all[:, tt, 0:1], scalar2=None, op0=ALU.mult),
                         reads=[ya[j], wt_all], writes=[ma[i]])
                    S.op("dve", lambda e: e.scalar_tensor_tensor(out=mb[i][:], in0=yb[j][:], scalar=wt_all[:, tt, 1:2], in1=ma[i][:],
                                                                 op0=ALU.mult, op1=ALU.add), reads=[yb[j], wt_all, ma[i]], writes=[mb[i]])
                    S.op("dve", lambda e: e.tensor_tensor(out=ma[i][:], in0=mb[i][:], in1=gt2[bb][:], op=ALU.mult), reads=[mb[i], gt2[bb]], writes=[ma[i]])
                    S.op("pool", lambda e: e.tensor_tensor(out=oo_[i][:], in0=ma[i][:], in1=xl[j][:], op=ALU.add), reads=[ma[i], xl[j]], writes=[oo_[i]])
                    S.dma("sp", "oo5_%d" % i, lambda e: e.dma_start(out=out[bb, t * 128:(t + 1) * 128, :], in_=oo_[i][:]), reads=[oo_[i]])
            S.barrier()

        for b in range(NB):
            phase1(b)
            if upto >= 2:
                with contextlib.ExitStack() as bst:
                    o_all = sb(bst, "o_all", [128, NT, D], BF16)
                    phase2(b, o_all)
                    if upto >= 3:
                        phase3(b, o_all)
        if upto >= 4:
            phase3b()
            phase4()
        if upto >= 5:
            phase5()
        S.barrier()
        S.final_wait()
    print("instructions:", S.n_ins, "dma sems:", len(S.dsem), "eng sems:", len(S.all_sems))
    return nc


_INVF = (10000.0 ** (-np.arange(0, 64, 2, dtype=np.float32) / np.float32(64))).astype(np.float32).reshape(1, 32)


def make_in_map(inp, core, NB):
    sl = slice(core * NB, (core + 1) * NB)
    f = lambda a: np.ascontiguousarray(a)
    m = {
        "x": f(inp["x"][sl]), "c": f(inp["c"][sl]), "positions": f(inp["positions"][sl]).astype(np.int32),
        "w_ada": f(inp["w_ada"][0]), "b_ada": f(inp["b_ada"][0:1]), "g_norm1": f(inp["g_norm1"][0:1]),
        "w_in": f(inp["w_in"][0]), "b_f": f(inp["b_f"][0:1]),
        "g_q_fox": f(inp["g_q_fox"][0:1]), "g_k_fox": f(inp["g_k_fox"][0:1]),
        "g_q_diff": f(inp["g_q_diff"][0:1]), "g_k_diff": f(inp["g_k_diff"][0:1]),
        "lam_q1": f(inp["lam_q1"][0:1]), "lam_k1": f(inp["lam_k1"][0:1]),
        "lam_q2": f(inp["lam_q2"][0:1]), "lam_k2": f(inp["lam_k2"][0:1]),
        "g_subln": f(inp["g_subln"][0:1]),
        "w_proj_fox": f(inp["w_proj_fox"][0]), "w_proj_diff": f(inp["w_proj_diff"][0]), "w_out": f(inp["w_out"][0]),
        "g_norm2": f(inp["g_norm2"][0:1]),
        "w_router_group": f(inp["w_router_group"][0]), "b_router_group": f(inp["b_router_group"][0:1]),
        "w_router_expert": f(inp["w_router_expert"][0]), "b_router_expert": f(inp["b_router_expert"][0:1]),
        "w1": f(inp["w1"][0]).reshape(NEXP * D, DEXP), "w3": f(inp["w3"][0]).reshape(NEXP * D, DEXP),
        "w2": f(inp["w2"][0]).reshape(NEXP * DEXP, D),
        "invf": _INVF,
    }
    return m


def kernel(**inputs):
    inp = {k: np.asarray(v) for k, v in inputs.items()}
    B, TT, _ = inp["x"].shape
    NB = B // N_CORES
    nc = build_nc(NB, TT)
    in_maps = [make_in_map(inp, c, NB) for c in range(N_CORES)]
    res = run_bass_kernel_spmd(nc, in_maps, core_ids=list(range(N_CORES)))
    return np.concatenate([np.asarray(r["out"]) for r in res.results], axis=0).astype(np.float32)
```
